# Optimizing a Trainium2 kernel written in Bass

```python
import math
import jax, jax.numpy as jnp
from jax import lax
import numpy as np

D_MODEL = 1024
BATCH = 2
SEQ = 16384
DEPTH = 2

N_HEADS_ATTN = 8
HEAD_DIM = 64
N_KV_GROUPS = 2
HEADS_PER_GROUP = N_HEADS_ATTN // N_KV_GROUPS
D_ATTN = N_HEADS_ATTN * HEAD_DIM
D_KV = N_KV_GROUPS * HEAD_DIM
D_CONV = D_MODEL - D_ATTN
CONV_WIDTH = 3
CMP_BLOCK = 32
CMP_STRIDE = 16
CMP_OVERLAP = CMP_BLOCK // CMP_STRIDE
CMP_HIDDEN = 256
SEL_BLOCK = 64
SEL_RATIO = SEL_BLOCK // CMP_STRIDE
N_SELECT = 16
WINDOW = 512
Q_BLOCK = 128
ROPE_THETA = 500000.0
ROPE_DIM = HEAD_DIM // 4
D_FF = 2816
N_GATES = 3 * N_HEADS_ATTN
D_IN_PROJ = D_ATTN + 6 * D_KV + N_GATES + 3 * D_CONV
EPS = 1e-6
NEG_INF = -1e30
FORCE_SCORE = 1e9
MAX_POS_OFFSET = 1024
SCALE = 1.0 / math.sqrt(HEAD_DIM)

kernel_name = "hybrid_nsa_shortconv_macaron"


def rms_norm(x, g):
    xf = x.astype(jnp.float32)
    y = xf * lax.rsqrt(jnp.mean(xf * xf, axis=-1, keepdims=True) + EPS)
    return (y * g.astype(jnp.float32)).astype(x.dtype)


def swiglu(x, w_gate, w_up, w_down):
    return (jax.nn.silu(x @ w_gate) * (x @ w_up)) @ w_down


def partial_rope(x, positions):
    half = ROPE_DIM // 2
    inv_freq = ROPE_THETA ** (-jnp.arange(half, dtype=jnp.float32) * 2.0 / ROPE_DIM)
    ang = positions.astype(jnp.float32)[..., None] * inv_freq
    cos = jnp.cos(ang)[:, :, None, :]
    sin = jnp.sin(ang)[:, :, None, :]
    xf = x.astype(jnp.float32)
    x1, x2, rest = xf[..., :half], xf[..., half:ROPE_DIM], xf[..., ROPE_DIM:]
    out = jnp.concatenate([x1 * cos - x2 * sin, x2 * cos + x1 * sin, rest], axis=-1)
    return out.astype(x.dtype)


def compress_blocks(k, pe, w1, w2):
    B, S, G, dk = k.shape
    n_chunks = S // CMP_STRIDE
    n_cmp = n_chunks - CMP_OVERLAP + 1
    chunks = k.reshape(B, n_chunks, CMP_STRIDE, G, dk)
    blocks = jnp.concatenate([chunks[:, j:j + n_cmp] for j in range(CMP_OVERLAP)], axis=2)
    blocks = blocks + pe[None, None, :, None, :]
    flat = blocks.transpose(0, 1, 3, 2, 4).reshape(B, n_cmp, G, CMP_BLOCK * dk)
    return jax.nn.gelu(flat @ w1) @ w2


def nsa_group(q, k_cmp, v_cmp, k_slc, v_slc, k_win, v_win, gates, positions,
              pe_k, w1_k, w2_k, pe_v, w1_v, w2_v):
    B, S = q.shape[:2]
    H, G, HPG, dk = N_HEADS_ATTN, N_KV_GROUPS, HEADS_PER_GROUP, HEAD_DIM
    q_raw = q.reshape(B, S, H, dk)
    q_rot = partial_rope(q_raw, positions)
    k_slc = partial_rope(k_slc.reshape(B, S, G, dk), positions)
    v_slc = v_slc.reshape(B, S, G, dk)
    k_win = partial_rope(k_win.reshape(B, S, G, dk), positions)
    v_win = v_win.reshape(B, S, G, dk)
    kc = compress_blocks(k_cmp.reshape(B, S, G, dk), pe_k, w1_k, w2_k)
    vc = compress_blocks(v_cmp.reshape(B, S, G, dk), pe_v, w1_v, w2_v)
    n_cmp = kc.shape[1]
    cmp_end = jnp.arange(n_cmp) * CMP_STRIDE + CMP_BLOCK - 1
    n_sel = S // SEL_BLOCK
    k_top = min(N_SELECT, n_sel)
    kb = k_slc.reshape(B, n_sel, SEL_BLOCK, G, dk).transpose(0, 3, 1, 2, 4)
    vb = v_slc.reshape(B, n_sel, SEL_BLOCK, G, dk).transpose(0, 3, 1, 2, 4)
    kw = jnp.pad(k_win, ((0, 0), (WINDOW, 0), (0, 0), (0, 0)))
    vw = jnp.pad(v_win, ((0, 0), (WINDOW, 0), (0, 0), (0, 0)))
    g = jax.nn.sigmoid(gates.astype(jnp.float32)).reshape(B, S, H, 3)
    agg_w = [float(c) for c in np.convolve(np.ones(SEL_RATIO), np.ones(CMP_OVERLAP))]
    pad_front = CMP_OVERLAP - 1
    pad_back = SEL_RATIO * n_sel + len(agg_w) - 1 - SEL_RATIO - n_cmp - pad_front + 1
    b_idx = jnp.arange(B)[:, None, None, None]
    g_idx = jnp.arange(G)[None, :, None, None]
    blk_j = jnp.arange(n_sel)

    def one_block(qb):
        s0 = qb * Q_BLOCK
        t = s0 + jnp.arange(Q_BLOCK)
        qr = lax.dynamic_slice_in_dim(q_raw, s0, Q_BLOCK, axis=1).reshape(B, Q_BLOCK, G, HPG, dk)
        qs = lax.dynamic_slice_in_dim(q_rot, s0, Q_BLOCK, axis=1).reshape(B, Q_BLOCK, G, HPG, dk)
        gb = lax.dynamic_slice_in_dim(g, s0, Q_BLOCK, axis=1)
        sc = jnp.einsum('bqghd,bngd->bghqn', qr, kc).astype(jnp.float32) * SCALE
        valid_c = cmp_end[None, :] <= t[:, None]
        pc = jax.nn.softmax(jnp.where(valid_c, sc, NEG_INF), axis=-1) * valid_c.astype(jnp.float32)
        o_cmp = jnp.einsum('bghqn,bngd->bqghd', pc.astype(vc.dtype), vc)
        imp = jnp.pad(pc.sum(axis=2), ((0, 0), (0, 0), (0, 0), (pad_front, pad_back)))
        p_slc = agg_w[0] * imp[..., 0:SEL_RATIO * n_sel:SEL_RATIO]
        for o in range(1, len(agg_w)):
            p_slc = p_slc + agg_w[o] * imp[..., o:o + SEL_RATIO * n_sel:SEL_RATIO]
        cur = (t // SEL_BLOCK)[:, None]
        valid_b = blk_j[None, :] * SEL_BLOCK <= t[:, None]
        forced = (blk_j[None, :] == 0) | (blk_j[None, :] == cur) | (blk_j[None, :] == cur - 1)
        score = jnp.where(valid_b, jnp.where(forced, FORCE_SCORE, p_slc), NEG_INF)
        _, idx = lax.top_k(score, k_top)
        kg = kb[b_idx, g_idx, idx].reshape(B, G, Q_BLOCK, k_top * SEL_BLOCK, dk)
        vg = vb[b_idx, g_idx, idx].reshape(B, G, Q_BLOCK, k_top * SEL_BLOCK, dk)
        tok = idx[..., None] * SEL_BLOCK + jnp.arange(SEL_BLOCK)
        mask_s = (tok <= t[None, None, :, None, None]).reshape(B, G, Q_BLOCK, k_top * SEL_BLOCK)
        ss = jnp.einsum('bqghd,bgqkd->bghqk', qs, kg).astype(jnp.float32) * SCALE
        ps = jax.nn.softmax(jnp.where(mask_s[:, :, None], ss, NEG_INF), axis=-1)
        o_slc = jnp.einsum('bghqk,bgqkd->bqghd', ps.astype(vg.dtype), vg)
        kwb = lax.dynamic_slice_in_dim(kw, s0, WINDOW + Q_BLOCK, axis=1)
        vwb = lax.dynamic_slice_in_dim(vw, s0, WINDOW + Q_BLOCK, axis=1)
        kpos = s0 - WINDOW + jnp.arange(WINDOW + Q_BLOCK)
        diff = t[:, None] - kpos[None, :]
        mask_w = (diff >= 0) & (diff < WINDOW) & (kpos[None, :] >= 0)
        sw = jnp.einsum('bqghd,bkgd->bghqk', qs, kwb).astype(jnp.float32) * SCALE
        pw = jax.nn.softmax(jnp.where(mask_w, sw, NEG_INF), axis=-1)
        o_win = jnp.einsum('bghqk,bkgd->bqghd', pw.astype(vwb.dtype), vwb)
        o = (gb[..., 0:1] * o_cmp.reshape(B, Q_BLOCK, H, dk).astype(jnp.float32)
             + gb[..., 1:2] * o_slc.reshape(B, Q_BLOCK, H, dk).astype(jnp.float32)
             + gb[..., 2:3] * o_win.reshape(B, Q_BLOCK, H, dk).astype(jnp.float32))
        return o.astype(q.dtype).reshape(B, Q_BLOCK, H * dk)

    out = lax.map(one_block, jnp.arange(S // Q_BLOCK))
    return out.transpose(1, 0, 2, 3).reshape(B, S, D_ATTN)


def short_conv_group(bg, cg, xc, w_conv):
    u = cg * xc
    y = lax.conv_general_dilated(u, w_conv[:, None, :].astype(u.dtype), window_strides=(1,),
                                 padding=[(CONV_WIDTH - 1, 0)],
                                 dimension_numbers=('NWC', 'WIO', 'NWC'),
                                 feature_group_count=D_CONV)
    return bg * y


def hybrid_mixer(h, positions, w_in, pe_k, w1_k, w2_k, pe_v, w1_v, w2_v,
                 w_conv, attn_out_norm, conv_out_norm, w_out):
    z = h @ w_in
    sizes = [D_ATTN] + [D_KV] * 6 + [N_GATES] + [D_CONV] * 3
    cuts = [int(c) for c in np.cumsum(sizes)[:-1]]
    q, kc, vc, ks, vs, kw, vw, gates, bg, cg, xc = jnp.split(z, cuts, axis=-1)
    attn = nsa_group(q, kc, vc, ks, vs, kw, vw, gates, positions, pe_k, w1_k, w2_k, pe_v, w1_v, w2_v)
    conv = short_conv_group(bg, cg, xc, w_conv)
    merged = jnp.concatenate([rms_norm(attn, attn_out_norm), rms_norm(conv, conv_out_norm)], axis=-1)
    return merged @ w_out


def setup_inputs(seed: int = 0) -> dict:
    key = jax.random.key(seed)
    ks = jax.random.split(key, 26)
    L = DEPTH

    def nrm(k, shape, scale):
        return jax.random.normal(k, shape, jnp.float32) * scale

    def gain(k, n):
        return 1.0 + 0.05 * jax.random.normal(k, (L, n), jnp.float32)

    x = nrm(ks[0], (BATCH, SEQ, D_MODEL), 1.0)
    positions = (jnp.arange(SEQ, dtype=jnp.int32)[None, :]
                 + jax.random.randint(ks[1], (BATCH, 1), 0, MAX_POS_OFFSET, dtype=jnp.int32))
    return {
        "x": x,
        "positions": positions,
        "ffn1_norm_pre": gain(ks[2], D_MODEL),
        "ffn1_w_gate": nrm(ks[3], (L, D_MODEL, D_FF), D_MODEL ** -0.5),
        "ffn1_w_up": nrm(ks[4], (L, D_MODEL, D_FF), D_MODEL ** -0.5),
        "ffn1_w_down": nrm(ks[5], (L, D_FF, D_MODEL), D_FF ** -0.5),
        "ffn1_norm_post": gain(ks[6], D_MODEL),
        "mix_norm_pre": gain(ks[7], D_MODEL),
        "w_in": nrm(ks[8], (L, D_MODEL, D_IN_PROJ), D_MODEL ** -0.5),
        "cmp_pe_k": nrm(ks[9], (L, CMP_BLOCK, HEAD_DIM), 0.02),
        "cmp_w1_k": nrm(ks[10], (L, CMP_BLOCK * HEAD_DIM, CMP_HIDDEN), (CMP_BLOCK * HEAD_DIM) ** -0.5),
        "cmp_w2_k": nrm(ks[11], (L, CMP_HIDDEN, HEAD_DIM), CMP_HIDDEN ** -0.5),
        "cmp_pe_v": nrm(ks[12], (L, CMP_BLOCK, HEAD_DIM), 0.02),
        "cmp_w1_v": nrm(ks[13], (L, CMP_BLOCK * HEAD_DIM, CMP_HIDDEN), (CMP_BLOCK * HEAD_DIM) ** -0.5),
        "cmp_w2_v": nrm(ks[14], (L, CMP_HIDDEN, HEAD_DIM), CMP_HIDDEN ** -0.5),
        "conv_w": nrm(ks[15], (L, CONV_WIDTH, D_CONV), CONV_WIDTH ** -0.5),
        "attn_out_norm": gain(ks[16], D_ATTN),
        "conv_out_norm": gain(ks[17], D_CONV),
        "w_out": nrm(ks[18], (L, D_MODEL, D_MODEL), D_MODEL ** -0.5),
        "mix_norm_post": gain(ks[19], D_MODEL),
        "ffn2_norm_pre": gain(ks[20], D_MODEL),
        "ffn2_w_gate": nrm(ks[21], (L, D_MODEL, D_FF), D_MODEL ** -0.5),
        "ffn2_w_up": nrm(ks[22], (L, D_MODEL, D_FF), D_MODEL ** -0.5),
        "ffn2_w_down": nrm(ks[23], (L, D_FF, D_MODEL), D_FF ** -0.5),
        "ffn2_norm_post": gain(ks[24], D_MODEL),
    }


def reference(x, positions, ffn1_norm_pre, ffn1_w_gate, ffn1_w_up, ffn1_w_down, ffn1_norm_post,
              mix_norm_pre, w_in, cmp_pe_k, cmp_w1_k, cmp_w2_k, cmp_pe_v, cmp_w1_v, cmp_w2_v,
              conv_w, attn_out_norm, conv_out_norm, w_out, mix_norm_post,
              ffn2_norm_pre, ffn2_w_gate, ffn2_w_up, ffn2_w_down, ffn2_norm_post):
    for l in range(DEPTH):
        h = swiglu(rms_norm(x, ffn1_norm_pre[l]), ffn1_w_gate[l], ffn1_w_up[l], ffn1_w_down[l])
        x = x + 0.5 * rms_norm(h, ffn1_norm_post[l])
        h = hybrid_mixer(rms_norm(x, mix_norm_pre[l]), positions, w_in[l],
                         cmp_pe_k[l], cmp_w1_k[l], cmp_w2_k[l], cmp_pe_v[l], cmp_w1_v[l], cmp_w2_v[l],
                         conv_w[l], attn_out_norm[l], conv_out_norm[l], w_out[l])
        x = x + rms_norm(h, mix_norm_post[l])
        h = swiglu(rms_norm(x, ffn2_norm_pre[l]), ffn2_w_gate[l], ffn2_w_up[l], ffn2_w_down[l])
        x = x + 0.5 * rms_norm(h, ffn2_norm_post[l])
    return x
```

```python
import math
import numpy as np
import concourse.bass as bass
import concourse.mybir as mybir
from concourse.bass_utils import run_bass_kernel_spmd

F32 = mybir.dt.float32
BF16 = mybir.dt.bfloat16
I32 = mybir.dt.int32
AF = mybir.ActivationFunctionType
ALU = mybir.AluOpType
AX = mybir.AxisListType

NCORES = 8
D = 1024
DFF = 2816
NFC = DFF // 128
SEQ = 16384
NTOK = 4096
TG = 1024
EPS = 1e-6


class Buf:
    __slots__ = ("w", "r", "excl")

    def __init__(self, excl=False):
        self.w = None
        self.r = {}
        self.excl = excl


class Tile:
    def __init__(self, handle, excl=False):
        self.h = handle
        self.bufs = {}
        self.excl = excl

    def b(self, key=None):
        bb = self.bufs.get(key)
        if bb is None:
            bb = self.bufs[key] = Buf(self.excl)
        return bb

    def __getitem__(self, k):
        return self.h[k]


EPOCH = 24000


class Prog:
    STREAMS = ("pe", "act", "dve", "pool", "sp")

    def __init__(self, nc):
        self.nc = nc
        self.streams = {s: [] for s in self.STREAMS}
        self.count = {}
        self.unit = {s: 1 for s in self.STREAMS}
        self.seen = {s: {} for s in self.STREAMS}
        self.sems = {}
        self.nt = 0

    def sb(self, shape, dt, name=None):
        self.nt += 1
        return Tile(self.nc.alloc_sbuf_tensor(name or f"t{self.nt}", list(shape), dt))

    def ps(self, shape, dt=F32, name=None):
        self.nt += 1
        return Tile(self.nc.alloc_psum_tensor(name or f"p{self.nt}", list(shape), dt), excl=True)

    def dram(self, name, shape, dt, kind="Internal"):
        return Tile(self.nc.dram_tensor(name, list(shape), dt, kind=kind).ap())

    def dma_chan(self, name):
        self.unit[name] = 16
        return name

    def op(self, stream, fn, reads=(), writes=(), chan=None):
        chan = chan or stream
        deps = {}
        for b in reads:
            if b.w is not None:
                c, n = b.w
                if deps.get(c, 0) < n:
                    deps[c] = n
            if b.excl:
                for c, n in b.r.items():
                    if c != chan and deps.get(c, 0) < n:
                        deps[c] = n
        for b in writes:
            if b.w is not None:
                c, n = b.w
                if deps.get(c, 0) < n:
                    deps[c] = n
            for c, n in b.r.items():
                if deps.get(c, 0) < n:
                    deps[c] = n
        waits = []
        seen = self.seen[stream]
        for c, n in deps.items():
            if c == "pe" and stream == "pe" and chan == "pe":
                continue
            if seen.get(c, 0) >= n:
                continue
            seen[c] = n
            waits.append((c, n))
        idx = self.count.get(chan, 0) + 1
        self.count[chan] = idx
        self.streams[stream].append((fn, waits, chan, idx))
        for b in reads:
            if b.r.get(chan, 0) < idx:
                b.r[chan] = idx
        for b in writes:
            b.w = (chan, idx)
            b.r = {}
        return idx

    def _semval(self, chan, idx):
        unit = self.unit[chan]
        per = EPOCH // unit
        ep = (idx - 1) // per
        key = (chan, ep)
        sem = self.sems.get(key)
        if sem is None:
            sem = self.sems[key] = self.nc.alloc_semaphore(f"s_{chan}_{ep}")
        return sem, ((idx - 1) % per + 1) * unit

    def _replay(self, stream, eng):
        for fn, waits, chan, idx in self.streams[stream]:
            for c, n in waits:
                sem, val = self._semval(c, n)
                eng.wait_ge(sem, val)
            ins = fn(eng)
            sem, _ = self._semval(chan, idx)
            ins.then_inc(sem, self.unit[chan])

    def finish(self):
        print("prog sizes", {k: len(v) for k, v in self.streams.items()}, flush=True)
        st = self.streams["sp"]
        final_waits = [(c, n) for c, n in self.count.items()]
        nc = self.nc
        with nc.Block() as block:
            @block.tensor
            def _(e):
                self._replay("pe", e)

            @block.scalar
            def _(e):
                self._replay("act", e)

            @block.vector
            def _(e):
                self._replay("dve", e)

            @block.gpsimd
            def _(e):
                self._replay("pool", e)

            @block.sync
            def _(e):
                self._replay("sp", e)
                for c, n in final_waits:
                    sem, val = self._semval(c, n)
                    e.wait_ge(sem, val)


def L(name, *args, **kw):
    return lambda e: getattr(e, name)(*args, **kw)

def rms_scale(P, ss, rstd, n, epsb, extra=1.0, cols=1):
    e2 = float(extra) ** 2
    P.op("act", L("activation", out=rstd[:, 0:cols], in_=ss[:, 0:cols], func=AF.Sqrt,
                  bias=epsb[:, 0:1], scale=1.0 / (n * e2)),
         reads=[ss.b(), epsb.b()], writes=[rstd.b()])
    P.op("dve", L("reciprocal", rstd[:, 0:cols], rstd[:, 0:cols]), reads=[rstd.b()], writes=[rstd.b()])


def make_consts(P, id_d, ld):
    C = {}
    C["idf"] = P.sb([128, 128], F32)
    C["idb"] = P.sb([128, 128], BF16)
    P.op("sp", L("dma_start", out=C["idf"][:, :], in_=id_d[:, :]), writes=[C["idf"].b()], chan=ld)
    P.op("dve", L("tensor_copy", C["idb"][:, :], C["idf"][:, :]), reads=[C["idf"].b()], writes=[C["idb"].b()])
    for nm, v in (("eps1", EPS), ("eps4", 4.0 * EPS)):
        C[nm] = P.sb([128, 1], F32)
        P.op("dve", L("memset", C[nm][:, :], v), writes=[C[nm].b()])
    return C


def prenorm_T(P, C, xsrc, xb, g_bc, hT, hT_b, col0, st):
    P.op("act", L("activation", out=st["sq"][:, :], in_=xsrc, func=AF.Square, accum_out=st["ss"][:, 0:1]),
         reads=[xb], writes=[st["sq"].b(), st["ss"].b()])
    rms_scale(P, st["ss"], st["rstd"], D, C["eps1"])
    P.op("dve", L("scalar_tensor_tensor", out=st["hb"][:, :], in0=xsrc, scalar=st["rstd"][:, 0:1], in1=g_bc[:, :],
                  op0=ALU.mult, op1=ALU.mult),
         reads=[xb, st["rstd"].b(), g_bc.b()], writes=[st["hb"].b()])
    for k in range(8):
        P.op("pe", L("transpose", st["tp"][:, k * 128:(k + 1) * 128], st["hb"][:, k * 128:(k + 1) * 128], C["idb"][:, :]),
             reads=[st["hb"].b(), C["idb"].b()], writes=[st["tp"].b()])
    P.op("act", L("copy", out=hT[:, :, col0:col0 + 128], in_=st["tp"][:, :].rearrange("p (k t) -> p k t", k=8)),
         reads=[st["tp"].b()], writes=[hT_b])


def prenorm_scratch(P):
    return {"sq": P.sb([128, D], F32), "ss": P.sb([128, 1], F32), "rstd": P.sb([128, 1], F32),
            "hb": P.sb([128, D], BF16), "tp": P.ps([128, D], BF16)}


def bcast_load(P, dst, src_d, n, ld):
    P.op("sp", L("dma_start", out=dst[:, :], in_=src_d[0:1, 0:n].partition_broadcast(128)), writes=[dst.b()], chan=ld)


def build_ffn(ntok=NTOK, tg=TG):
    nc = bass.Bass("TRN2", target_bir_lowering=False)
    P = Prog(nc)
    x = P.dram("x", [ntok, D], F32, kind="ExternalInput")
    gpre_d = P.dram("g_pre", [1, D], F32, kind="ExternalInput")
    gpost_d = P.dram("g_post", [1, D], F32, kind="ExternalInput")
    wg_d = P.dram("w_gate", [D, DFF], F32, kind="ExternalInput")
    wu_d = P.dram("w_up", [D, DFF], F32, kind="ExternalInput")
    wd_d = P.dram("w_down", [DFF, D], F32, kind="ExternalInput")
    id_d = P.dram("ident", [128, 128], F32, kind="ExternalInput")
    y = P.dram("y", [ntok, D], F32, kind="ExternalOutput")
    ld = P.dma_chan("ld")
    wl = P.dma_chan("wl")
    stc = P.dma_chan("st")
    ntt = tg // 128
    ntb = tg // 512
    C = make_consts(P, id_d, ld)
    gpre = P.sb([128, D], F32)
    gpost = P.sb([128, D], F32)
    bcast_load(P, gpre, gpre_d, D, ld)
    bcast_load(P, gpost, gpost_d, D, ld)
    xg = P.sb([128, ntt, D], F32)
    st = prenorm_scratch(P)
    hT = P.sb([128, 8, tg], BF16)
    wg = [P.sb([128, 8, 256], BF16) for _ in range(2)]
    wu = [P.sb([128, 8, 256], BF16) for _ in range(2)]
    wd = P.sb([128, NFC, D], BF16)
    aT = P.sb([128, NFC, tg], BF16)
    sg = [P.sb([128, 512], F32) for _ in range(2)]
    ss2 = [P.sb([128, 2], F32) for _ in range(2)]
    ss2s = [P.sb([128, 1], F32) for _ in range(2)]
    rstd2 = [P.sb([128, 1], F32) for _ in range(2)]
    yt = [P.sb([128, D], F32) for _ in range(2)]
    pg = [P.ps([128, 512], F32) for _ in range(2)]
    pu = [P.ps([128, 512], F32) for _ in range(2)]
    py = [P.ps([128, 512], F32) for _ in range(2)]
    for fc in range(NFC):
        P.op("pool", L("dma_start", out=wd[:, fc, :], in_=wd_d[fc * 128:(fc + 1) * 128, :]), writes=[wd.b(fc)], chan=wl)
    for g in range(ntok // tg):
        t0 = g * tg
        for tt in range(ntt):
            r0 = t0 + tt * 128
            P.op("sp", L("dma_start", out=xg[:, tt, :], in_=x[r0:r0 + 128, :]), writes=[xg.b(tt)], chan=ld)
            prenorm_T(P, C, xg[:, tt, :], xg.b(tt), gpre, hT, hT.b(tt), tt * 128, st)
        for fg in range(NFC // 2):
            s = fg % 2
            f0 = fg * 256
            P.op("pool", L("dma_start", out=wg[s][:, :, :], in_=wg_d[:, f0:f0 + 256].rearrange("(k p) f -> p k f", p=128)),
                 writes=[wg[s].b()], chan=wl)
            P.op("pool", L("dma_start", out=wu[s][:, :, :], in_=wu_d[:, f0:f0 + 256].rearrange("(k p) f -> p k f", p=128)),
                 writes=[wu[s].b()], chan=wl)
            for c2 in range(2):
                fc = fg * 2 + c2
                for tb in range(ntb):
                    ps_ = (c2 * ntb + tb) % 2
                    hreads = [hT.b(tt) for tt in range(tb * 4, tb * 4 + 4)]
                    for k in range(8):
                        P.op("pe", L("matmul", pg[ps_][:, :], lhsT=wg[s][:, k, c2 * 128:(c2 + 1) * 128],
                                     rhs=hT[:, k, tb * 512:(tb + 1) * 512], start=(k == 0), stop=(k == 7)),
                             reads=[wg[s].b()] + hreads, writes=[pg[ps_].b()])
                    for k in range(8):
                        P.op("pe", L("matmul", pu[ps_][:, :], lhsT=wu[s][:, k, c2 * 128:(c2 + 1) * 128],
                                     rhs=hT[:, k, tb * 512:(tb + 1) * 512], start=(k == 0), stop=(k == 7)),
                             reads=[wu[s].b()] + hreads, writes=[pu[ps_].b()])
                    P.op("act", L("activation", out=sg[ps_][:, :], in_=pg[ps_][:, :], func=AF.Silu),
                         reads=[pg[ps_].b()], writes=[sg[ps_].b()])
                    P.op("dve", L("tensor_tensor", out=aT[:, fc, tb * 512:(tb + 1) * 512], in0=pu[ps_][:, :],
                                  in1=sg[ps_][:, :], op=ALU.mult),
                         reads=[sg[ps_].b(), pu[ps_].b()], writes=[aT.b((fc, tb))])
        for tt in range(ntt):
            s = tt % 2
            tb = tt // 4
            for nh in range(2):
                for fc in range(NFC):
                    P.op("pe", L("matmul", py[nh][:, :], lhsT=aT[:, fc, tt * 128:(tt + 1) * 128],
                                 rhs=wd[:, fc, nh * 512:(nh + 1) * 512], start=(fc == 0), stop=(fc == NFC - 1)),
                         reads=[aT.b((fc, tb)), wd.b(fc)], writes=[py[nh].b()])
                P.op("act", L("activation", out=st["sq"][:, nh * 512:(nh + 1) * 512], in_=py[nh][:, :],
                              func=AF.Square, accum_out=ss2[s][:, nh:nh + 1]),
                     reads=[py[nh].b()], writes=[st["sq"].b(), ss2[s].b(nh)])
            P.op("dve", L("tensor_tensor", out=ss2s[s][:, 0:1], in0=ss2[s][:, 0:1], in1=ss2[s][:, 1:2], op=ALU.add),
                 reads=[ss2[s].b(0), ss2[s].b(1)], writes=[ss2s[s].b()])
            rms_scale(P, ss2s[s], rstd2[s], D, C["eps4"], extra=0.5)
            for nh in range(2):
                P.op("dve", L("scalar_tensor_tensor", out=yt[s][:, nh * 512:(nh + 1) * 512], in0=py[nh][:, :],
                              scalar=rstd2[s][:, 0:1], in1=gpost[:, nh * 512:(nh + 1) * 512], op0=ALU.mult, op1=ALU.mult),
                     reads=[py[nh].b(), rstd2[s].b(), gpost.b()], writes=[yt[s].b(nh)])
            P.op("dve", L("tensor_tensor", out=yt[s][:, :], in0=yt[s][:, :], in1=xg[:, tt, :], op=ALU.add),
                 reads=[yt[s].b(0), yt[s].b(1), xg.b(tt)], writes=[yt[s].b(0), yt[s].b(1)])
            r0 = t0 + tt * 128
            P.op("sp", L("dma_start", out=y[r0:r0 + 128, :], in_=yt[s][:, :]),
                 reads=[yt[s].b(0), yt[s].b(1)], writes=[y.b(r0)], chan=stc)
    P.finish()
    return nc

DIN = 2840
C_Q, C_KC, C_VC, C_KS, C_VS, C_KW, C_VW, C_GT, C_BG, C_CG, C_XC = 0, 512, 640, 768, 896, 1024, 1152, 1280, 1304, 1816, 2328
TWO_PI = 2.0 * math.pi
CW1 = 6.28125
CW2 = TWO_PI - CW1


def build_inproj(ntok=NTOK, stage=99):
    nc = bass.Bass("TRN2", target_bir_lowering=False)
    P = Prog(nc)
    x = P.dram("x", [ntok, D], F32, kind="ExternalInput")
    g_d = P.dram("g_pre", [1, D], F32, kind="ExternalInput")
    win_d = P.dram("w_in", [D, DIN], F32, kind="ExternalInput")
    pos_d = P.dram("pos", [1, ntok], I32, kind="ExternalInput")
    rc_d = P.dram("ropec", [16, 4], F32, kind="ExternalInput")
    id_d = P.dram("ident", [128, 128], F32, kind="ExternalInput")
    qT_d = P.dram("qT", [8, 64, ntok], BF16, kind="ExternalOutput")
    qrT_d = P.dram("qrT", [8, 64, ntok], BF16, kind="ExternalOutput")
    ksT_d = P.dram("ksT", [2, 64, ntok], BF16, kind="ExternalOutput")
    kwT_d = P.dram("kwT", [2, 64, ntok], BF16, kind="ExternalOutput")
    kcvc_d = P.dram("kcvcT", [2, 128, ntok], BF16, kind="ExternalOutput")
    vsw_d = P.dram("vsw", [ntok, 256], BF16, kind="ExternalOutput")
    gates_d = P.dram("gates", [ntok, 24], F32, kind="ExternalOutput")
    bgT_d = P.dram("bgT", [4, 128, ntok], BF16, kind="ExternalOutput")
    uT_d = P.dram("uT", [4, 128, ntok], BF16, kind="ExternalOutput")
    ld = P.dma_chan("ld")
    wl = P.dma_chan("wl")
    stc = P.dma_chan("st")
    C = make_consts(P, id_d, ld)
    gpre = P.sb([128, D], F32)
    bcast_load(P, gpre, g_d, D, ld)
    st = prenorm_scratch(P)
    w = P.sb([128, 8, DIN], BF16)
    for k in range(8):
        for (c0, c1) in ((0, 1304), (1304, DIN)):
            P.op("pool", L("dma_start", out=w[:, k, c0:c1], in_=win_d[k * 128:(k + 1) * 128, c0:c1]),
                 writes=[w.b()], chan=wl)
    wP = P.sb([128, 8, 12, 32], BF16)
    P.op("dve", L("memset", wP[:, :, :, :], 0.0), writes=[wP.b()])
    for (u0, nu, c0) in ((0, 8, C_Q), (8, 2, C_KS), (10, 2, C_KW)):
        src = w[:, :, c0:c0 + nu * 64].rearrange("p k (u d) -> p k u d", d=64)
        P.op("dve", L("tensor_scalar", wP[:, :, u0:u0 + nu, 0:8], src[:, :, :, 8:16], -1.0, None, ALU.mult),
             reads=[w.b()], writes=[wP.b()])
        P.op("dve", L("tensor_copy", wP[:, :, u0:u0 + nu, 8:16], src[:, :, :, 0:8]), reads=[w.b()], writes=[wP.b()])
    wt = P.sb([128, 8, 280], BF16)
    for (d0, c0, n) in ((0, C_VS, 128), (128, C_VW, 128), (256, C_GT, 24)):
        P.op("dve", L("tensor_copy", wt[:, :, d0:d0 + n], w[:, :, c0:c0 + n]), reads=[w.b()], writes=[wt.b()])
    rc = P.sb([16, 4], F32)
    P.op("sp", L("dma_start", out=rc[:, :], in_=rc_d[:, :]), writes=[rc.b()], chan=ld)
    ctab = P.sb([32, 512], F32)
    stab = P.sb([32, 512], F32)
    P.op("dve", L("memset", ctab[:, :], 1.0), writes=[ctab.b()])
    P.op("dve", L("memset", stab[:, :], 0.0), writes=[stab.b()])
    posi = P.sb([16, 512], I32)
    ang = P.sb([16, 512], F32)
    rr = P.sb([16, 512], F32)
    sn = P.sb([16, 512], F32)
    xt = [P.sb([128, D], F32) for _ in range(2)]
    hT = P.sb([128, 8, 512], BF16)
    pq = [P.ps([64, 512], F32) for _ in range(2)]
    ppq = [P.ps([32, 512], F32) for _ in range(2)]
    pm = [P.ps([128, 512], F32) for _ in range(2)]
    ptk = P.ps([128, 280], F32)
    t1 = [P.sb([32, 512], F32) for _ in range(2)]
    t2 = [P.sb([32, 512], F32) for _ in range(2)]
    ob = [P.sb([64, 512], BF16) for _ in range(2)]
    obr = [P.sb([64, 512], BF16) for _ in range(2)]
    om = [P.sb([128, 512], BF16) for _ in range(2)]
    cgs = [P.sb([128, 512], F32) for _ in range(2)]
    otk = [P.sb([128, 256], BF16) for _ in range(2)]
    ogt = [P.sb([128, 24], F32) for _ in range(2)]
    units = [(h, C_Q + 64 * h, qT_d, h) for h in range(8)] + \
            [(8 + g, C_KS + 64 * g, ksT_d, g) for g in range(2)] + \
            [(10 + g, C_KW + 64 * g, kwT_d, g) for g in range(2)]
    nm = 0
    nu_ = 0
    for blk in range(ntok // 512):
        b0 = blk * 512
        P.op("sp", L("dma_start", out=posi[:, :], in_=pos_d[0:1, b0:b0 + 512].partition_broadcast(16)),
             writes=[posi.b()], chan=ld)
        P.op("dve", L("tensor_copy", ang[:, :], posi[:, :]), reads=[posi.b()], writes=[ang.b()])
        P.op("dve", L("tensor_scalar", ang[:, :], ang[:, :], rc[:, 0:1], None, ALU.mult),
             reads=[ang.b(), rc.b()], writes=[ang.b()])
        for (tab, shift) in ((stab, 0.0), (ctab, 0.5 * math.pi)):
            P.op("dve", L("tensor_scalar", rr[:, :], ang[:, :], 1.0 / TWO_PI, shift / TWO_PI, ALU.mult, ALU.add),
                 reads=[ang.b()], writes=[rr.b()])
            P.op("dve", L("tensor_copy", posi[:, :], rr[:, :]), reads=[rr.b()], writes=[posi.b()])
            P.op("dve", L("tensor_copy", rr[:, :], posi[:, :]), reads=[posi.b()], writes=[rr.b()])
            P.op("dve", L("scalar_tensor_tensor", out=sn[:, :], in0=rr[:, :], scalar=-CW1, in1=ang[:, :],
                          op0=ALU.mult, op1=ALU.add), reads=[rr.b(), ang.b()], writes=[sn.b()])
            P.op("dve", L("scalar_tensor_tensor", out=sn[:, :], in0=rr[:, :], scalar=-CW2, in1=sn[:, :],
                          op0=ALU.mult, op1=ALU.add), reads=[rr.b(), sn.b()], writes=[sn.b()])
            if shift:
                P.op("dve", L("tensor_scalar", sn[:, :], sn[:, :], shift, None, ALU.add), reads=[sn.b()], writes=[sn.b()])
            P.op("dve", L("tensor_scalar", rr[:, :], sn[:, :], math.pi, -TWO_PI, ALU.is_gt, ALU.mult),
                 reads=[sn.b()], writes=[rr.b()])
            P.op("dve", L("tensor_tensor", out=sn[:, :], in0=sn[:, :], in1=rr[:, :], op=ALU.add),
                 reads=[sn.b(), rr.b()], writes=[sn.b()])
            P.op("dve", L("tensor_scalar", sn[:, :], sn[:, :], math.pi, -math.pi, ALU.min, ALU.max),
                 reads=[sn.b()], writes=[sn.b()])
            P.op("act", L("activation", out=tab[0:16, :], in_=sn[:, :], func=AF.Sin), reads=[sn.b()], writes=[tab.b()])
        for tt in range(4):
            r0 = b0 + tt * 128
            s = tt % 2
            P.op("sp", L("dma_start", out=xt[s][:, :], in_=x[r0:r0 + 128, :]), writes=[xt[s].b()], chan=ld)
            prenorm_T(P, C, xt[s][:, :], xt[s].b(), gpre, hT, hT.b(tt), tt * 128, st)
        hreads = [hT.b(tt) for tt in range(4)]
        if stage < 1:
            continue
        for (u, c0, dst, di) in units:
            s = nu_ % 2
            nu_ += 1
            for k in range(8):
                P.op("pe", L("matmul", pq[s][:, :], lhsT=w[:, k, c0:c0 + 64], rhs=hT[:, k, :], start=(k == 0), stop=(k == 7)),
                     reads=[w.b()] + hreads, writes=[pq[s].b()])
            for k in range(8):
                P.op("pe", L("matmul", ppq[s][:, :], lhsT=wP[:, k, u, :], rhs=hT[:, k, :], start=(k == 0), stop=(k == 7)),
                     reads=[wP.b()] + hreads, writes=[ppq[s].b()])
            P.op("dve", L("tensor_tensor", out=t1[s][:, :], in0=pq[s][0:32, :], in1=ctab[:, :], op=ALU.mult),
                 reads=[pq[s].b(), ctab.b()], writes=[t1[s].b()])
            P.op("dve", L("tensor_tensor", out=t2[s][:, :], in0=ppq[s][:, :], in1=stab[:, :], op=ALU.mult),
                 reads=[ppq[s].b(), stab.b()], writes=[t2[s].b()])
            P.op("dve", L("tensor_tensor", out=ob[s][0:32, :], in0=t1[s][:, :], in1=t2[s][:, :], op=ALU.add),
                 reads=[t1[s].b(), t2[s].b()], writes=[ob[s].b(0)])
            P.op("act", L("copy", out=ob[s][32:64, :], in_=pq[s][32:64, :]), reads=[pq[s].b()], writes=[ob[s].b(1)])
            if u < 8:
                P.op("act", L("copy", out=obr[s][:, :], in_=pq[s][:, :]), reads=[pq[s].b()], writes=[obr[s].b()])
                P.op("sp", L("dma_start", out=qrT_d[di, :, b0:b0 + 512], in_=obr[s][:, :]),
                     reads=[obr[s].b()], writes=[qrT_d.b((di, blk))], chan=stc)
            P.op("sp", L("dma_start", out=dst[di, :, b0:b0 + 512], in_=ob[s][:, :]),
                 reads=[ob[s].b(0), ob[s].b(1)], writes=[dst.b((di, blk))], chan=stc)
        if stage < 2:
            continue
        for (c0, dst, di) in ((C_KC, kcvc_d, 0), (C_VC, kcvc_d, 1)) + tuple((C_BG + 128 * c, bgT_d, c) for c in range(4)):
            s = nm % 2
            nm += 1
            for k in range(8):
                P.op("pe", L("matmul", pm[s][:, :], lhsT=w[:, k, c0:c0 + 128], rhs=hT[:, k, :], start=(k == 0), stop=(k == 7)),
                     reads=[w.b()] + hreads, writes=[pm[s].b()])
            P.op("act", L("copy", out=om[s][:, :], in_=pm[s][:, :]), reads=[pm[s].b()], writes=[om[s].b()])
            P.op("sp", L("dma_start", out=dst[di, :, b0:b0 + 512], in_=om[s][:, :]),
                 reads=[om[s].b()], writes=[dst.b((di, blk))], chan=stc)
        for c in range(4):
            s0 = nm % 2
            s1 = (nm + 1) % 2
            nm += 2
            for (s, c0) in ((s0, C_CG + 128 * c), (s1, C_XC + 128 * c)):
                for k in range(8):
                    P.op("pe", L("matmul", pm[s][:, :], lhsT=w[:, k, c0:c0 + 128], rhs=hT[:, k, :], start=(k == 0), stop=(k == 7)),
                         reads=[w.b()] + hreads, writes=[pm[s].b()])
            P.op("act", L("copy", out=cgs[c % 2][:, :], in_=pm[s0][:, :]), reads=[pm[s0].b()], writes=[cgs[c % 2].b()])
            P.op("dve", L("tensor_tensor", out=om[s0][:, :], in0=pm[s1][:, :], in1=cgs[c % 2][:, :], op=ALU.mult),
                 reads=[cgs[c % 2].b(), pm[s1].b()], writes=[om[s0].b()])
            P.op("sp", L("dma_start", out=uT_d[c, :, b0:b0 + 512], in_=om[s0][:, :]),
                 reads=[om[s0].b()], writes=[uT_d.b((c, blk))], chan=stc)
        if stage < 3:
            continue
        for tt in range(4):
            s = tt % 2
            r0 = b0 + tt * 128
            for k in range(8):
                P.op("pe", L("matmul", ptk[:, :], lhsT=hT[:, k, tt * 128:(tt + 1) * 128], rhs=wt[:, k, :],
                             start=(k == 0), stop=(k == 7)),
                     reads=[wt.b(), hT.b(tt)], writes=[ptk.b()])
            P.op("act", L("copy", out=otk[s][:, :], in_=ptk[:, 0:256]), reads=[ptk.b()], writes=[otk[s].b()])
            P.op("act", L("copy", out=ogt[s][:, :], in_=ptk[:, 256:280]), reads=[ptk.b()], writes=[ogt[s].b()])
            P.op("sp", L("dma_start", out=vsw_d[r0:r0 + 128, :], in_=otk[s][:, :]),
                 reads=[otk[s].b()], writes=[vsw_d.b(r0)], chan=stc)
            P.op("sp", L("dma_start", out=gates_d[r0:r0 + 128, :], in_=ogt[s][:, :]),
                 reads=[ogt[s].b()], writes=[gates_d.b(r0)], chan=stc)
    P.finish()
    return nc


def rope_consts():
    half = 8
    invf = (np.float32(500000.0) ** (-np.arange(half, dtype=np.float32) * np.float32(2.0) / np.float32(16.0))).astype(np.float32)
    rc = np.zeros((16, 4), np.float32)
    rc[:, 0] = np.concatenate([invf, invf])
    rc[:, 1] = -math.pi
    return rc

SCALE = 0.125
LOOKAHEAD = 2
NEGB = -30000.0
GELU_C = 0.7978845608028654


def build_attn(jlist=None, ntok=NTOK, debug=False):
    if jlist is None:
        jlist = list(range(ntok // 128))
    nq = ntok // 128
    nc = bass.Bass("TRN2", target_bir_lowering=False)
    P = Prog(nc)
    EI = "ExternalInput"
    qT_d = P.dram("qT", [8, 64, ntok], BF16, kind=EI)
    qrT_d = P.dram("qrT", [8, 64, ntok], BF16, kind=EI)
    ksT_d = P.dram("ksT", [2, 64, SEQ], BF16, kind=EI)
    vs_d = P.dram("vs", [SEQ, 128], BF16, kind=EI)
    kwin_d = P.dram("kwin", [nq, 2, 64, 640], BF16, kind=EI)
    vwin_d = P.dram("vwin", [nq, 640, 128], BF16, kind=EI)
    kc2_d = P.dram("kc2", [2, 2, 128, 8192], BF16, kind=EI)
    gates_d = P.dram("gates", [ntok, 24], F32, kind=EI)
    bgT_d = P.dram("bgT", [4, 128, ntok], BF16, kind=EI)
    uT_d = P.dram("uT", [4, 128, ntok], BF16, kind=EI)
    uhalo_d = P.dram("uhalo", [nq, 4, 128, 2], BF16, kind=EI)
    x1_d = P.dram("x1", [ntok, D], F32, kind=EI)
    pe2_d = P.dram("pe2", [2, 128, 16], F32, kind=EI)
    w1_d = P.dram("w1", [2, 2048, 256], F32, kind=EI)
    w2_d = P.dram("w2", [2, 256, 64], F32, kind=EI)
    convw_d = P.dram("convw", [128, 12], F32, kind=EI)
    ga_d = P.dram("ga", [1, 512], F32, kind=EI)
    gc_d = P.dram("gc", [128, 4], F32, kind=EI)
    wout_d = P.dram("w_out", [D, D], F32, kind=EI)
    gpost_d = P.dram("g_post", [1, D], F32, kind=EI)
    id_d = P.dram("ident", [128, 128], F32, kind=EI)
    onehot_d = P.dram("onehot", [64, SEQ], BF16, kind=EI)
    amat_d = P.dram("amat", [1024, 256], BF16, kind=EI)
    masks_d = P.dram("masks", [128, 16, 128], BF16, kind=EI)
    fpat_d = P.dram("fpat", [128, 768], BF16, kind=EI)
    x2_d = P.dram("x2", [ntok, D], F32, kind="ExternalOutput")
    if debug:
        dbg_o = P.dram("dbg_o", [ntok, 3, 512], F32, kind="ExternalOutput")
        dbg_psl = P.dram("dbg_psl", [ntok, 2, 256], F32, kind="ExternalOutput")
        dbg_bias = P.dram("dbg_bias", [ntok, 2, 256], BF16, kind="ExternalOutput")
    ld = P.dma_chan("ld")
    wl = P.dma_chan("wl")
    stc = P.dma_chan("st")

    C = make_consts(P, id_d, ld)
    idb, idf = C["idb"], C["idf"]
    Kaug = P.sb([128, 2, SEQ], BF16)
    arena = P.sb([128, 16640], BF16)
    Vaug = arena[:, :].rearrange("p (t g e) -> p t g e", t=128, g=2, e=65)
    X2 = arena[:, 0:8192]
    w1b = arena[:, 8192:12288].rearrange("p (j c) -> p j c", j=16)
    hg = arena[:, 12288:13312].rearrange("p (c n) -> p c n", c=2)
    b_x2, b_w1, b_hg = arena.b("x2"), arena.b("w1"), arena.b("hg")
    wout = P.sb([128, 8, D], BF16)
    kcmpT = P.sb([64, 2, 1024], BF16)
    AV = P.sb([128, 8, 2, 321], BF16)
    masks = P.sb([128, 16, 128], BF16)
    fpat = P.sb([128, 768], BF16)
    gpost = P.sb([128, D], F32)
    ga = P.sb([128, 512], F32)
    gc = P.sb([128, 4], F32)
    cw = P.sb([128, 12], F32)
    ones_f = P.sb([128, 1], F32)
    QTs = [P.sb([64, 8, 128], BF16) for _ in range(2)]
    QRs = [P.sb([64, 8, 128], BF16) for _ in range(2)]
    Qaug = P.sb([128, 4, 2, 512], BF16)
    kwts = [P.sb([64, 2, 640], BF16) for _ in range(2)]
    vwas = [P.sb([128, 5, 2, 65], BF16) for _ in range(2)]
    gts = [P.sb([128, 24], F32) for _ in range(2)]
    sgt = P.sb([128, 24], F32)
    uts = [P.sb([128, 4, 130], BF16) for _ in range(2)]
    bgts = [P.sb([128, 4, 128], BF16) for _ in range(2)]
    cvy = P.sb([128, 128], F32)
    cvt = P.sb([128, 128], F32)
    cvq = P.sb([128, 4, 128], F32)
    x1t = P.sb([128, D], F32)
    EP = [P.sb([128, 512], BF16) for _ in range(4)]
    osb = [P.sb([65, 512], F32) for _ in range(2)]
    psl = P.sb([128, 256], F32)
    score = P.sb([128, 256], F32)
    swk = P.sb([128, 256], F32)
    m8 = P.sb([128, 16], F32)
    biaspad = P.sb([128, 320], BF16)
    oacc = P.sb([128, 512], F32)
    attb = P.sb([128, 512], BF16)
    mT = P.sb([128, 8, 128], BF16)
    hA = P.sb([128, D], F32)
    sq = P.sb([128, D], F32)
    sm = P.sb([128, 16], F32)
    b_sm = [sm.b(i) for i in range(16)]
    gx = P.sb([128, 512], F32)
    gtmp = P.sb([128, 512], F32)
    cb = P.sb([128, 2], F32)
    pe2f = P.sb([128, 16], F32)
    pe2b = P.sb([128, 16], BF16)
    w2b = P.sb([128, 2, 64], BF16)
    S = [P.ps([128, 512], F32) for _ in range(4)]
    G = [P.ps([128, 512], F32) for _ in range(3)]
    GB = P.ps([128, 1024], BF16)
    print("sbuf left", nc.sbuf_bytes_remaining, flush=True)

    def sp_load(dst_ap, src_ap, bufs):
        P.op("sp", L("dma_start", out=dst_ap, in_=src_ap), writes=bufs, chan=ld)

    sp_load(masks[:, :, :], masks_d[:, :, :], [masks.b()])
    sp_load(fpat[:, :], fpat_d[:, :], [fpat.b()])
    bcast_load(P, gpost, gpost_d, D, ld)
    bcast_load(P, ga, ga_d, 512, ld)
    sp_load(gc[:, :], gc_d[:, :], [gc.b()])
    sp_load(cw[:, :], convw_d[:, :], [cw.b()])
    P.op("dve", L("memset", ones_f[:, :], 1.0), writes=[ones_f.b()])
    for g in range(2):
        sp_load(AV[:, :, g, 0:256], amat_d[:, :].rearrange("(ct p) j -> p ct j", p=128), [AV.b(("a", g))])
    P.op("dve", L("memset", AV[:, :, :, 320:321], 1.0), writes=[AV.b("one")])
    for vwa in vwas:
        P.op("dve", L("memset", vwa[:, :, :, 64:65], 1.0), writes=[vwa.b("one")])
    P.op("dve", L("memset", kcmpT[:, :, :], 0.0), writes=[kcmpT.b(0), kcmpT.b(1)])
    P.op("dve", L("memset", hg, 0.0), writes=[b_hg])
    P.op("dve", L("memset", biaspad[:, 0:64], 0.0), writes=[biaspad.b("pad")])
    for k in range(8):
        P.op("pool", L("dma_start", out=wout[:, k, :], in_=wout_d[k * 128:(k + 1) * 128, :]), writes=[wout.b(k)], chan=wl)
    for g in range(2):
        sp_load(Kaug[0:64, g, :], ksT_d[g, :, :], [Kaug.b(("k", g))])
        sp_load(Kaug[64:128, g, :], onehot_d[:, :], [Kaug.b(("o", g))])

    for kv in range(2):
        P.op("pool", L("dma_start", out=w1b, in_=w1_d[kv, :, :].rearrange("(j p) c -> p j c", p=128)), writes=[b_w1], chan=wl)
        P.op("pool", L("dma_start", out=w2b[:, :, :], in_=w2_d[kv, :, :].rearrange("(c p) d -> p c d", p=128)),
             writes=[w2b.b()], chan=wl)
        sp_load(pe2f[:, :], pe2_d[kv, :, :], [pe2f.b()])
        P.op("dve", L("tensor_copy", pe2b[:, :], pe2f[:, :]), reads=[pe2f.b()], writes=[pe2b.b()])
        for c2 in range(2):
            for jj in range(16):
                P.op("pe", L("matmul", G[0][:, c2:c2 + 1], lhsT=w1b[:, jj, c2 * 128:(c2 + 1) * 128], rhs=pe2b[:, jj:jj + 1],
                             start=(jj == 0), stop=(jj == 15)), reads=[b_w1, pe2b.b()], writes=[G[0].b()])
        P.op("act", L("copy", out=cb[:, :], in_=G[0][:, 0:2]), reads=[G[0].b()], writes=[cb.b()])
        for g in range(2):
            sp_load(X2, kc2_d[kv, g, :, :], [b_x2])
            for nt in range(2):
                n0 = 512 * nt
                N = 512 if nt == 0 else 511
                for c2 in range(2):
                    Sx = S[c2]
                    for jj in range(16):
                        lo = 8 * n0 + jj
                        P.op("pe", L("matmul", Sx[:, 0:N], lhsT=w1b[:, jj, c2 * 128:(c2 + 1) * 128],
                                     rhs=X2[:, lo:lo + 8 * (N - 1) + 1:8], start=(jj == 0), stop=(jj == 15)),
                             reads=[b_w1, b_x2], writes=[Sx.b()])
                    P.op("act", L("activation", out=gx[:, 0:N], in_=Sx[:, 0:N], func=AF.Identity, bias=cb[:, c2:c2 + 1]),
                         reads=[Sx.b(), cb.b()], writes=[gx.b()])
                    P.op("dve", L("tensor_tensor", out=gtmp[:, 0:N], in0=gx[:, 0:N], in1=gx[:, 0:N], op=ALU.mult),
                         reads=[gx.b()], writes=[gtmp.b()])
                    P.op("dve", L("tensor_scalar", gtmp[:, 0:N], gtmp[:, 0:N], 0.044715, 1.0, ALU.mult, ALU.add),
                         reads=[gtmp.b()], writes=[gtmp.b()])
                    P.op("dve", L("tensor_tensor", out=gtmp[:, 0:N], in0=gtmp[:, 0:N], in1=gx[:, 0:N], op=ALU.mult),
                         reads=[gtmp.b(), gx.b()], writes=[gtmp.b()])
                    P.op("act", L("activation", out=gtmp[:, 0:N], in_=gtmp[:, 0:N], func=AF.Tanh, scale=GELU_C),
                         reads=[gtmp.b()], writes=[gtmp.b()])
                    P.op("dve", L("tensor_scalar", gtmp[:, 0:N], gtmp[:, 0:N], 0.5, 0.5, ALU.mult, ALU.add),
                         reads=[gtmp.b()], writes=[gtmp.b()])
                    P.op("dve", L("tensor_tensor", out=hg[:, c2, 0:N], in0=gtmp[:, 0:N], in1=gx[:, 0:N], op=ALU.mult),
                         reads=[gtmp.b(), gx.b()], writes=[b_hg])
                if kv == 0:
                    for c2 in range(2):
                        P.op("pe", L("matmul", S[2][0:64, 0:N], lhsT=w2b[:, c2, :], rhs=hg[:, c2, 0:N],
                                     start=(c2 == 0), stop=(c2 == 1)), reads=[w2b.b(), b_hg], writes=[S[2].b()])
                    P.op("act", L("copy", out=kcmpT[:, g, n0:n0 + N], in_=S[2][0:64, 0:N]),
                         reads=[S[2].b()], writes=[kcmpT.b(g)])
                else:
                    for t4 in range(4):
                        ct = nt * 4 + t4
                        for c2 in range(2):
                            P.op("pe", L("matmul", S[2][:, t4 * 64:(t4 + 1) * 64], lhsT=hg[:, c2, t4 * 128:(t4 + 1) * 128],
                                         rhs=w2b[:, c2, :], start=(c2 == 0), stop=(c2 == 1)),
                                 reads=[w2b.b(), b_hg], writes=[S[2].b()])
                    P.op("act", L("copy", out=AV[:, nt * 4:nt * 4 + 4, g, 256:320],
                                  in_=S[2][:, 0:256].rearrange("p (t d) -> p t d", t=4)),
                         reads=[S[2].b()], writes=[AV.b(("v", g, nt))])
    av_reads = {g: [AV.b(("a", g)), AV.b("one"), AV.b(("v", g, 0)), AV.b(("v", g, 1))] for g in range(2)}

    for c in range(8):
        for g in range(2):
            sp_load(Vaug[:, c * 16:(c + 1) * 16, g, 0:64],
                    vs_d[c * 2048:(c + 1) * 2048, g * 64:(g + 1) * 64].rearrange("(t p) d -> p t d", p=128),
                    [arena.b(("v", c, g)), b_x2, b_w1, b_hg])
    P.op("dve", L("memset", Vaug[:, :, :, 64:65], 1.0), writes=[arena.b("vone"), b_x2, b_w1, b_hg])

    def vreads(kt, g):
        return [arena.b(("v", kt // 16, g)), arena.b("vone")]

    def mask_rhs(i):
        return masks[:, i:i + 1, :].to_broadcast([128, 4, 128])

    def branch_epilogue(g, gate_idx, oT, Tt):
        first = False
        P.op("act", L("copy", out=osb[g][:, :], in_=oT[0:65, :]), reads=[oT.b()], writes=[osb[g].b()])
        T = Tt[:, 0:260].rearrange("p (h e) -> p h e", h=4)
        for h in range(4):
            P.op("pe", L("transpose", T[:, h, :], osb[g][:, h * 128:(h + 1) * 128], idf[0:65, 0:65]),
                 reads=[osb[g].b(), idf.b()], writes=[Tt.b()])
        rd = sm[:, 0:4]
        P.op("dve", L("tensor_scalar", rd, T[:, :, 64], 1e-30, None, ALU.max), reads=[Tt.b()], writes=[b_sm[0]])
        P.op("dve", L("reciprocal", rd, rd), reads=[b_sm[0]], writes=[b_sm[0]])
        sg3 = sgt[:, :].rearrange("p (h b) -> p h b", b=3)
        P.op("dve", L("tensor_tensor", out=rd, in0=rd, in1=sg3[:, 4 * g:4 * g + 4, gate_idx], op=ALU.mult),
             reads=[b_sm[0], sgt.b()], writes=[b_sm[0]])
        for h in range(4):
            c0 = (4 * g + h) * 64
            if first:
                P.op("dve", L("tensor_scalar", oacc[:, c0:c0 + 64], T[:, h, 0:64], sm[:, h:h + 1], None, ALU.mult),
                     reads=[Tt.b(), b_sm[0]], writes=[oacc.b(g)])
            else:
                P.op("dve", L("scalar_tensor_tensor", out=oacc[:, c0:c0 + 64], in0=T[:, h, 0:64], scalar=sm[:, h:h + 1],
                              in1=oacc[:, c0:c0 + 64], op0=ALU.mult, op1=ALU.add),
                     reads=[Tt.b(), b_sm[0], oacc.b(g)], writes=[oacc.b(g)])

    cnt = {"s": 0}

    def score_tile(lhsT, lhs_reads, rhs, rhs_reads, mask_i, nslots=4):
        si = cnt["s"] % nslots
        i = cnt["s"] % 4
        cnt["s"] += 1
        P.op("pe", L("matmul", S[si][:, :], lhsT=lhsT, rhs=rhs, start=True, stop=(mask_i is None)),
             reads=lhs_reads + rhs_reads, writes=[S[si].b()])
        if mask_i is not None:
            P.op("pe", L("matmul", S[si][:, :], lhsT=idb[:, :], rhs=mask_rhs(mask_i), start=False, stop=True),
                 reads=[idb.b(), masks.b()], writes=[S[si].b()])
        P.op("act", L("activation", out=EP[i][:, :], in_=S[si][:, :], func=AF.Exp, scale=SCALE),
             reads=[S[si].b()], writes=[EP[i].b()])
        return i

    for jidx, j in enumerate(jlist):
        t0 = j * 128
        QT, QR, kwt, vwa, gt, ut, bgt = (t_[jidx % 2] for t_ in (QTs, QRs, kwts, vwas, gts, uts, bgts))
        sp_load(QT[:, :, :], qT_d[:, :, t0:t0 + 128].rearrange("h d q -> d h q"), [QT.b()])
        sp_load(QR[:, :, :], qrT_d[:, :, t0:t0 + 128].rearrange("h d q -> d h q"), [QR.b()])
        sp_load(kwt[:, :, :], kwin_d[j, :, :, :].rearrange("g d k -> d g k"), [kwt.b()])
        for g in range(2):
            sp_load(vwa[:, :, g, 0:64], vwin_d[j, :, g * 64:(g + 1) * 64].rearrange("(r p) d -> p r d", p=128), [vwa.b(("v", g))])
        sp_load(gt[:, :], gates_d[t0:t0 + 128, :], [gt.b()])
        sp_load(ut[:, :, 2:130], uT_d[:, :, t0:t0 + 128].rearrange("c p q -> p c q"), [ut.b("m")])
        sp_load(ut[:, :, 0:2], uhalo_d[j, :, :, :].rearrange("c p k -> p c k"), [ut.b("h")])
        sp_load(bgt[:, :, :], bgT_d[:, :, t0:t0 + 128].rearrange("c p q -> p c q"), [bgt.b()])
        sp_load(x1t[:, :], x1_d[t0:t0 + 128, :], [x1t.b()])
        P.op("act", L("activation", out=sgt[:, :], in_=gt[:, :], func=AF.Exp, scale=-1.0), reads=[gt.b()], writes=[sgt.b()])
        P.op("dve", L("tensor_scalar", sgt[:, :], sgt[:, :], 1.0, None, ALU.add), reads=[sgt.b()], writes=[sgt.b()])
        P.op("dve", L("reciprocal", sgt[:, :], sgt[:, :]), reads=[sgt.b()], writes=[sgt.b()])
        for c in range(4):
            P.op("pool", L("tensor_scalar", cvy[:, :], ut[:, c, 0:128], cw[:, 3 * c:3 * c + 1], None, ALU.mult),
                 reads=[ut.b("m"), ut.b("h"), cw.b()], writes=[cvy.b()])
            for k in (1, 2):
                P.op("pool", L("tensor_scalar", cvt[:, :], ut[:, c, k:k + 128], cw[:, 3 * c + k:3 * c + k + 1], None, ALU.mult),
                     reads=[ut.b("m"), ut.b("h"), cw.b()], writes=[cvt.b()])
                P.op("pool", L("tensor_tensor", out=cvy[:, :], in0=cvy[:, :], in1=cvt[:, :], op=ALU.add),
                     reads=[cvt.b(), cvy.b()], writes=[cvy.b()])
            P.op("pool", L("tensor_tensor", out=cvy[:, :], in0=cvy[:, :], in1=bgt[:, c, :], op=ALU.mult),
                 reads=[cvy.b(), bgt.b()], writes=[cvy.b()])
            P.op("pool", L("tensor_scalar", mT[:, 4 + c, :], cvy[:, :], gc[:, c:c + 1], None, ALU.mult),
                 reads=[cvy.b(), gc.b()], writes=[mT.b(4 + c)])
            P.op("pool", L("tensor_tensor", out=cvq[:, c, :], in0=cvy[:, :], in1=cvy[:, :], op=ALU.mult),
                 reads=[cvy.b()], writes=[cvq.b(c)])
        sg3 = sgt[:, :].rearrange("p (h b) -> p h b", b=3)
        nct = (32 * j + 30) // 128 + 1
        nkt = 4 * j + 4
        nW = (nkt - 1) // 32 + 1
        def front(g, part):
            qrhs = QT[:, 4 * g:4 * g + 4, :]
            pc = [S[2], S[3], G[0], G[1]]
            if part == "A":
                cslot = {}

                def cmp_qk(ct):
                    rp = 4 * j - 16 * ct
                    i = cnt["s"] % 2
                    cnt["s"] += 1
                    msk = rp // 4 if rp <= 16 else None
                    P.op("pe", L("matmul", S[i][:, :], lhsT=kcmpT[:, g, ct * 128:(ct + 1) * 128], rhs=QR[:, 4 * g:4 * g + 4, :],
                                 start=True, stop=(msk is None)), reads=[kcmpT.b(g), QR.b()], writes=[S[i].b()])
                    if msk is not None:
                        P.op("pe", L("matmul", S[i][:, :], lhsT=idb[:, :], rhs=mask_rhs(msk), start=False, stop=True),
                             reads=[idb.b(), masks.b()], writes=[S[i].b()])
                    P.op("act", L("activation", out=EP[i][:, :], in_=S[i][:, :], func=AF.Exp, scale=SCALE),
                         reads=[S[i].b()], writes=[EP[i].b()])
                    cslot[ct] = i

                cmp_qk(0)
                for ct in range(nct):
                    if ct + 1 < nct:
                        cmp_qk(ct + 1)
                    i = cslot[ct]
                    for h in range(4):
                        P.op("pe", L("matmul", pc[h][:, 0:321], lhsT=EP[i][:, h * 128:(h + 1) * 128], rhs=AV[:, ct, g, :],
                                     start=(ct == 0), stop=(ct == nct - 1)),
                             reads=[EP[i].b()] + av_reads[g], writes=[pc[h].b()])
            if part == "B":
                rd = sm[:, 4:8]
                for h in range(4):
                    P.op("dve", L("tensor_scalar", sm[:, 4 + h:5 + h], pc[h][:, 320:321], 1e-30, None, ALU.max),
                         reads=[pc[h].b()], writes=[b_sm[1]])
                P.op("dve", L("reciprocal", rd, rd), reads=[b_sm[1]], writes=[b_sm[1]])
                P.op("dve", L("tensor_scalar", psl[:, :], pc[0][:, 0:256], sm[:, 4:5], None, ALU.mult),
                     reads=[pc[0].b(), b_sm[1]], writes=[psl.b()])
                for h in range(1, 4):
                    P.op("dve", L("scalar_tensor_tensor", out=psl[:, :], in0=pc[h][:, 0:256], scalar=sm[:, 4 + h:5 + h],
                                  in1=psl[:, :], op0=ALU.mult, op1=ALU.add),
                         reads=[pc[h].b(), b_sm[1], psl.b()], writes=[psl.b()])
                wc = sm[:, 8:12]
                P.op("dve", L("tensor_tensor", out=wc, in0=rd, in1=sg3[:, 4 * g:4 * g + 4, 0], op=ALU.mult),
                     reads=[b_sm[1], sgt.b()], writes=[b_sm[2]])
                for h in range(4):
                    c0 = (4 * g + h) * 64
                    P.op("dve", L("tensor_scalar", oacc[:, c0:c0 + 64], pc[h][:, 256:320], sm[:, 8 + h:9 + h], None, ALU.mult),
                         reads=[pc[h].b(), b_sm[2]], writes=[oacc.b(g)])
                P.op("dve", L("tensor_tensor", out=score[:, :], in0=psl[:, :], in1=fpat[:, 256 - 8 * j:512 - 8 * j], op=ALU.add),
                     reads=[psl.b(), fpat.b()], writes=[score.b()])
                P.op("dve", L("memset", score[:, 0:1], 1e9), reads=[], writes=[score.b()])
                P.op("dve", L("max", out=m8[:, 0:8], in_=score[:, :]), reads=[score.b()], writes=[m8.b(0)])
                P.op("dve", L("match_replace", out=swk[:, :], in_to_replace=m8[:, 0:8], in_values=score[:, :], imm_value=-3e9),
                     reads=[score.b(), m8.b(0)], writes=[swk.b()])
                P.op("dve", L("max", out=m8[:, 8:16], in_=swk[:, :]), reads=[swk.b()], writes=[m8.b(1)])
                P.op("dve", L("tensor_reduce", out=sm[:, 15:16], in_=m8[:, 8:16], axis=AX.X, op=ALU.min),
                     reads=[m8.b(1)], writes=[b_sm[6]])
                P.op("dve", L("tensor_scalar", swk[:, :], score[:, :], sm[:, 15:16], None, ALU.is_lt),
                     reads=[score.b(), b_sm[6]], writes=[swk.b()])
                P.op("dve", L("tensor_scalar", biaspad[:, 64:320], swk[:, :], NEGB, None, ALU.mult),
                     reads=[swk.b()], writes=[biaspad.b("b")])
                if debug:
                    P.op("sp", L("dma_start", out=dbg_psl[t0:t0 + 128, g, :], in_=psl[:, :]), reads=[psl.b()],
                         writes=[dbg_psl.b((t0, g))], chan=stc)
                    P.op("sp", L("dma_start", out=dbg_bias[t0:t0 + 128, g, :], in_=biaspad[:, 64:320]), reads=[biaspad.b("b")],
                         writes=[dbg_bias.b((t0, g))], chan=stc)
            if part == "C":
                for w in range(nW):
                    P.op("pe", L("transpose", GB[:, w * 128:(w + 1) * 128], biaspad[:, 64 * w:64 * w + 128], idb[:, :]),
                         reads=[biaspad.b("b"), biaspad.b("pad"), idb.b()], writes=[GB.b()])
                for w in range(nW):
                    P.op("act", L("copy", out=Qaug[64:128, w, g, :].rearrange("p (h q) -> p h q", h=4),
                                  in_=GB[64:128, w * 128:(w + 1) * 128].unsqueeze(1).to_broadcast([64, 4, 128])),
                         reads=[GB.b()], writes=[Qaug.b((w, g, 1))])
                    P.op("pool", L("tensor_copy", Qaug[0:64, w, g, :].rearrange("p (h q) -> p h q", h=4), qrhs),
                         reads=[QT.b()], writes=[Qaug.b((w, g, 0))])
        if debug:
            P.op("sp", L("dma_start", out=dbg_o[t0:t0 + 128, 0, :], in_=oacc[:, :]), reads=[oacc.b(0), oacc.b(1)],
                 writes=[dbg_o.b((t0, 0))], chan=stc)
        def sel_loop(g, oT, nslots):
            slot = {}
            for n in range(nkt + LOOKAHEAD):
                if n < nkt:
                    kt = n
                    w = kt // 32
                    slot[n] = score_tile(Kaug[:, g, kt * 128:(kt + 1) * 128], [Kaug.b(("k", g)), Kaug.b(("o", g))],
                                         Qaug[:, w, g, :], [Qaug.b((w, g, 0)), Qaug.b((w, g, 1))],
                                         (5 + kt - 4 * j) if kt >= 4 * j else None, nslots)
                m = n - LOOKAHEAD
                if m >= 0:
                    kt = m
                    i = slot[m]
                    P.op("pe", L("matmul", oT[0:65, :], lhsT=Vaug[:, kt, g, :], rhs=EP[i][:, :],
                                 start=(kt == 0), stop=(kt == nkt - 1)), reads=[EP[i].b()] + vreads(kt, g), writes=[oT.b()])

        front(0, "A")
        front(0, "B")
        front(1, "A")
        front(0, "C")
        sel_loop(0, G[2], 2)
        front(1, "B")
        front(1, "C")
        branch_epilogue(0, 1, G[2], G[0])
        sel_loop(1, G[1], 4)
        branch_epilogue(1, 1, G[1], G[2])
        if debug:
            P.op("sp", L("dma_start", out=dbg_o[t0:t0 + 128, 1, :], in_=oacc[:, :]), reads=[oacc.b(0), oacc.b(1)],
                 writes=[dbg_o.b((t0, 1))], chan=stc)
        tiles = [(r, g) for r in range(5) for g in range(2)]
        slot = {}
        for n in range(len(tiles) + LOOKAHEAD):
            if n < len(tiles):
                r, g = tiles[n]
                if j == 0:
                    mi = 9 + r
                else:
                    mi = 14 if r == 0 else (15 if r == 4 else None)
                slot[n] = score_tile(kwt[:, g, r * 128:(r + 1) * 128], [kwt.b()], QT[:, 4 * g:4 * g + 4, :], [QT.b()], mi)
            m = n - LOOKAHEAD
            if m >= 0:
                r, g = tiles[m]
                i = slot[m]
                P.op("pe", L("matmul", G[g][0:65, :], lhsT=vwa[:, r, g, :], rhs=EP[i][:, :],
                             start=(r == 0), stop=(r == 4)), reads=[EP[i].b(), vwa.b(("v", g)), vwa.b("one")], writes=[G[g].b()])
        for g in range(2):
            branch_epilogue(g, 2, G[g], G[2])
        if debug:
            P.op("sp", L("dma_start", out=dbg_o[t0:t0 + 128, 2, :], in_=oacc[:, :]), reads=[oacc.b(0), oacc.b(1)],
                 writes=[dbg_o.b((t0, 2))], chan=stc)
        for c in range(4):
            P.op("pe", L("matmul", G[2][:, 300:301], lhsT=cvq[:, c, :], rhs=ones_f[:, :], start=(c == 0), stop=(c == 3)),
                 reads=[cvq.b(c), ones_f.b()], writes=[G[2].b()])
        P.op("act", L("copy", out=sm[:, 13:14], in_=G[2][:, 300:301]), reads=[G[2].b()], writes=[b_sm[4]])
        P.op("act", L("activation", out=sq[:, 0:512], in_=oacc[:, :], func=AF.Square, accum_out=sm[:, 12:13]),
             reads=[oacc.b(0), oacc.b(1)], writes=[sq.b(), b_sm[4]])
        P.op("act", L("activation", out=sm[:, 12:14], in_=sm[:, 12:14], func=AF.Sqrt, bias=C["eps1"][:, 0:1], scale=1.0 / 512),
             reads=[b_sm[4], C["eps1"].b()], writes=[b_sm[4]])
        P.op("dve", L("reciprocal", sm[:, 12:14], sm[:, 12:14]), reads=[b_sm[4]], writes=[b_sm[4]])
        P.op("dve", L("scalar_tensor_tensor", out=attb[:, :], in0=oacc[:, :], scalar=sm[:, 12:13], in1=ga[:, :],
                      op0=ALU.mult, op1=ALU.mult), reads=[oacc.b(0), oacc.b(1), b_sm[4], ga.b()], writes=[attb.b()])
        for k in range(4):
            P.op("pe", L("transpose", GB[:, k * 128:(k + 1) * 128], attb[:, k * 128:(k + 1) * 128], idb[:, :]),
                 reads=[attb.b(), idb.b()], writes=[GB.b()])
        P.op("act", L("copy", out=mT[:, 0:4, :], in_=GB[:, 0:512].rearrange("p (k q) -> p k q", k=4)),
             reads=[GB.b()], writes=[mT.b(k) for k in range(4)])
        for nh in range(2):
            for k in range(4):
                P.op("pe", L("matmul", S[nh][:, :], lhsT=mT[:, k, :], rhs=wout[:, k, nh * 512:(nh + 1) * 512],
                             start=(k == 0), stop=(k == 3)), reads=[mT.b(k), wout.b(k)], writes=[S[nh].b()])
            for k in range(4, 8):
                P.op("pe", L("matmul", S[2 + nh][:, :], lhsT=mT[:, k, :], rhs=wout[:, k, nh * 512:(nh + 1) * 512],
                             start=(k == 4), stop=(k == 7)), reads=[mT.b(k), wout.b(k)], writes=[S[2 + nh].b()])
        for nh in range(2):
            P.op("act", L("copy", out=hA[:, nh * 512:(nh + 1) * 512], in_=S[nh][:, :]), reads=[S[nh].b()], writes=[hA.b(nh)])
            P.op("dve", L("scalar_tensor_tensor", out=hA[:, nh * 512:(nh + 1) * 512], in0=S[2 + nh][:, :], scalar=sm[:, 13:14],
                          in1=hA[:, nh * 512:(nh + 1) * 512], op0=ALU.mult, op1=ALU.add),
                 reads=[S[2 + nh].b(), b_sm[4], hA.b(nh)], writes=[hA.b(nh)])
        P.op("act", L("activation", out=sq[:, :], in_=hA[:, :], func=AF.Square, accum_out=sm[:, 14:15]),
             reads=[hA.b(0), hA.b(1)], writes=[sq.b(), b_sm[5]])
        P.op("act", L("activation", out=sm[:, 14:15], in_=sm[:, 14:15], func=AF.Sqrt, bias=C["eps1"][:, 0:1], scale=1.0 / D),
             reads=[b_sm[5], C["eps1"].b()], writes=[b_sm[5]])
        P.op("dve", L("reciprocal", sm[:, 14:15], sm[:, 14:15]), reads=[b_sm[5]], writes=[b_sm[5]])
        P.op("dve", L("scalar_tensor_tensor", out=hA[:, :], in0=hA[:, :], scalar=sm[:, 14:15], in1=gpost[:, :],
                      op0=ALU.mult, op1=ALU.mult), reads=[hA.b(0), hA.b(1), b_sm[5], gpost.b()], writes=[hA.b(0), hA.b(1)])
        P.op("dve", L("tensor_tensor", out=hA[:, :], in0=hA[:, :], in1=x1t[:, :], op=ALU.add),
             reads=[hA.b(0), hA.b(1), x1t.b()], writes=[hA.b(0), hA.b(1)])
        P.op("sp", L("dma_start", out=x2_d[t0:t0 + 128, :], in_=hA[:, :]), reads=[hA.b(0), hA.b(1)],
             writes=[x2_d.b(t0)], chan=stc)
    P.finish()
    return nc

import ml_dtypes
NPBF = ml_dtypes.bfloat16
NQB = NTOK // 128


def own_tokens(arr_seq, i):
    a = arr_seq.reshape(NQB, 4, 128, *arr_seq.shape[1:])
    return np.ascontiguousarray(a[:, i].reshape(NTOK, *arr_seq.shape[1:]))


def scatter_tokens(shards):
    a = np.stack([s.reshape(NQB, 128, *s.shape[1:]) for s in shards], axis=1)
    return a.reshape(SEQ, *shards[0].shape[1:])


def full_T(shards):
    lead = shards[0].shape[:-1]
    a = np.stack([s.reshape(*lead, NQB, 128) for s in shards], axis=-2)
    return a.reshape(*lead, SEQ)


def attn_consts(i):
    kk = np.arange(128)[:, None]
    q = np.arange(128)[None, :]
    m = np.zeros((128, 16, 128), np.float32)
    for mi in range(5):
        valid = (16 * kk + 31 - q) <= 128 * (4 * mi + i)
        m[:, mi, :] = np.where(valid, 0.0, NEGB)
    for r in range(4):
        if r == i:
            m[:, 5 + r, :] = np.where(kk > q, NEGB, 0.0)
        elif r > i:
            m[:, 5 + r, :] = NEGB
    for r in range(5):
        diff = 512 - 128 * r + q - kk
        key = 128 * i - 512 + 128 * r + kk
        valid = (key >= 0) & (diff >= 0) & (diff < 512)
        m[:, 9 + r, :] = np.where(valid, 0.0, NEGB)
    m[:, 14, :] = np.where(kk > q, 0.0, NEGB)
    m[:, 15, :] = np.where(kk <= q, 0.0, NEGB)
    fp = np.zeros((128, 768), np.float32)
    for cp in range(768):
        c = cp - 2 * i
        if c < 0:
            continue
        for half, cur in ((slice(0, 64), 256), (slice(64, 128), 257)):
            if c == cur or c == cur - 1:
                fp[half, cp] = 1e9
            elif c > cur:
                fp[half, cp] = -1e9
    return m.astype(NPBF), fp.astype(NPBF)


def static_consts():
    key = np.arange(SEQ)
    onehot = (((key // 64) % 64)[None, :] == np.arange(64)[:, None]).astype(NPBF)
    agg = [1.0, 2.0, 2.0, 2.0, 1.0]
    A = np.zeros((1024, 256), np.float32)
    for jb in range(256):
        for o in range(5):
            n = 4 * jb + o - 1
            if 0 <= n < 1023:
                A[n, jb] = agg[o]
    return onehot, A.astype(NPBF)


def attn_in_maps(batch_outs, x1_shards, params):
    ksT = full_T([o["ksT"] for o in batch_outs])
    kwT = full_T([o["kwT"] for o in batch_outs])
    kcvc = full_T([o["kcvcT"] for o in batch_outs])
    uT = full_T([o["uT"] for o in batch_outs])
    vsw = scatter_tokens([o["vsw"] for o in batch_outs])
    vs = np.ascontiguousarray(vsw[:, :128])
    vw = vsw[:, 128:]
    kc2 = np.ascontiguousarray(kcvc.reshape(2, 2, 64, SEQ // 2, 2).transpose(0, 1, 4, 2, 3).reshape(2, 2, 128, SEQ // 2))
    kw_pad = np.concatenate([np.zeros((2, 64, 512), NPBF), kwT], axis=2)
    vw_pad = np.concatenate([np.zeros((512, 128), NPBF), vw], axis=0)
    u_pad = np.concatenate([np.zeros((4, 128, 2), NPBF), uT], axis=2)
    onehot, amat = static_consts()
    maps = []
    for i in range(4):
        s0 = (4 * np.arange(NQB) + i) * 128
        kwin = np.stack([kw_pad[:, :, s:s + 640] for s in s0])
        vwin = np.stack([vw_pad[s:s + 640] for s in s0])
        uhalo = np.stack([u_pad[:, :, s:s + 2] for s in s0])
        masks, fpat = attn_consts(i)
        o = batch_outs[i]
        m = {"qT": o["qT"], "qrT": o["qrT"], "ksT": ksT, "vs": vs, "kwin": np.ascontiguousarray(kwin), "vwin": np.ascontiguousarray(vwin),
             "kc2": kc2, "gates": o["gates"], "bgT": o["bgT"], "uT": o["uT"], "uhalo": np.ascontiguousarray(uhalo),
             "x1": x1_shards[i], "ident": _ident(), "onehot": onehot, "amat": amat, "masks": masks, "fpat": fpat}
        m.update(params)
        maps.append({k: np.ascontiguousarray(v) for k, v in m.items()})
    return maps


def attn_params(inp, l):
    pe2 = np.stack([inp[n][l].reshape(16, 2, 64).transpose(1, 2, 0).reshape(128, 16) for n in ("cmp_pe_k", "cmp_pe_v")])
    return {
        "pe2": np.ascontiguousarray(pe2), "w1": np.stack([inp["cmp_w1_k"][l], inp["cmp_w1_v"][l]]),
        "w2": np.stack([inp["cmp_w2_k"][l], inp["cmp_w2_v"][l]]),
        "convw": np.ascontiguousarray(inp["conv_w"][l].reshape(3, 4, 128).transpose(2, 1, 0).reshape(128, 12)),
        "ga": inp["attn_out_norm"][l].reshape(1, 512),
        "gc": np.ascontiguousarray(inp["conv_out_norm"][l].reshape(4, 128).T),
        "w_out": inp["w_out"][l], "g_post": inp["mix_norm_post"][l].reshape(1, D),
    }


def _ident():
    return np.eye(128, dtype=np.float32)


def _launch(nc, in_maps):
    res = run_bass_kernel_spmd(nc, in_maps, core_ids=list(range(NCORES)))
    return res.results


def _run_ffn(xs, inp, pref, l):
    nc = build_ffn()
    maps = [{"x": np.ascontiguousarray(x_), "g_pre": np.ascontiguousarray(inp[pref + "_norm_pre"][l].reshape(1, D)),
             "g_post": np.ascontiguousarray(inp[pref + "_norm_post"][l].reshape(1, D)),
             "w_gate": np.ascontiguousarray(inp[pref + "_w_gate"][l]), "w_up": np.ascontiguousarray(inp[pref + "_w_up"][l]),
             "w_down": np.ascontiguousarray(inp[pref + "_w_down"][l]), "ident": _ident()} for x_ in xs]
    return [r["y"] for r in _launch(nc, maps)]


def _run_inproj(xs, pos_shards, inp, l):
    nc = build_inproj()
    rc = rope_consts()
    maps = [{"x": np.ascontiguousarray(x_), "g_pre": np.ascontiguousarray(inp["mix_norm_pre"][l].reshape(1, D)),
             "w_in": np.ascontiguousarray(inp["w_in"][l]), "pos": p_, "ropec": rc, "ident": _ident()}
            for x_, p_ in zip(xs, pos_shards)]
    return _launch(nc, maps)


def _run_attn(outs, xs, inp, l):
    nc = build_attn()
    params = attn_params(inp, l)
    maps = []
    for b in range(2):
        maps += attn_in_maps(outs[4 * b:4 * b + 4], xs[4 * b:4 * b + 4], params)
    return [r["x2"] for r in _launch(nc, maps)]


def kernel(**inp):
    inp = {k: np.asarray(v) for k, v in inp.items()}
    x = inp["x"].astype(np.float32, copy=False)
    pos = inp["positions"].astype(np.int32, copy=False)
    xs = [own_tokens(x[c // 4], c % 4) for c in range(NCORES)]
    ps = [np.ascontiguousarray(own_tokens(pos[c // 4], c % 4).reshape(1, NTOK)) for c in range(NCORES)]
    for l in range(2):
        xs = _run_ffn(xs, inp, "ffn1", l)
        outs = _run_inproj(xs, ps, inp, l)
        xs = _run_attn(outs, xs, inp, l)
        xs = _run_ffn(xs, inp, "ffn2", l)
    out = np.stack([scatter_tokens(xs[4 * b:4 * b + 4]) for b in range(2)])
    return np.ascontiguousarray(out.astype(np.float32, copy=False))
```

```python
import math
import numpy as np
import concourse.bass as bass
import concourse.mybir as mybir
from concourse.bass_utils import run_bass_kernel_spmd

F32 = mybir.dt.float32
BF16 = mybir.dt.bfloat16
I32 = mybir.dt.int32
AF = mybir.ActivationFunctionType
ALU = mybir.AluOpType
AX = mybir.AxisListType

NCORES = 8
D = 1024
DFF = 2816
NFC = DFF // 128
SEQ = 16384
NTOK = 4096
TG = 1024
EPS = 1e-6


class Buf:
    __slots__ = ("w", "r", "excl")

    def __init__(self, excl=False):
        self.w = None
        self.r = {}
        self.excl = excl


class Tile:
    def __init__(self, handle, excl=False):
        self.h = handle
        self.bufs = {}
        self.excl = excl

    def b(self, key=None):
        bb = self.bufs.get(key)
        if bb is None:
            bb = self.bufs[key] = Buf(self.excl)
        return bb

    def __getitem__(self, k):
        return self.h[k]


EPOCH = 24000


class Prog:
    STREAMS = ("pe", "act", "dve", "pool", "sp")

    def __init__(self, nc):
        self.nc = nc
        self.streams = {s: [] for s in self.STREAMS}
        self.count = {}
        self.unit = {s: 1 for s in self.STREAMS}
        self.seen = {s: {} for s in self.STREAMS}
        self.sems = {}
        self.nt = 0

    def sb(self, shape, dt, name=None):
        self.nt += 1
        return Tile(self.nc.alloc_sbuf_tensor(name or f"t{self.nt}", list(shape), dt))

    def ps(self, shape, dt=F32, name=None):
        self.nt += 1
        return Tile(self.nc.alloc_psum_tensor(name or f"p{self.nt}", list(shape), dt), excl=True)

    def dram(self, name, shape, dt, kind="Internal"):
        return Tile(self.nc.dram_tensor(name, list(shape), dt, kind=kind).ap())

    def dma_chan(self, name):
        self.unit[name] = 16
        return name

    def op(self, stream, fn, reads=(), writes=(), chan=None):
        chan = chan or stream
        deps = {}
        for b in reads:
            if b.w is not None:
                c, n = b.w
                if deps.get(c, 0) < n:
                    deps[c] = n
            if b.excl:
                for c, n in b.r.items():
                    if c != chan and deps.get(c, 0) < n:
                        deps[c] = n
        for b in writes:
            if b.w is not None:
                c, n = b.w
                if deps.get(c, 0) < n:
                    deps[c] = n
            for c, n in b.r.items():
                if deps.get(c, 0) < n:
                    deps[c] = n
        waits = []
        seen = self.seen[stream]
        for c, n in deps.items():
            if c == "pe" and stream == "pe" and chan == "pe":
                continue
            if seen.get(c, 0) >= n:
                continue
            seen[c] = n
            waits.append((c, n))
        idx = self.count.get(chan, 0) + 1
        self.count[chan] = idx
        self.streams[stream].append((fn, waits, chan, idx))
        for b in reads:
            if b.r.get(chan, 0) < idx:
                b.r[chan] = idx
        for b in writes:
            b.w = (chan, idx)
            b.r = {}
        return idx

    def _semval(self, chan, idx):
        unit = self.unit[chan]
        per = EPOCH // unit
        ep = (idx - 1) // per
        key = (chan, ep)
        sem = self.sems.get(key)
        if sem is None:
            sem = self.sems[key] = self.nc.alloc_semaphore(f"s_{chan}_{ep}")
        return sem, ((idx - 1) % per + 1) * unit

    def _replay(self, stream, eng):
        for fn, waits, chan, idx in self.streams[stream]:
            for c, n in waits:
                sem, val = self._semval(c, n)
                eng.wait_ge(sem, val)
            ins = fn(eng)
            sem, _ = self._semval(chan, idx)
            ins.then_inc(sem, self.unit[chan])

    def finish(self):
        print("prog sizes", {k: len(v) for k, v in self.streams.items()}, flush=True)
        st = self.streams["sp"]
        final_waits = [(c, n) for c, n in self.count.items()]
        nc = self.nc
        with nc.Block() as block:
            @block.tensor
            def _(e):
                self._replay("pe", e)

            @block.scalar
            def _(e):
                self._replay("act", e)

            @block.vector
            def _(e):
                self._replay("dve", e)

            @block.gpsimd
            def _(e):
                self._replay("pool", e)

            @block.sync
            def _(e):
                self._replay("sp", e)
                for c, n in final_waits:
                    sem, val = self._semval(c, n)
                    e.wait_ge(sem, val)


def L(name, *args, **kw):
    return lambda e: getattr(e, name)(*args, **kw)

def rms_scale(P, ss, rstd, n, epsb, extra=1.0, cols=1):
    e2 = float(extra) ** 2
    P.op("act", L("activation", out=rstd[:, 0:cols], in_=ss[:, 0:cols], func=AF.Sqrt,
                  bias=epsb[:, 0:1], scale=1.0 / (n * e2)),
         reads=[ss.b(), epsb.b()], writes=[rstd.b()])
    P.op("dve", L("reciprocal", rstd[:, 0:cols], rstd[:, 0:cols]), reads=[rstd.b()], writes=[rstd.b()])


def make_consts(P, id_d, ld):
    C = {}
    C["idf"] = P.sb([128, 128], F32)
    C["idb"] = P.sb([128, 128], BF16)
    P.op("sp", L("dma_start", out=C["idf"][:, :], in_=id_d[:, :]), writes=[C["idf"].b()], chan=ld)
    P.op("dve", L("tensor_copy", C["idb"][:, :], C["idf"][:, :]), reads=[C["idf"].b()], writes=[C["idb"].b()])
    for nm, v in (("eps1", EPS), ("eps4", 4.0 * EPS)):
        C[nm] = P.sb([128, 1], F32)
        P.op("dve", L("memset", C[nm][:, :], v), writes=[C[nm].b()])
    return C


def prenorm_T(P, C, xsrc, xb, g_bc, hT, hT_b, col0, st):
    P.op("act", L("activation", out=st["sq"][:, :], in_=xsrc, func=AF.Square, accum_out=st["ss"][:, 0:1]),
         reads=[xb], writes=[st["sq"].b(), st["ss"].b()])
    rms_scale(P, st["ss"], st["rstd"], D, C["eps1"])
    P.op("dve", L("scalar_tensor_tensor", out=st["hb"][:, :], in0=xsrc, scalar=st["rstd"][:, 0:1], in1=g_bc[:, :],
                  op0=ALU.mult, op1=ALU.mult),
         reads=[xb, st["rstd"].b(), g_bc.b()], writes=[st["hb"].b()])
    for k in range(8):
        P.op("pe", L("transpose", st["tp"][:, k * 128:(k + 1) * 128], st["hb"][:, k * 128:(k + 1) * 128], C["idb"][:, :]),
             reads=[st["hb"].b(), C["idb"].b()], writes=[st["tp"].b()])
    P.op("act", L("copy", out=hT[:, :, col0:col0 + 128], in_=st["tp"][:, :].rearrange("p (k t) -> p k t", k=8)),
         reads=[st["tp"].b()], writes=[hT_b])


def prenorm_scratch(P):
    return {"sq": P.sb([128, D], F32), "ss": P.sb([128, 1], F32), "rstd": P.sb([128, 1], F32),
            "hb": P.sb([128, D], BF16), "tp": P.ps([128, D], BF16)}


def bcast_load(P, dst, src_d, n, ld):
    P.op("sp", L("dma_start", out=dst[:, :], in_=src_d[0:1, 0:n].partition_broadcast(128)), writes=[dst.b()], chan=ld)


def build_ffn(ntok=NTOK, tg=TG):
    nc = bass.Bass("TRN2", target_bir_lowering=False)
    P = Prog(nc)
    x = P.dram("x", [ntok, D], F32, kind="ExternalInput")
    gpre_d = P.dram("g_pre", [1, D], F32, kind="ExternalInput")
    gpost_d = P.dram("g_post", [1, D], F32, kind="ExternalInput")
    wg_d = P.dram("w_gate", [D, DFF], F32, kind="ExternalInput")
    wu_d = P.dram("w_up", [D, DFF], F32, kind="ExternalInput")
    wd_d = P.dram("w_down", [DFF, D], F32, kind="ExternalInput")
    id_d = P.dram("ident", [128, 128], F32, kind="ExternalInput")
    y = P.dram("y", [ntok, D], F32, kind="ExternalOutput")
    ld = P.dma_chan("ld")
    wl = P.dma_chan("wl")
    stc = P.dma_chan("st")
    ntt = tg // 128
    ntb = tg // 512
    C = make_consts(P, id_d, ld)
    gpre = P.sb([128, D], F32)
    gpost = P.sb([128, D], F32)
    bcast_load(P, gpre, gpre_d, D, ld)
    bcast_load(P, gpost, gpost_d, D, ld)
    xg = P.sb([128, ntt, D], F32)
    st = prenorm_scratch(P)
    hT = P.sb([128, 8, tg], BF16)
    wg = [P.sb([128, 8, 256], BF16) for _ in range(2)]
    wu = [P.sb([128, 8, 256], BF16) for _ in range(2)]
    wd = P.sb([128, NFC, D], BF16)
    aT = P.sb([128, NFC, tg], BF16)
    sg = [P.sb([128, 512], F32) for _ in range(2)]
    ss2 = [P.sb([128, 2], F32) for _ in range(2)]
    ss2s = [P.sb([128, 1], F32) for _ in range(2)]
    rstd2 = [P.sb([128, 1], F32) for _ in range(2)]
    yt = [P.sb([128, D], F32) for _ in range(2)]
    pg = [P.ps([128, 512], F32) for _ in range(2)]
    pu = [P.ps([128, 512], F32) for _ in range(2)]
    py = [P.ps([128, 512], F32) for _ in range(2)]
    for fc in range(NFC):
        P.op("pool", L("dma_start", out=wd[:, fc, :], in_=wd_d[fc * 128:(fc + 1) * 128, :]), writes=[wd.b(fc)], chan=wl)
    for g in range(ntok // tg):
        t0 = g * tg
        for tt in range(ntt):
            r0 = t0 + tt * 128
            P.op("sp", L("dma_start", out=xg[:, tt, :], in_=x[r0:r0 + 128, :]), writes=[xg.b(tt)], chan=ld)
            prenorm_T(P, C, xg[:, tt, :], xg.b(tt), gpre, hT, hT.b(tt), tt * 128, st)
        for fg in range(NFC // 2):
            s = fg % 2
            f0 = fg * 256
            P.op("pool", L("dma_start", out=wg[s][:, :, :], in_=wg_d[:, f0:f0 + 256].rearrange("(k p) f -> p k f", p=128)),
                 writes=[wg[s].b()], chan=wl)
            P.op("pool", L("dma_start", out=wu[s][:, :, :], in_=wu_d[:, f0:f0 + 256].rearrange("(k p) f -> p k f", p=128)),
                 writes=[wu[s].b()], chan=wl)
            for c2 in range(2):
                fc = fg * 2 + c2
                for tb in range(ntb):
                    ps_ = (c2 * ntb + tb) % 2
                    hreads = [hT.b(tt) for tt in range(tb * 4, tb * 4 + 4)]
                    for k in range(8):
                        P.op("pe", L("matmul", pg[ps_][:, :], lhsT=wg[s][:, k, c2 * 128:(c2 + 1) * 128],
                                     rhs=hT[:, k, tb * 512:(tb + 1) * 512], start=(k == 0), stop=(k == 7)),
                             reads=[wg[s].b()] + hreads, writes=[pg[ps_].b()])
                    for k in range(8):
                        P.op("pe", L("matmul", pu[ps_][:, :], lhsT=wu[s][:, k, c2 * 128:(c2 + 1) * 128],
                                     rhs=hT[:, k, tb * 512:(tb + 1) * 512], start=(k == 0), stop=(k == 7)),
                             reads=[wu[s].b()] + hreads, writes=[pu[ps_].b()])
                    P.op("act", L("activation", out=sg[ps_][:, :], in_=pg[ps_][:, :], func=AF.Silu),
                         reads=[pg[ps_].b()], writes=[sg[ps_].b()])
                    P.op("dve", L("tensor_tensor", out=aT[:, fc, tb * 512:(tb + 1) * 512], in0=pu[ps_][:, :],
                                  in1=sg[ps_][:, :], op=ALU.mult),
                         reads=[sg[ps_].b(), pu[ps_].b()], writes=[aT.b((fc, tb))])
        for tt in range(ntt):
            s = tt % 2
            tb = tt // 4
            for nh in range(2):
                for fc in range(NFC):
                    P.op("pe", L("matmul", py[nh][:, :], lhsT=aT[:, fc, tt * 128:(tt + 1) * 128],
                                 rhs=wd[:, fc, nh * 512:(nh + 1) * 512], start=(fc == 0), stop=(fc == NFC - 1)),
                         reads=[aT.b((fc, tb)), wd.b(fc)], writes=[py[nh].b()])
                P.op("act", L("activation", out=st["sq"][:, nh * 512:(nh + 1) * 512], in_=py[nh][:, :],
                              func=AF.Square, accum_out=ss2[s][:, nh:nh + 1]),
                     reads=[py[nh].b()], writes=[st["sq"].b(), ss2[s].b(nh)])
            P.op("dve", L("tensor_tensor", out=ss2s[s][:, 0:1], in0=ss2[s][:, 0:1], in1=ss2[s][:, 1:2], op=ALU.add),
                 reads=[ss2[s].b(0), ss2[s].b(1)], writes=[ss2s[s].b()])
            rms_scale(P, ss2s[s], rstd2[s], D, C["eps4"], extra=0.5)
            for nh in range(2):
                P.op("dve", L("scalar_tensor_tensor", out=yt[s][:, nh * 512:(nh + 1) * 512], in0=py[nh][:, :],
                              scalar=rstd2[s][:, 0:1], in1=gpost[:, nh * 512:(nh + 1) * 512], op0=ALU.mult, op1=ALU.mult),
                     reads=[py[nh].b(), rstd2[s].b(), gpost.b()], writes=[yt[s].b(nh)])
            P.op("dve", L("tensor_tensor", out=yt[s][:, :], in0=yt[s][:, :], in1=xg[:, tt, :], op=ALU.add),
                 reads=[yt[s].b(0), yt[s].b(1), xg.b(tt)], writes=[yt[s].b(0), yt[s].b(1)])
            r0 = t0 + tt * 128
            P.op("sp", L("dma_start", out=y[r0:r0 + 128, :], in_=yt[s][:, :]),
                 reads=[yt[s].b(0), yt[s].b(1)], writes=[y.b(r0)], chan=stc)
    P.finish()
    return nc

DIN = 2840
C_Q, C_KC, C_VC, C_KS, C_VS, C_KW, C_VW, C_GT, C_BG, C_CG, C_XC = 0, 512, 640, 768, 896, 1024, 1152, 1280, 1304, 1816, 2328
TWO_PI = 2.0 * math.pi
CW1 = 6.28125
CW2 = TWO_PI - CW1


def build_inproj(ntok=NTOK, stage=99):
    nc = bass.Bass("TRN2", target_bir_lowering=False)
    P = Prog(nc)
    x = P.dram("x", [ntok, D], F32, kind="ExternalInput")
    g_d = P.dram("g_pre", [1, D], F32, kind="ExternalInput")
    win_d = P.dram("w_in", [D, DIN], F32, kind="ExternalInput")
    pos_d = P.dram("pos", [1, ntok], I32, kind="ExternalInput")
    rc_d = P.dram("ropec", [16, 4], F32, kind="ExternalInput")
    id_d = P.dram("ident", [128, 128], F32, kind="ExternalInput")
    qT_d = P.dram("qT", [8, 64, ntok], BF16, kind="ExternalOutput")
    qrT_d = P.dram("qrT", [8, 64, ntok], BF16, kind="ExternalOutput")
    ksT_d = P.dram("ksT", [2, 64, ntok], BF16, kind="ExternalOutput")
    kwT_d = P.dram("kwT", [2, 64, ntok], BF16, kind="ExternalOutput")
    kcvc_d = P.dram("kcvcT", [2, 128, ntok], BF16, kind="ExternalOutput")
    vsw_d = P.dram("vsw", [ntok, 256], BF16, kind="ExternalOutput")
    gates_d = P.dram("gates", [ntok, 24], F32, kind="ExternalOutput")
    bgT_d = P.dram("bgT", [4, 128, ntok], BF16, kind="ExternalOutput")
    uT_d = P.dram("uT", [4, 128, ntok], BF16, kind="ExternalOutput")
    ld = P.dma_chan("ld")
    wl = P.dma_chan("wl")
    stc = P.dma_chan("st")
    C = make_consts(P, id_d, ld)
    gpre = P.sb([128, D], F32)
    bcast_load(P, gpre, g_d, D, ld)
    st = prenorm_scratch(P)
    w = P.sb([128, 8, DIN], BF16)
    for k in range(8):
        for (c0, c1) in ((0, 1304), (1304, DIN)):
            P.op("pool", L("dma_start", out=w[:, k, c0:c1], in_=win_d[k * 128:(k + 1) * 128, c0:c1]),
                 writes=[w.b()], chan=wl)
    wP = P.sb([128, 8, 12, 32], BF16)
    P.op("dve", L("memset", wP[:, :, :, :], 0.0), writes=[wP.b()])
    for (u0, nu, c0) in ((0, 8, C_Q), (8, 2, C_KS), (10, 2, C_KW)):
        src = w[:, :, c0:c0 + nu * 64].rearrange("p k (u d) -> p k u d", d=64)
        P.op("dve", L("tensor_scalar", wP[:, :, u0:u0 + nu, 0:8], src[:, :, :, 8:16], -1.0, None, ALU.mult),
             reads=[w.b()], writes=[wP.b()])
        P.op("dve", L("tensor_copy", wP[:, :, u0:u0 + nu, 8:16], src[:, :, :, 0:8]), reads=[w.b()], writes=[wP.b()])
    wt = P.sb([128, 8, 280], BF16)
    for (d0, c0, n) in ((0, C_VS, 128), (128, C_VW, 128), (256, C_GT, 24)):
        P.op("dve", L("tensor_copy", wt[:, :, d0:d0 + n], w[:, :, c0:c0 + n]), reads=[w.b()], writes=[wt.b()])
    rc = P.sb([16, 4], F32)
    P.op("sp", L("dma_start", out=rc[:, :], in_=rc_d[:, :]), writes=[rc.b()], chan=ld)
    ctab = P.sb([32, 512], F32)
    stab = P.sb([32, 512], F32)
    P.op("dve", L("memset", ctab[:, :], 1.0), writes=[ctab.b()])
    P.op("dve", L("memset", stab[:, :], 0.0), writes=[stab.b()])
    posi = P.sb([16, 512], I32)
    ang = P.sb([16, 512], F32)
    rr = P.sb([16, 512], F32)
    sn = P.sb([16, 512], F32)
    xt = [P.sb([128, D], F32) for _ in range(2)]
    hT = P.sb([128, 8, 512], BF16)
    pq = [P.ps([64, 512], F32) for _ in range(2)]
    ppq = [P.ps([32, 512], F32) for _ in range(2)]
    pm = [P.ps([128, 512], F32) for _ in range(2)]
    ptk = P.ps([128, 280], F32)
    t1 = [P.sb([32, 512], F32) for _ in range(2)]
    t2 = [P.sb([32, 512], F32) for _ in range(2)]
    ob = [P.sb([64, 512], BF16) for _ in range(2)]
    obr = [P.sb([64, 512], BF16) for _ in range(2)]
    om = [P.sb([128, 512], BF16) for _ in range(2)]
    cgs = [P.sb([128, 512], F32) for _ in range(2)]
    otk = [P.sb([128, 256], BF16) for _ in range(2)]
    ogt = [P.sb([128, 24], F32) for _ in range(2)]
    units = [(h, C_Q + 64 * h, qT_d, h) for h in range(8)] + \
            [(8 + g, C_KS + 64 * g, ksT_d, g) for g in range(2)] + \
            [(10 + g, C_KW + 64 * g, kwT_d, g) for g in range(2)]
    nm = 0
    nu_ = 0
    for blk in range(ntok // 512):
        b0 = blk * 512
        P.op("sp", L("dma_start", out=posi[:, :], in_=pos_d[0:1, b0:b0 + 512].partition_broadcast(16)),
             writes=[posi.b()], chan=ld)
        P.op("dve", L("tensor_copy", ang[:, :], posi[:, :]), reads=[posi.b()], writes=[ang.b()])
        P.op("dve", L("tensor_scalar", ang[:, :], ang[:, :], rc[:, 0:1], None, ALU.mult),
             reads=[ang.b(), rc.b()], writes=[ang.b()])
        for (tab, shift) in ((stab, 0.0), (ctab, 0.5 * math.pi)):
            P.op("dve", L("tensor_scalar", rr[:, :], ang[:, :], 1.0 / TWO_PI, shift / TWO_PI, ALU.mult, ALU.add),
                 reads=[ang.b()], writes=[rr.b()])
            P.op("dve", L("tensor_copy", posi[:, :], rr[:, :]), reads=[rr.b()], writes=[posi.b()])
            P.op("dve", L("tensor_copy", rr[:, :], posi[:, :]), reads=[posi.b()], writes=[rr.b()])
            P.op("dve", L("scalar_tensor_tensor", out=sn[:, :], in0=rr[:, :], scalar=-CW1, in1=ang[:, :],
                          op0=ALU.mult, op1=ALU.add), reads=[rr.b(), ang.b()], writes=[sn.b()])
            P.op("dve", L("scalar_tensor_tensor", out=sn[:, :], in0=rr[:, :], scalar=-CW2, in1=sn[:, :],
                          op0=ALU.mult, op1=ALU.add), reads=[rr.b(), sn.b()], writes=[sn.b()])
            if shift:
                P.op("dve", L("tensor_scalar", sn[:, :], sn[:, :], shift, None, ALU.add), reads=[sn.b()], writes=[sn.b()])
            P.op("dve", L("tensor_scalar", rr[:, :], sn[:, :], math.pi, -TWO_PI, ALU.is_gt, ALU.mult),
                 reads=[sn.b()], writes=[rr.b()])
            P.op("dve", L("tensor_tensor", out=sn[:, :], in0=sn[:, :], in1=rr[:, :], op=ALU.add),
                 reads=[sn.b(), rr.b()], writes=[sn.b()])
            P.op("dve", L("tensor_scalar", sn[:, :], sn[:, :], math.pi, -math.pi, ALU.min, ALU.max),
                 reads=[sn.b()], writes=[sn.b()])
            P.op("act", L("activation", out=tab[0:16, :], in_=sn[:, :], func=AF.Sin), reads=[sn.b()], writes=[tab.b()])
        for tt in range(4):
            r0 = b0 + tt * 128
            s = tt % 2
            P.op("sp", L("dma_start", out=xt[s][:, :], in_=x[r0:r0 + 128, :]), writes=[xt[s].b()], chan=ld)
            prenorm_T(P, C, xt[s][:, :], xt[s].b(), gpre, hT, hT.b(tt), tt * 128, st)
        hreads = [hT.b(tt) for tt in range(4)]
        if stage < 1:
            continue
        for (u, c0, dst, di) in units:
            s = nu_ % 2
            nu_ += 1
            for k in range(8):
                P.op("pe", L("matmul", pq[s][:, :], lhsT=w[:, k, c0:c0 + 64], rhs=hT[:, k, :], start=(k == 0), stop=(k == 7)),
                     reads=[w.b()] + hreads, writes=[pq[s].b()])
            for k in range(8):
                P.op("pe", L("matmul", ppq[s][:, :], lhsT=wP[:, k, u, :], rhs=hT[:, k, :], start=(k == 0), stop=(k == 7)),
                     reads=[wP.b()] + hreads, writes=[ppq[s].b()])
            P.op("dve", L("tensor_tensor", out=t1[s][:, :], in0=pq[s][0:32, :], in1=ctab[:, :], op=ALU.mult),
                 reads=[pq[s].b(), ctab.b()], writes=[t1[s].b()])
            P.op("dve", L("tensor_tensor", out=t2[s][:, :], in0=ppq[s][:, :], in1=stab[:, :], op=ALU.mult),
                 reads=[ppq[s].b(), stab.b()], writes=[t2[s].b()])
            P.op("dve", L("tensor_tensor", out=ob[s][0:32, :], in0=t1[s][:, :], in1=t2[s][:, :], op=ALU.add),
                 reads=[t1[s].b(), t2[s].b()], writes=[ob[s].b(0)])
            P.op("act", L("copy", out=ob[s][32:64, :], in_=pq[s][32:64, :]), reads=[pq[s].b()], writes=[ob[s].b(1)])
            if u < 8:
                P.op("act", L("copy", out=obr[s][:, :], in_=pq[s][:, :]), reads=[pq[s].b()], writes=[obr[s].b()])
                P.op("sp", L("dma_start", out=qrT_d[di, :, b0:b0 + 512], in_=obr[s][:, :]),
                     reads=[obr[s].b()], writes=[qrT_d.b((di, blk))], chan=stc)
            P.op("sp", L("dma_start", out=dst[di, :, b0:b0 + 512], in_=ob[s][:, :]),
                 reads=[ob[s].b(0), ob[s].b(1)], writes=[dst.b((di, blk))], chan=stc)
        if stage < 2:
            continue
        for (c0, dst, di) in ((C_KC, kcvc_d, 0), (C_VC, kcvc_d, 1)) + tuple((C_BG + 128 * c, bgT_d, c) for c in range(4)):
            s = nm % 2
            nm += 1
            for k in range(8):
                P.op("pe", L("matmul", pm[s][:, :], lhsT=w[:, k, c0:c0 + 128], rhs=hT[:, k, :], start=(k == 0), stop=(k == 7)),
                     reads=[w.b()] + hreads, writes=[pm[s].b()])
            P.op("act", L("copy", out=om[s][:, :], in_=pm[s][:, :]), reads=[pm[s].b()], writes=[om[s].b()])
            P.op("sp", L("dma_start", out=dst[di, :, b0:b0 + 512], in_=om[s][:, :]),
                 reads=[om[s].b()], writes=[dst.b((di, blk))], chan=stc)
        for c in range(4):
            s0 = nm % 2
            s1 = (nm + 1) % 2
            nm += 2
            for (s, c0) in ((s0, C_CG + 128 * c), (s1, C_XC + 128 * c)):
                for k in range(8):
                    P.op("pe", L("matmul", pm[s][:, :], lhsT=w[:, k, c0:c0 + 128], rhs=hT[:, k, :], start=(k == 0), stop=(k == 7)),
                         reads=[w.b()] + hreads, writes=[pm[s].b()])
            P.op("act", L("copy", out=cgs[c % 2][:, :], in_=pm[s0][:, :]), reads=[pm[s0].b()], writes=[cgs[c % 2].b()])
            P.op("dve", L("tensor_tensor", out=om[s0][:, :], in0=pm[s1][:, :], in1=cgs[c % 2][:, :], op=ALU.mult),
                 reads=[cgs[c % 2].b(), pm[s1].b()], writes=[om[s0].b()])
            P.op("sp", L("dma_start", out=uT_d[c, :, b0:b0 + 512], in_=om[s0][:, :]),
                 reads=[om[s0].b()], writes=[uT_d.b((c, blk))], chan=stc)
        if stage < 3:
            continue
        for tt in range(4):
            s = tt % 2
            r0 = b0 + tt * 128
            for k in range(8):
                P.op("pe", L("matmul", ptk[:, :], lhsT=hT[:, k, tt * 128:(tt + 1) * 128], rhs=wt[:, k, :],
                             start=(k == 0), stop=(k == 7)),
                     reads=[wt.b(), hT.b(tt)], writes=[ptk.b()])
            P.op("act", L("copy", out=otk[s][:, :], in_=ptk[:, 0:256]), reads=[ptk.b()], writes=[otk[s].b()])
            P.op("act", L("copy", out=ogt[s][:, :], in_=ptk[:, 256:280]), reads=[ptk.b()], writes=[ogt[s].b()])
            P.op("sp", L("dma_start", out=vsw_d[r0:r0 + 128, :], in_=otk[s][:, :]),
                 reads=[otk[s].b()], writes=[vsw_d.b(r0)], chan=stc)
            P.op("sp", L("dma_start", out=gates_d[r0:r0 + 128, :], in_=ogt[s][:, :]),
                 reads=[ogt[s].b()], writes=[gates_d.b(r0)], chan=stc)
    P.finish()
    return nc


def rope_consts():
    half = 8
    invf = (np.float32(500000.0) ** (-np.arange(half, dtype=np.float32) * np.float32(2.0) / np.float32(16.0))).astype(np.float32)
    rc = np.zeros((16, 4), np.float32)
    rc[:, 0] = np.concatenate([invf, invf])
    rc[:, 1] = -math.pi
    return rc

SCALE = 0.125
LOOKAHEAD = 2
NEGB = -30000.0
GELU_C = 0.7978845608028654


def build_attn(jlist=None, ntok=NTOK, debug=False):
    if jlist is None:
        jlist = list(range(ntok // 128))
    nq = ntok // 128
    nc = bass.Bass("TRN2", target_bir_lowering=False)
    P = Prog(nc)
    EI = "ExternalInput"
    qT_d = P.dram("qT", [8, 64, ntok], BF16, kind=EI)
    qrT_d = P.dram("qrT", [8, 64, ntok], BF16, kind=EI)
    ksT_d = P.dram("ksT", [2, 64, SEQ], BF16, kind=EI)
    vs_d = P.dram("vs", [SEQ, 128], BF16, kind=EI)
    kwin_d = P.dram("kwin", [nq, 2, 64, 640], BF16, kind=EI)
    vwin_d = P.dram("vwin", [nq, 640, 128], BF16, kind=EI)
    kc2_d = P.dram("kc2", [2, 2, 128, 8192], BF16, kind=EI)
    gates_d = P.dram("gates", [ntok, 24], F32, kind=EI)
    bgT_d = P.dram("bgT", [4, 128, ntok], BF16, kind=EI)
    uT_d = P.dram("uT", [4, 128, ntok], BF16, kind=EI)
    uhalo_d = P.dram("uhalo", [nq, 4, 128, 2], BF16, kind=EI)
    x1_d = P.dram("x1", [ntok, D], F32, kind=EI)
    pe2_d = P.dram("pe2", [2, 128, 16], F32, kind=EI)
    w1_d = P.dram("w1", [2, 2048, 256], F32, kind=EI)
    w2_d = P.dram("w2", [2, 256, 64], F32, kind=EI)
    convw_d = P.dram("convw", [128, 12], F32, kind=EI)
    ga_d = P.dram("ga", [1, 512], F32, kind=EI)
    gc_d = P.dram("gc", [128, 4], F32, kind=EI)
    wout_d = P.dram("w_out", [D, D], F32, kind=EI)
    gpost_d = P.dram("g_post", [1, D], F32, kind=EI)
    id_d = P.dram("ident", [128, 128], F32, kind=EI)
    onehot_d = P.dram("onehot", [64, SEQ], BF16, kind=EI)
    amat_d = P.dram("amat", [1024, 256], BF16, kind=EI)
    masks_d = P.dram("masks", [128, 16, 128], BF16, kind=EI)
    fpat_d = P.dram("fpat", [128, 768], BF16, kind=EI)
    x2_d = P.dram("x2", [ntok, D], F32, kind="ExternalOutput")
    if debug:
        dbg_o = P.dram("dbg_o", [ntok, 3, 512], F32, kind="ExternalOutput")
        dbg_psl = P.dram("dbg_psl", [ntok, 2, 256], F32, kind="ExternalOutput")
        dbg_bias = P.dram("dbg_bias", [ntok, 2, 256], BF16, kind="ExternalOutput")
    ld = P.dma_chan("ld")
    wl = P.dma_chan("wl")
    stc = P.dma_chan("st")

    C = make_consts(P, id_d, ld)
    idb, idf = C["idb"], C["idf"]
    Kaug = P.sb([128, 2, SEQ], BF16)
    arena = P.sb([128, 16640], BF16)
    Vaug = arena[:, :].rearrange("p (t g e) -> p t g e", t=128, g=2, e=65)
    X2 = arena[:, 0:8192]
    w1b = arena[:, 8192:12288].rearrange("p (j c) -> p j c", j=16)
    hg = arena[:, 12288:13312].rearrange("p (c n) -> p c n", c=2)
    b_x2, b_w1, b_hg = arena.b("x2"), arena.b("w1"), arena.b("hg")
    wout = P.sb([128, 8, D], BF16)
    kcmpT = P.sb([64, 2, 1024], BF16)
    AV = P.sb([128, 8, 2, 321], BF16)
    masks = P.sb([128, 16, 128], BF16)
    fpat = P.sb([128, 768], BF16)
    gpost = P.sb([128, D], F32)
    ga = P.sb([128, 512], F32)
    gc = P.sb([128, 4], F32)
    cw = P.sb([128, 12], F32)
    ones_f = P.sb([128, 1], F32)
    QTs = [P.sb([64, 8, 128], BF16) for _ in range(2)]
    QRs = [P.sb([64, 8, 128], BF16) for _ in range(2)]
    Qaug = P.sb([128, 4, 2, 512], BF16)
    kwts = [P.sb([64, 2, 640], BF16) for _ in range(2)]
    vwas = [P.sb([128, 5, 2, 65], BF16) for _ in range(2)]
    gts = [P.sb([128, 24], F32) for _ in range(2)]
    sgt = P.sb([128, 24], F32)
    uts = [P.sb([128, 4, 130], BF16) for _ in range(2)]
    bgts = [P.sb([128, 4, 128], BF16) for _ in range(2)]
    cvy = P.sb([128, 128], F32)
    cvt = P.sb([128, 128], F32)
    cvq = P.sb([128, 4, 128], F32)
    x1t = P.sb([128, D], F32)
    EP = [P.sb([128, 512], BF16) for _ in range(4)]
    osb = [P.sb([65, 512], F32) for _ in range(2)]
    psl = P.sb([128, 256], F32)
    score = P.sb([128, 256], F32)
    swk = P.sb([128, 256], F32)
    m8 = P.sb([128, 16], F32)
    biaspad = P.sb([128, 320], BF16)
    oacc = P.sb([128, 512], F32)
    attb = P.sb([128, 512], BF16)
    mT = P.sb([128, 8, 128], BF16)
    hA = P.sb([128, D], F32)
    sq = P.sb([128, D], F32)
    sm = P.sb([128, 16], F32)
    b_sm = [sm.b(i) for i in range(16)]
    gx = P.sb([128, 512], F32)
    gtmp = P.sb([128, 512], F32)
    cb = P.sb([128, 2], F32)
    pe2f = P.sb([128, 16], F32)
    pe2b = P.sb([128, 16], BF16)
    w2b = P.sb([128, 2, 64], BF16)
    S = [P.ps([128, 512], F32) for _ in range(4)]
    G = [P.ps([128, 512], F32) for _ in range(3)]
    GB = P.ps([128, 1024], BF16)
    print("sbuf left", nc.sbuf_bytes_remaining, flush=True)

    def sp_load(dst_ap, src_ap, bufs):
        P.op("sp", L("dma_start", out=dst_ap, in_=src_ap), writes=bufs, chan=ld)

    sp_load(masks[:, :, :], masks_d[:, :, :], [masks.b()])
    sp_load(fpat[:, :], fpat_d[:, :], [fpat.b()])
    bcast_load(P, gpost, gpost_d, D, ld)
    bcast_load(P, ga, ga_d, 512, ld)
    sp_load(gc[:, :], gc_d[:, :], [gc.b()])
    sp_load(cw[:, :], convw_d[:, :], [cw.b()])
    P.op("dve", L("memset", ones_f[:, :], 1.0), writes=[ones_f.b()])
    for g in range(2):
        sp_load(AV[:, :, g, 0:256], amat_d[:, :].rearrange("(ct p) j -> p ct j", p=128), [AV.b(("a", g))])
    P.op("dve", L("memset", AV[:, :, :, 320:321], 1.0), writes=[AV.b("one")])
    for vwa in vwas:
        P.op("dve", L("memset", vwa[:, :, :, 64:65], 1.0), writes=[vwa.b("one")])
    P.op("dve", L("memset", kcmpT[:, :, :], 0.0), writes=[kcmpT.b(0), kcmpT.b(1)])
    P.op("dve", L("memset", hg, 0.0), writes=[b_hg])
    P.op("dve", L("memset", biaspad[:, 0:64], 0.0), writes=[biaspad.b("pad")])
    for k in range(8):
        P.op("pool", L("dma_start", out=wout[:, k, :], in_=wout_d[k * 128:(k + 1) * 128, :]), writes=[wout.b(k)], chan=wl)
    for g in range(2):
        sp_load(Kaug[0:64, g, :], ksT_d[g, :, :], [Kaug.b(("k", g))])
        sp_load(Kaug[64:128, g, :], onehot_d[:, :], [Kaug.b(("o", g))])

    for kv in range(2):
        P.op("pool", L("dma_start", out=w1b, in_=w1_d[kv, :, :].rearrange("(j p) c -> p j c", p=128)), writes=[b_w1], chan=wl)
        P.op("pool", L("dma_start", out=w2b[:, :, :], in_=w2_d[kv, :, :].rearrange("(c p) d -> p c d", p=128)),
             writes=[w2b.b()], chan=wl)
        sp_load(pe2f[:, :], pe2_d[kv, :, :], [pe2f.b()])
        P.op("dve", L("tensor_copy", pe2b[:, :], pe2f[:, :]), reads=[pe2f.b()], writes=[pe2b.b()])
        for c2 in range(2):
            for jj in range(16):
                P.op("pe", L("matmul", G[0][:, c2:c2 + 1], lhsT=w1b[:, jj, c2 * 128:(c2 + 1) * 128], rhs=pe2b[:, jj:jj + 1],
                             start=(jj == 0), stop=(jj == 15)), reads=[b_w1, pe2b.b()], writes=[G[0].b()])
        P.op("act", L("copy", out=cb[:, :], in_=G[0][:, 0:2]), reads=[G[0].b()], writes=[cb.b()])
        for g in range(2):
            sp_load(X2, kc2_d[kv, g, :, :], [b_x2])
            for nt in range(2):
                n0 = 512 * nt
                N = 512 if nt == 0 else 511
                for c2 in range(2):
                    Sx = S[c2]
                    for jj in range(16):
                        lo = 8 * n0 + jj
                        P.op("pe", L("matmul", Sx[:, 0:N], lhsT=w1b[:, jj, c2 * 128:(c2 + 1) * 128],
                                     rhs=X2[:, lo:lo + 8 * (N - 1) + 1:8], start=(jj == 0), stop=(jj == 15)),
                             reads=[b_w1, b_x2], writes=[Sx.b()])
                    P.op("act", L("activation", out=gx[:, 0:N], in_=Sx[:, 0:N], func=AF.Identity, bias=cb[:, c2:c2 + 1]),
                         reads=[Sx.b(), cb.b()], writes=[gx.b()])
                    P.op("dve", L("tensor_tensor", out=gtmp[:, 0:N], in0=gx[:, 0:N], in1=gx[:, 0:N], op=ALU.mult),
                         reads=[gx.b()], writes=[gtmp.b()])
                    P.op("dve", L("tensor_scalar", gtmp[:, 0:N], gtmp[:, 0:N], 0.044715, 1.0, ALU.mult, ALU.add),
                         reads=[gtmp.b()], writes=[gtmp.b()])
                    P.op("dve", L("tensor_tensor", out=gtmp[:, 0:N], in0=gtmp[:, 0:N], in1=gx[:, 0:N], op=ALU.mult),
                         reads=[gtmp.b(), gx.b()], writes=[gtmp.b()])
                    P.op("act", L("activation", out=gtmp[:, 0:N], in_=gtmp[:, 0:N], func=AF.Tanh, scale=GELU_C),
                         reads=[gtmp.b()], writes=[gtmp.b()])
                    P.op("dve", L("tensor_scalar", gtmp[:, 0:N], gtmp[:, 0:N], 0.5, 0.5, ALU.mult, ALU.add),
                         reads=[gtmp.b()], writes=[gtmp.b()])
                    P.op("dve", L("tensor_tensor", out=hg[:, c2, 0:N], in0=gtmp[:, 0:N], in1=gx[:, 0:N], op=ALU.mult),
                         reads=[gtmp.b(), gx.b()], writes=[b_hg])
                if kv == 0:
                    for c2 in range(2):
                        P.op("pe", L("matmul", S[2][0:64, 0:N], lhsT=w2b[:, c2, :], rhs=hg[:, c2, 0:N],
                                     start=(c2 == 0), stop=(c2 == 1)), reads=[w2b.b(), b_hg], writes=[S[2].b()])
                    P.op("act", L("copy", out=kcmpT[:, g, n0:n0 + N], in_=S[2][0:64, 0:N]),
                         reads=[S[2].b()], writes=[kcmpT.b(g)])
                else:
                    for t4 in range(4):
                        ct = nt * 4 + t4
                        for c2 in range(2):
                            P.op("pe", L("matmul", S[2][:, t4 * 64:(t4 + 1) * 64], lhsT=hg[:, c2, t4 * 128:(t4 + 1) * 128],
                                         rhs=w2b[:, c2, :], start=(c2 == 0), stop=(c2 == 1)),
                                 reads=[w2b.b(), b_hg], writes=[S[2].b()])
                    P.op("act", L("copy", out=AV[:, nt * 4:nt * 4 + 4, g, 256:320],
                                  in_=S[2][:, 0:256].rearrange("p (t d) -> p t d", t=4)),
                         reads=[S[2].b()], writes=[AV.b(("v", g, nt))])
    av_reads = {g: [AV.b(("a", g)), AV.b("one"), AV.b(("v", g, 0)), AV.b(("v", g, 1))] for g in range(2)}

    for c in range(8):
        for g in range(2):
            sp_load(Vaug[:, c * 16:(c + 1) * 16, g, 0:64],
                    vs_d[c * 2048:(c + 1) * 2048, g * 64:(g + 1) * 64].rearrange("(t p) d -> p t d", p=128),
                    [arena.b(("v", c, g)), b_x2, b_w1, b_hg])
    P.op("dve", L("memset", Vaug[:, :, :, 64:65], 1.0), writes=[arena.b("vone"), b_x2, b_w1, b_hg])

    def vreads(kt, g):
        return [arena.b(("v", kt // 16, g)), arena.b("vone")]

    def mask_rhs(i):
        return masks[:, i:i + 1, :].to_broadcast([128, 4, 128])

    def branch_epilogue(g, gate_idx, oT, Tt):
        first = False
        P.op("act", L("copy", out=osb[g][:, :], in_=oT[0:65, :]), reads=[oT.b()], writes=[osb[g].b()])
        T = Tt[:, 0:260].rearrange("p (h e) -> p h e", h=4)
        for h in range(4):
            P.op("pe", L("transpose", T[:, h, :], osb[g][:, h * 128:(h + 1) * 128], idf[0:65, 0:65]),
                 reads=[osb[g].b(), idf.b()], writes=[Tt.b()])
        rd = sm[:, 0:4]
        P.op("dve", L("tensor_scalar", rd, T[:, :, 64], 1e-30, None, ALU.max), reads=[Tt.b()], writes=[b_sm[0]])
        P.op("dve", L("reciprocal", rd, rd), reads=[b_sm[0]], writes=[b_sm[0]])
        sg3 = sgt[:, :].rearrange("p (h b) -> p h b", b=3)
        P.op("dve", L("tensor_tensor", out=rd, in0=rd, in1=sg3[:, 4 * g:4 * g + 4, gate_idx], op=ALU.mult),
             reads=[b_sm[0], sgt.b()], writes=[b_sm[0]])
        for h in range(4):
            c0 = (4 * g + h) * 64
            if first:
                P.op("dve", L("tensor_scalar", oacc[:, c0:c0 + 64], T[:, h, 0:64], sm[:, h:h + 1], None, ALU.mult),
                     reads=[Tt.b(), b_sm[0]], writes=[oacc.b(g)])
            else:
                P.op("dve", L("scalar_tensor_tensor", out=oacc[:, c0:c0 + 64], in0=T[:, h, 0:64], scalar=sm[:, h:h + 1],
                              in1=oacc[:, c0:c0 + 64], op0=ALU.mult, op1=ALU.add),
                     reads=[Tt.b(), b_sm[0], oacc.b(g)], writes=[oacc.b(g)])

    cnt = {"s": 0}

    def score_tile(lhsT, lhs_reads, rhs, rhs_reads, mask_i, nslots=4):
        si = cnt["s"] % nslots
        i = cnt["s"] % 4
        cnt["s"] += 1
        P.op("pe", L("matmul", S[si][:, :], lhsT=lhsT, rhs=rhs, start=True, stop=(mask_i is None)),
             reads=lhs_reads + rhs_reads, writes=[S[si].b()])
        if mask_i is not None:
            P.op("pe", L("matmul", S[si][:, :], lhsT=idb[:, :], rhs=mask_rhs(mask_i), start=False, stop=True),
                 reads=[idb.b(), masks.b()], writes=[S[si].b()])
        P.op("act", L("activation", out=EP[i][:, :], in_=S[si][:, :], func=AF.Exp, scale=SCALE),
             reads=[S[si].b()], writes=[EP[i].b()])
        return i

    for jidx, j in enumerate(jlist):
        t0 = j * 128
        QT, QR, kwt, vwa, gt, ut, bgt = (t_[jidx % 2] for t_ in (QTs, QRs, kwts, vwas, gts, uts, bgts))
        sp_load(QT[:, :, :], qT_d[:, :, t0:t0 + 128].rearrange("h d q -> d h q"), [QT.b()])
        sp_load(QR[:, :, :], qrT_d[:, :, t0:t0 + 128].rearrange("h d q -> d h q"), [QR.b()])
        sp_load(kwt[:, :, :], kwin_d[j, :, :, :].rearrange("g d k -> d g k"), [kwt.b()])
        for g in range(2):
            sp_load(vwa[:, :, g, 0:64], vwin_d[j, :, g * 64:(g + 1) * 64].rearrange("(r p) d -> p r d", p=128), [vwa.b(("v", g))])
        sp_load(gt[:, :], gates_d[t0:t0 + 128, :], [gt.b()])
        sp_load(ut[:, :, 2:130], uT_d[:, :, t0:t0 + 128].rearrange("c p q -> p c q"), [ut.b("m")])
        sp_load(ut[:, :, 0:2], uhalo_d[j, :, :, :].rearrange("c p k -> p c k"), [ut.b("h")])
        sp_load(bgt[:, :, :], bgT_d[:, :, t0:t0 + 128].rearrange("c p q -> p c q"), [bgt.b()])
        sp_load(x1t[:, :], x1_d[t0:t0 + 128, :], [x1t.b()])
        P.op("act", L("activation", out=sgt[:, :], in_=gt[:, :], func=AF.Exp, scale=-1.0), reads=[gt.b()], writes=[sgt.b()])
        P.op("dve", L("tensor_scalar", sgt[:, :], sgt[:, :], 1.0, None, ALU.add), reads=[sgt.b()], writes=[sgt.b()])
        P.op("dve", L("reciprocal", sgt[:, :], sgt[:, :]), reads=[sgt.b()], writes=[sgt.b()])
        for g in range(2):
            for w in range((4 * j + 3) // 32 + 1):
                P.op("pool", L("tensor_copy", Qaug[0:64, w, g, :].rearrange("p (h q) -> p h q", h=4), QT[:, 4 * g:4 * g + 4, :]),
                     reads=[QT.b()], writes=[Qaug.b((w, g, 0))])
        for c in range(4):
            P.op("pool", L("tensor_scalar", cvy[:, :], ut[:, c, 0:128], cw[:, 3 * c:3 * c + 1], None, ALU.mult),
                 reads=[ut.b("m"), ut.b("h"), cw.b()], writes=[cvy.b()])
            for k in (1, 2):
                P.op("pool", L("tensor_scalar", cvt[:, :], ut[:, c, k:k + 128], cw[:, 3 * c + k:3 * c + k + 1], None, ALU.mult),
                     reads=[ut.b("m"), ut.b("h"), cw.b()], writes=[cvt.b()])
                P.op("pool", L("tensor_tensor", out=cvy[:, :], in0=cvy[:, :], in1=cvt[:, :], op=ALU.add),
                     reads=[cvt.b(), cvy.b()], writes=[cvy.b()])
            P.op("pool", L("tensor_tensor", out=cvy[:, :], in0=cvy[:, :], in1=bgt[:, c, :], op=ALU.mult),
                 reads=[cvy.b(), bgt.b()], writes=[cvy.b()])
            P.op("pool", L("tensor_scalar", mT[:, 4 + c, :], cvy[:, :], gc[:, c:c + 1], None, ALU.mult),
                 reads=[cvy.b(), gc.b()], writes=[mT.b(4 + c)])
            P.op("pool", L("tensor_tensor", out=cvq[:, c, :], in0=cvy[:, :], in1=cvy[:, :], op=ALU.mult),
                 reads=[cvy.b()], writes=[cvq.b(c)])
        sg3 = sgt[:, :].rearrange("p (h b) -> p h b", b=3)
        nct = (32 * j + 30) // 128 + 1
        nkt = 4 * j + 4
        nW = (nkt - 1) // 32 + 1
        def front(g, part):
            qrhs = QT[:, 4 * g:4 * g + 4, :]
            pc = [S[2], S[3], G[0], G[1]]
            if part == "A":
                cslot = {}

                def cmp_qk(ct):
                    rp = 4 * j - 16 * ct
                    i = cnt["s"] % 2
                    cnt["s"] += 1
                    msk = rp // 4 if rp <= 16 else None
                    P.op("pe", L("matmul", S[i][:, :], lhsT=kcmpT[:, g, ct * 128:(ct + 1) * 128], rhs=QR[:, 4 * g:4 * g + 4, :],
                                 start=True, stop=(msk is None)), reads=[kcmpT.b(g), QR.b()], writes=[S[i].b()])
                    if msk is not None:
                        P.op("pe", L("matmul", S[i][:, :], lhsT=idb[:, :], rhs=mask_rhs(msk), start=False, stop=True),
                             reads=[idb.b(), masks.b()], writes=[S[i].b()])
                    P.op("act", L("activation", out=EP[i][:, :], in_=S[i][:, :], func=AF.Exp, scale=SCALE),
                         reads=[S[i].b()], writes=[EP[i].b()])
                    cslot[ct] = i

                cmp_qk(0)
                for ct in range(nct):
                    if ct + 1 < nct:
                        cmp_qk(ct + 1)
                    i = cslot[ct]
                    for h in range(4):
                        P.op("pe", L("matmul", pc[h][:, 0:321], lhsT=EP[i][:, h * 128:(h + 1) * 128], rhs=AV[:, ct, g, :],
                                     start=(ct == 0), stop=(ct == nct - 1)),
                             reads=[EP[i].b()] + av_reads[g], writes=[pc[h].b()])
            if part == "B":
                rd = sm[:, 4:8]
                for h in range(4):
                    P.op("dve", L("tensor_scalar", sm[:, 4 + h:5 + h], pc[h][:, 320:321], 1e-30, None, ALU.max),
                         reads=[pc[h].b()], writes=[b_sm[1]])
                P.op("dve", L("reciprocal", rd, rd), reads=[b_sm[1]], writes=[b_sm[1]])
                P.op("dve", L("tensor_scalar", psl[:, :], pc[0][:, 0:256], sm[:, 4:5], None, ALU.mult),
                     reads=[pc[0].b(), b_sm[1]], writes=[psl.b()])
                for h in range(1, 4):
                    P.op("dve", L("scalar_tensor_tensor", out=psl[:, :], in0=pc[h][:, 0:256], scalar=sm[:, 4 + h:5 + h],
                                  in1=psl[:, :], op0=ALU.mult, op1=ALU.add),
                         reads=[pc[h].b(), b_sm[1], psl.b()], writes=[psl.b()])
                wc = sm[:, 8:12]
                P.op("dve", L("tensor_tensor", out=wc, in0=rd, in1=sg3[:, 4 * g:4 * g + 4, 0], op=ALU.mult),
                     reads=[b_sm[1], sgt.b()], writes=[b_sm[2]])
                for h in range(4):
                    c0 = (4 * g + h) * 64
                    P.op("dve", L("tensor_scalar", oacc[:, c0:c0 + 64], pc[h][:, 256:320], sm[:, 8 + h:9 + h], None, ALU.mult),
                         reads=[pc[h].b(), b_sm[2]], writes=[oacc.b(g)])
                P.op("dve", L("tensor_tensor", out=score[:, :], in0=psl[:, :], in1=fpat[:, 256 - 8 * j:512 - 8 * j], op=ALU.add),
                     reads=[psl.b(), fpat.b()], writes=[score.b()])
                P.op("dve", L("memset", score[:, 0:1], 1e9), reads=[], writes=[score.b()])
                P.op("dve", L("max", out=m8[:, 0:8], in_=score[:, :]), reads=[score.b()], writes=[m8.b(0)])
                P.op("dve", L("match_replace", out=swk[:, :], in_to_replace=m8[:, 0:8], in_values=score[:, :], imm_value=-3e9),
                     reads=[score.b(), m8.b(0)], writes=[swk.b()])
                P.op("dve", L("max", out=m8[:, 8:16], in_=swk[:, :]), reads=[swk.b()], writes=[m8.b(1)])
                P.op("dve", L("tensor_reduce", out=sm[:, 15:16], in_=m8[:, 8:16], axis=AX.X, op=ALU.min),
                     reads=[m8.b(1)], writes=[b_sm[6]])
                P.op("dve", L("tensor_scalar", swk[:, :], score[:, :], sm[:, 15:16], None, ALU.is_lt),
                     reads=[score.b(), b_sm[6]], writes=[swk.b()])
                P.op("dve", L("tensor_scalar", biaspad[:, 64:320], swk[:, :], NEGB, None, ALU.mult),
                     reads=[swk.b()], writes=[biaspad.b("b")])
                if debug:
                    P.op("sp", L("dma_start", out=dbg_psl[t0:t0 + 128, g, :], in_=psl[:, :]), reads=[psl.b()],
                         writes=[dbg_psl.b((t0, g))], chan=stc)
                    P.op("sp", L("dma_start", out=dbg_bias[t0:t0 + 128, g, :], in_=biaspad[:, 64:320]), reads=[biaspad.b("b")],
                         writes=[dbg_bias.b((t0, g))], chan=stc)
            if part == "C":
                for w in range(nW):
                    P.op("pe", L("transpose", GB[:, w * 128:(w + 1) * 128], biaspad[:, 64 * w:64 * w + 128], idb[:, :]),
                         reads=[biaspad.b("b"), biaspad.b("pad"), idb.b()], writes=[GB.b()])
                for w in range(nW):
                    P.op("act", L("copy", out=Qaug[64:128, w, g, :].rearrange("p (h q) -> p h q", h=4),
                                  in_=GB[64:128, w * 128:(w + 1) * 128].unsqueeze(1).to_broadcast([64, 4, 128])),
                         reads=[GB.b()], writes=[Qaug.b((w, g, 1))])
        if debug:
            P.op("sp", L("dma_start", out=dbg_o[t0:t0 + 128, 0, :], in_=oacc[:, :]), reads=[oacc.b(0), oacc.b(1)],
                 writes=[dbg_o.b((t0, 0))], chan=stc)
        def sel_loop(g, oT, nslots):
            slot = {}
            for n in range(nkt + LOOKAHEAD):
                if n < nkt:
                    kt = n
                    w = kt // 32
                    slot[n] = score_tile(Kaug[:, g, kt * 128:(kt + 1) * 128], [Kaug.b(("k", g)), Kaug.b(("o", g))],
                                         Qaug[:, w, g, :], [Qaug.b((w, g, 0)), Qaug.b((w, g, 1))],
                                         (5 + kt - 4 * j) if kt >= 4 * j else None, nslots)
                m = n - LOOKAHEAD
                if m >= 0:
                    kt = m
                    i = slot[m]
                    P.op("pe", L("matmul", oT[0:65, :], lhsT=Vaug[:, kt, g, :], rhs=EP[i][:, :],
                                 start=(kt == 0), stop=(kt == nkt - 1)), reads=[EP[i].b()] + vreads(kt, g), writes=[oT.b()])

        front(0, "A")
        front(0, "B")
        front(1, "A")
        front(0, "C")
        sel_loop(0, G[2], 2)
        front(1, "B")
        front(1, "C")
        branch_epilogue(0, 1, G[2], G[0])
        sel_loop(1, G[1], 4)
        branch_epilogue(1, 1, G[1], G[2])
        if debug:
            P.op("sp", L("dma_start", out=dbg_o[t0:t0 + 128, 1, :], in_=oacc[:, :]), reads=[oacc.b(0), oacc.b(1)],
                 writes=[dbg_o.b((t0, 1))], chan=stc)
        tiles = [(r, g) for r in range(5) for g in range(2)]
        slot = {}
        for n in range(len(tiles) + LOOKAHEAD):
            if n < len(tiles):
                r, g = tiles[n]
                if j == 0:
                    mi = 9 + r
                else:
                    mi = 14 if r == 0 else (15 if r == 4 else None)
                slot[n] = score_tile(kwt[:, g, r * 128:(r + 1) * 128], [kwt.b()], QT[:, 4 * g:4 * g + 4, :], [QT.b()], mi)
            m = n - LOOKAHEAD
            if m >= 0:
                r, g = tiles[m]
                i = slot[m]
                P.op("pe", L("matmul", G[g][0:65, :], lhsT=vwa[:, r, g, :], rhs=EP[i][:, :],
                             start=(r == 0), stop=(r == 4)), reads=[EP[i].b(), vwa.b(("v", g)), vwa.b("one")], writes=[G[g].b()])
        for g in range(2):
            branch_epilogue(g, 2, G[g], G[2])
        if debug:
            P.op("sp", L("dma_start", out=dbg_o[t0:t0 + 128, 2, :], in_=oacc[:, :]), reads=[oacc.b(0), oacc.b(1)],
                 writes=[dbg_o.b((t0, 2))], chan=stc)
        for c in range(4):
            P.op("pe", L("matmul", G[2][:, 300:301], lhsT=cvq[:, c, :], rhs=ones_f[:, :], start=(c == 0), stop=(c == 3)),
                 reads=[cvq.b(c), ones_f.b()], writes=[G[2].b()])
        P.op("act", L("copy", out=sm[:, 13:14], in_=G[2][:, 300:301]), reads=[G[2].b()], writes=[b_sm[4]])
        P.op("act", L("activation", out=sq[:, 0:512], in_=oacc[:, :], func=AF.Square, accum_out=sm[:, 12:13]),
             reads=[oacc.b(0), oacc.b(1)], writes=[sq.b(), b_sm[4]])
        P.op("act", L("activation", out=sm[:, 12:14], in_=sm[:, 12:14], func=AF.Sqrt, bias=C["eps1"][:, 0:1], scale=1.0 / 512),
             reads=[b_sm[4], C["eps1"].b()], writes=[b_sm[4]])
        P.op("dve", L("reciprocal", sm[:, 12:14], sm[:, 12:14]), reads=[b_sm[4]], writes=[b_sm[4]])
        P.op("dve", L("scalar_tensor_tensor", out=attb[:, :], in0=oacc[:, :], scalar=sm[:, 12:13], in1=ga[:, :],
                      op0=ALU.mult, op1=ALU.mult), reads=[oacc.b(0), oacc.b(1), b_sm[4], ga.b()], writes=[attb.b()])
        for k in range(4):
            P.op("pe", L("transpose", GB[:, k * 128:(k + 1) * 128], attb[:, k * 128:(k + 1) * 128], idb[:, :]),
                 reads=[attb.b(), idb.b()], writes=[GB.b()])
        P.op("act", L("copy", out=mT[:, 0:4, :], in_=GB[:, 0:512].rearrange("p (k q) -> p k q", k=4)),
             reads=[GB.b()], writes=[mT.b(k) for k in range(4)])
        for nh in range(2):
            for k in range(4):
                P.op("pe", L("matmul", S[nh][:, :], lhsT=mT[:, k, :], rhs=wout[:, k, nh * 512:(nh + 1) * 512],
                             start=(k == 0), stop=(k == 3)), reads=[mT.b(k), wout.b(k)], writes=[S[nh].b()])
            for k in range(4, 8):
                P.op("pe", L("matmul", S[2 + nh][:, :], lhsT=mT[:, k, :], rhs=wout[:, k, nh * 512:(nh + 1) * 512],
                             start=(k == 4), stop=(k == 7)), reads=[mT.b(k), wout.b(k)], writes=[S[2 + nh].b()])
        for nh in range(2):
            P.op("act", L("copy", out=hA[:, nh * 512:(nh + 1) * 512], in_=S[nh][:, :]), reads=[S[nh].b()], writes=[hA.b(nh)])
            P.op("dve", L("scalar_tensor_tensor", out=hA[:, nh * 512:(nh + 1) * 512], in0=S[2 + nh][:, :], scalar=sm[:, 13:14],
                          in1=hA[:, nh * 512:(nh + 1) * 512], op0=ALU.mult, op1=ALU.add),
                 reads=[S[2 + nh].b(), b_sm[4], hA.b(nh)], writes=[hA.b(nh)])
        P.op("act", L("activation", out=sq[:, :], in_=hA[:, :], func=AF.Square, accum_out=sm[:, 14:15]),
             reads=[hA.b(0), hA.b(1)], writes=[sq.b(), b_sm[5]])
        P.op("act", L("activation", out=sm[:, 14:15], in_=sm[:, 14:15], func=AF.Sqrt, bias=C["eps1"][:, 0:1], scale=1.0 / D),
             reads=[b_sm[5], C["eps1"].b()], writes=[b_sm[5]])
        P.op("dve", L("reciprocal", sm[:, 14:15], sm[:, 14:15]), reads=[b_sm[5]], writes=[b_sm[5]])
        P.op("dve", L("scalar_tensor_tensor", out=hA[:, :], in0=hA[:, :], scalar=sm[:, 14:15], in1=gpost[:, :],
                      op0=ALU.mult, op1=ALU.mult), reads=[hA.b(0), hA.b(1), b_sm[5], gpost.b()], writes=[hA.b(0), hA.b(1)])
        P.op("dve", L("tensor_tensor", out=hA[:, :], in0=hA[:, :], in1=x1t[:, :], op=ALU.add),
             reads=[hA.b(0), hA.b(1), x1t.b()], writes=[hA.b(0), hA.b(1)])
        P.op("sp", L("dma_start", out=x2_d[t0:t0 + 128, :], in_=hA[:, :]), reads=[hA.b(0), hA.b(1)],
             writes=[x2_d.b(t0)], chan=stc)
    P.finish()
    return nc

import ml_dtypes
NPBF = ml_dtypes.bfloat16
NQB = NTOK // 128


def own_tokens(arr_seq, i):
    a = arr_seq.reshape(NQB, 4, 128, *arr_seq.shape[1:])
    return np.ascontiguousarray(a[:, i].reshape(NTOK, *arr_seq.shape[1:]))


def scatter_tokens(shards):
    a = np.stack([s.reshape(NQB, 128, *s.shape[1:]) for s in shards], axis=1)
    return a.reshape(SEQ, *shards[0].shape[1:])


def full_T(shards):
    lead = shards[0].shape[:-1]
    a = np.stack([s.reshape(*lead, NQB, 128) for s in shards], axis=-2)
    return a.reshape(*lead, SEQ)


def attn_consts(i):
    kk = np.arange(128)[:, None]
    q = np.arange(128)[None, :]
    m = np.zeros((128, 16, 128), np.float32)
    for mi in range(5):
        valid = (16 * kk + 31 - q) <= 128 * (4 * mi + i)
        m[:, mi, :] = np.where(valid, 0.0, NEGB)
    for r in range(4):
        if r == i:
            m[:, 5 + r, :] = np.where(kk > q, NEGB, 0.0)
        elif r > i:
            m[:, 5 + r, :] = NEGB
    for r in range(5):
        diff = 512 - 128 * r + q - kk
        key = 128 * i - 512 + 128 * r + kk
        valid = (key >= 0) & (diff >= 0) & (diff < 512)
        m[:, 9 + r, :] = np.where(valid, 0.0, NEGB)
    m[:, 14, :] = np.where(kk > q, 0.0, NEGB)
    m[:, 15, :] = np.where(kk <= q, 0.0, NEGB)
    fp = np.zeros((128, 768), np.float32)
    for cp in range(768):
        c = cp - 2 * i
        if c < 0:
            continue
        for half, cur in ((slice(0, 64), 256), (slice(64, 128), 257)):
            if c == cur or c == cur - 1:
                fp[half, cp] = 1e9
            elif c > cur:
                fp[half, cp] = -1e9
    return m.astype(NPBF), fp.astype(NPBF)


def static_consts():
    key = np.arange(SEQ)
    onehot = (((key // 64) % 64)[None, :] == np.arange(64)[:, None]).astype(NPBF)
    agg = [1.0, 2.0, 2.0, 2.0, 1.0]
    A = np.zeros((1024, 256), np.float32)
    for jb in range(256):
        for o in range(5):
            n = 4 * jb + o - 1
            if 0 <= n < 1023:
                A[n, jb] = agg[o]
    return onehot, A.astype(NPBF)


def attn_in_maps(batch_outs, x1_shards, params):
    ksT = full_T([o["ksT"] for o in batch_outs])
    kwT = full_T([o["kwT"] for o in batch_outs])
    kcvc = full_T([o["kcvcT"] for o in batch_outs])
    uT = full_T([o["uT"] for o in batch_outs])
    vsw = scatter_tokens([o["vsw"] for o in batch_outs])
    vs = np.ascontiguousarray(vsw[:, :128])
    vw = vsw[:, 128:]
    kc2 = np.ascontiguousarray(kcvc.reshape(2, 2, 64, SEQ // 2, 2).transpose(0, 1, 4, 2, 3).reshape(2, 2, 128, SEQ // 2))
    kw_pad = np.concatenate([np.zeros((2, 64, 512), NPBF), kwT], axis=2)
    vw_pad = np.concatenate([np.zeros((512, 128), NPBF), vw], axis=0)
    u_pad = np.concatenate([np.zeros((4, 128, 2), NPBF), uT], axis=2)
    onehot, amat = static_consts()
    maps = []
    for i in range(4):
        s0 = (4 * np.arange(NQB) + i) * 128
        kwin = np.stack([kw_pad[:, :, s:s + 640] for s in s0])
        vwin = np.stack([vw_pad[s:s + 640] for s in s0])
        uhalo = np.stack([u_pad[:, :, s:s + 2] for s in s0])
        masks, fpat = attn_consts(i)
        o = batch_outs[i]
        m = {"qT": o["qT"], "qrT": o["qrT"], "ksT": ksT, "vs": vs, "kwin": np.ascontiguousarray(kwin), "vwin": np.ascontiguousarray(vwin),
             "kc2": kc2, "gates": o["gates"], "bgT": o["bgT"], "uT": o["uT"], "uhalo": np.ascontiguousarray(uhalo),
             "x1": x1_shards[i], "ident": _ident(), "onehot": onehot, "amat": amat, "masks": masks, "fpat": fpat}
        m.update(params)
        maps.append({k: np.ascontiguousarray(v) for k, v in m.items()})
    return maps


def attn_params(inp, l):
    pe2 = np.stack([inp[n][l].reshape(16, 2, 64).transpose(1, 2, 0).reshape(128, 16) for n in ("cmp_pe_k", "cmp_pe_v")])
    return {
        "pe2": np.ascontiguousarray(pe2), "w1": np.stack([inp["cmp_w1_k"][l], inp["cmp_w1_v"][l]]),
        "w2": np.stack([inp["cmp_w2_k"][l], inp["cmp_w2_v"][l]]),
        "convw": np.ascontiguousarray(inp["conv_w"][l].reshape(3, 4, 128).transpose(2, 1, 0).reshape(128, 12)),
        "ga": inp["attn_out_norm"][l].reshape(1, 512),
        "gc": np.ascontiguousarray(inp["conv_out_norm"][l].reshape(4, 128).T),
        "w_out": inp["w_out"][l], "g_post": inp["mix_norm_post"][l].reshape(1, D),
    }


def _ident():
    return np.eye(128, dtype=np.float32)


def _launch(nc, in_maps):
    res = run_bass_kernel_spmd(nc, in_maps, core_ids=list(range(NCORES)))
    return res.results


def _run_ffn(xs, inp, pref, l):
    nc = build_ffn()
    maps = [{"x": np.ascontiguousarray(x_), "g_pre": np.ascontiguousarray(inp[pref + "_norm_pre"][l].reshape(1, D)),
             "g_post": np.ascontiguousarray(inp[pref + "_norm_post"][l].reshape(1, D)),
             "w_gate": np.ascontiguousarray(inp[pref + "_w_gate"][l]), "w_up": np.ascontiguousarray(inp[pref + "_w_up"][l]),
             "w_down": np.ascontiguousarray(inp[pref + "_w_down"][l]), "ident": _ident()} for x_ in xs]
    return [r["y"] for r in _launch(nc, maps)]


def _run_inproj(xs, pos_shards, inp, l):
    nc = build_inproj()
    rc = rope_consts()
    maps = [{"x": np.ascontiguousarray(x_), "g_pre": np.ascontiguousarray(inp["mix_norm_pre"][l].reshape(1, D)),
             "w_in": np.ascontiguousarray(inp["w_in"][l]), "pos": p_, "ropec": rc, "ident": _ident()}
            for x_, p_ in zip(xs, pos_shards)]
    return _launch(nc, maps)


def _run_attn(outs, xs, inp, l):
    nc = build_attn()
    params = attn_params(inp, l)
    maps = []
    for b in range(2):
        maps += attn_in_maps(outs[4 * b:4 * b + 4], xs[4 * b:4 * b + 4], params)
    return [r["x2"] for r in _launch(nc, maps)]


def kernel(**inp):
    inp = {k: np.asarray(v) for k, v in inp.items()}
    x = inp["x"].astype(np.float32, copy=False)
    pos = inp["positions"].astype(np.int32, copy=False)
    xs = [own_tokens(x[c // 4], c % 4) for c in range(NCORES)]
    ps = [np.ascontiguousarray(own_tokens(pos[c // 4], c % 4).reshape(1, NTOK)) for c in range(NCORES)]
    for l in range(2):
        xs = _run_ffn(xs, inp, "ffn1", l)
        outs = _run_inproj(xs, ps, inp, l)
        xs = _run_attn(outs, xs, inp, l)
        xs = _run_ffn(xs, inp, "ffn2", l)
    out = np.stack([scatter_tokens(xs[4 * b:4 * b + 4]) for b in range(2)])
    return np.ascontiguousarray(out.astype(np.float32, copy=False))
```

```python
import math
import numpy as np
import concourse.bass as bass
import concourse.mybir as mybir
from concourse.bass_utils import run_bass_kernel_spmd

F32 = mybir.dt.float32
BF16 = mybir.dt.bfloat16
I32 = mybir.dt.int32
AF = mybir.ActivationFunctionType
ALU = mybir.AluOpType
AX = mybir.AxisListType

NCORES = 8
D = 1024
DFF = 2816
NFC = DFF // 128
SEQ = 16384
NTOK = 4096
TG = 1024
EPS = 1e-6


class Buf:
    __slots__ = ("w", "r", "excl")

    def __init__(self, excl=False):
        self.w = None
        self.r = {}
        self.excl = excl


class Tile:
    def __init__(self, handle, excl=False):
        self.h = handle
        self.bufs = {}
        self.excl = excl

    def b(self, key=None):
        bb = self.bufs.get(key)
        if bb is None:
            bb = self.bufs[key] = Buf(self.excl)
        return bb

    def __getitem__(self, k):
        return self.h[k]


EPOCH = 24000


class Prog:
    STREAMS = ("pe", "act", "dve", "pool", "sp")

    def __init__(self, nc):
        self.nc = nc
        self.streams = {s: [] for s in self.STREAMS}
        self.count = {}
        self.unit = {s: 1 for s in self.STREAMS}
        self.seen = {s: {} for s in self.STREAMS}
        self.sems = {}
        self.nt = 0
        self.ways = {}
        self.rr = {}

    def sb(self, shape, dt, name=None):
        self.nt += 1
        return Tile(self.nc.alloc_sbuf_tensor(name or f"t{self.nt}", list(shape), dt))

    def ps(self, shape, dt=F32, name=None):
        self.nt += 1
        return Tile(self.nc.alloc_psum_tensor(name or f"p{self.nt}", list(shape), dt), excl=True)

    def dram(self, name, shape, dt, kind="Internal"):
        return Tile(self.nc.dram_tensor(name, list(shape), dt, kind=kind).ap())

    def dma_chan(self, name, ways=4):
        self.ways[name] = ways
        self.rr[name] = 0
        for k in range(ways):
            self.unit[f"{name}#{k}"] = 16
        return name

    def op(self, stream, fn, reads=(), writes=(), chan=None):
        chan = chan or stream
        deps = {}
        if chan in self.ways:
            k = self.rr[chan] % self.ways[chan]
            self.rr[chan] += 1
            chan = f"{chan}#{k}"
            prev = self.count.get(chan, 0)
            if prev:
                deps[chan] = prev
        for b in reads:
            if b.w is not None:
                c, n = b.w
                if deps.get(c, 0) < n:
                    deps[c] = n
            if b.excl:
                for c, n in b.r.items():
                    if c != chan and deps.get(c, 0) < n:
                        deps[c] = n
        for b in writes:
            if b.w is not None:
                c, n = b.w
                if deps.get(c, 0) < n:
                    deps[c] = n
            for c, n in b.r.items():
                if deps.get(c, 0) < n:
                    deps[c] = n
        waits = []
        seen = self.seen[stream]
        for c, n in deps.items():
            if c == "pe" and stream == "pe" and chan == "pe":
                continue
            if seen.get(c, 0) >= n:
                continue
            seen[c] = n
            waits.append((c, n))
        idx = self.count.get(chan, 0) + 1
        self.count[chan] = idx
        self.streams[stream].append((fn, waits, chan, idx))
        for b in reads:
            if b.r.get(chan, 0) < idx:
                b.r[chan] = idx
        for b in writes:
            b.w = (chan, idx)
            b.r = {}
        return idx

    def _semval(self, chan, idx):
        unit = self.unit[chan]
        per = EPOCH // unit
        ep = (idx - 1) // per
        key = (chan, ep)
        sem = self.sems.get(key)
        if sem is None:
            sem = self.sems[key] = self.nc.alloc_semaphore(f"s_{chan}_{ep}")
        return sem, ((idx - 1) % per + 1) * unit

    def _replay(self, stream, eng):
        for fn, waits, chan, idx in self.streams[stream]:
            for c, n in waits:
                sem, val = self._semval(c, n)
                eng.wait_ge(sem, val)
            ins = fn(eng)
            sem, _ = self._semval(chan, idx)
            ins.then_inc(sem, self.unit[chan])

    def finish(self):
        print("prog sizes", {k: len(v) for k, v in self.streams.items()}, flush=True)
        st = self.streams["sp"]
        final_waits = [(c, n) for c, n in self.count.items()]
        nc = self.nc
        with nc.Block() as block:
            @block.tensor
            def _(e):
                self._replay("pe", e)

            @block.scalar
            def _(e):
                self._replay("act", e)

            @block.vector
            def _(e):
                self._replay("dve", e)

            @block.gpsimd
            def _(e):
                self._replay("pool", e)

            @block.sync
            def _(e):
                self._replay("sp", e)
                for c, n in final_waits:
                    sem, val = self._semval(c, n)
                    e.wait_ge(sem, val)


def L(name, *args, **kw):
    return lambda e: getattr(e, name)(*args, **kw)

def rms_scale(P, ss, rstd, n, epsb, extra=1.0, cols=1):
    e2 = float(extra) ** 2
    P.op("act", L("activation", out=rstd[:, 0:cols], in_=ss[:, 0:cols], func=AF.Sqrt,
                  bias=epsb[:, 0:1], scale=1.0 / (n * e2)),
         reads=[ss.b(), epsb.b()], writes=[rstd.b()])
    P.op("dve", L("reciprocal", rstd[:, 0:cols], rstd[:, 0:cols]), reads=[rstd.b()], writes=[rstd.b()])


def make_consts(P, id_d, ld):
    C = {}
    C["idf"] = P.sb([128, 128], F32)
    C["idb"] = P.sb([128, 128], BF16)
    P.op("sp", L("dma_start", out=C["idf"][:, :], in_=id_d[:, :]), writes=[C["idf"].b()], chan=ld)
    P.op("dve", L("tensor_copy", C["idb"][:, :], C["idf"][:, :]), reads=[C["idf"].b()], writes=[C["idb"].b()])
    for nm, v in (("eps1", EPS), ("eps4", 4.0 * EPS)):
        C[nm] = P.sb([128, 1], F32)
        P.op("dve", L("memset", C[nm][:, :], v), writes=[C[nm].b()])
    return C


def prenorm_T(P, C, xsrc, xb, g_bc, hT, hT_b, col0, st):
    P.op("act", L("activation", out=st["sq"][:, :], in_=xsrc, func=AF.Square, accum_out=st["ss"][:, 0:1]),
         reads=[xb], writes=[st["sq"].b(), st["ss"].b()])
    rms_scale(P, st["ss"], st["rstd"], D, C["eps1"])
    P.op("dve", L("scalar_tensor_tensor", out=st["hb"][:, :], in0=xsrc, scalar=st["rstd"][:, 0:1], in1=g_bc[:, :],
                  op0=ALU.mult, op1=ALU.mult),
         reads=[xb, st["rstd"].b(), g_bc.b()], writes=[st["hb"].b()])
    for k in range(8):
        P.op("pe", L("transpose", st["tp"][:, k * 128:(k + 1) * 128], st["hb"][:, k * 128:(k + 1) * 128], C["idb"][:, :]),
             reads=[st["hb"].b(), C["idb"].b()], writes=[st["tp"].b()])
    P.op("act", L("copy", out=hT[:, :, col0:col0 + 128], in_=st["tp"][:, :].rearrange("p (k t) -> p k t", k=8)),
         reads=[st["tp"].b()], writes=[hT_b])


def prenorm_scratch(P):
    return {"sq": P.sb([128, D], F32), "ss": P.sb([128, 1], F32), "rstd": P.sb([128, 1], F32),
            "hb": P.sb([128, D], BF16), "tp": P.ps([128, D], BF16)}


def bcast_load(P, dst, src_d, n, ld):
    P.op("sp", L("dma_start", out=dst[:, :], in_=src_d[0:1, 0:n].partition_broadcast(128)), writes=[dst.b()], chan=ld)


def build_ffn(ntok=NTOK, tg=TG):
    nc = bass.Bass("TRN2", target_bir_lowering=False)
    P = Prog(nc)
    x = P.dram("x", [ntok, D], F32, kind="ExternalInput")
    gpre_d = P.dram("g_pre", [1, D], F32, kind="ExternalInput")
    gpost_d = P.dram("g_post", [1, D], F32, kind="ExternalInput")
    wg_d = P.dram("w_gate", [D, DFF], F32, kind="ExternalInput")
    wu_d = P.dram("w_up", [D, DFF], F32, kind="ExternalInput")
    wd_d = P.dram("w_down", [DFF, D], F32, kind="ExternalInput")
    id_d = P.dram("ident", [128, 128], F32, kind="ExternalInput")
    y = P.dram("y", [ntok, D], F32, kind="ExternalOutput")
    ld = P.dma_chan("ld")
    wl = P.dma_chan("wl")
    stc = P.dma_chan("st")
    ntt = tg // 128
    ntb = tg // 512
    C = make_consts(P, id_d, ld)
    gpre = P.sb([128, D], F32)
    gpost = P.sb([128, D], F32)
    bcast_load(P, gpre, gpre_d, D, ld)
    bcast_load(P, gpost, gpost_d, D, ld)
    xg = P.sb([128, ntt, D], F32)
    st = prenorm_scratch(P)
    hT = P.sb([128, 8, tg], BF16)
    wg = [P.sb([128, 8, 256], BF16) for _ in range(2)]
    wu = [P.sb([128, 8, 256], BF16) for _ in range(2)]
    wd = P.sb([128, NFC, D], BF16)
    aT = P.sb([128, NFC, tg], BF16)
    sg = [P.sb([128, 512], F32) for _ in range(2)]
    ss2 = [P.sb([128, 2], F32) for _ in range(2)]
    ss2s = [P.sb([128, 1], F32) for _ in range(2)]
    rstd2 = [P.sb([128, 1], F32) for _ in range(2)]
    yt = [P.sb([128, D], F32) for _ in range(2)]
    pg = [P.ps([128, 512], F32) for _ in range(2)]
    pu = [P.ps([128, 512], F32) for _ in range(2)]
    py = [P.ps([128, 512], F32) for _ in range(2)]
    for fc in range(NFC):
        P.op("pool", L("dma_start", out=wd[:, fc, :], in_=wd_d[fc * 128:(fc + 1) * 128, :]), writes=[wd.b(fc)], chan=wl)
    for g in range(ntok // tg):
        t0 = g * tg
        for tt in range(ntt):
            r0 = t0 + tt * 128
            P.op("sp", L("dma_start", out=xg[:, tt, :], in_=x[r0:r0 + 128, :]), writes=[xg.b(tt)], chan=ld)
            prenorm_T(P, C, xg[:, tt, :], xg.b(tt), gpre, hT, hT.b(tt), tt * 128, st)
        for fg in range(NFC // 2):
            s = fg % 2
            f0 = fg * 256
            P.op("pool", L("dma_start", out=wg[s][:, :, :], in_=wg_d[:, f0:f0 + 256].rearrange("(k p) f -> p k f", p=128)),
                 writes=[wg[s].b()], chan=wl)
            P.op("pool", L("dma_start", out=wu[s][:, :, :], in_=wu_d[:, f0:f0 + 256].rearrange("(k p) f -> p k f", p=128)),
                 writes=[wu[s].b()], chan=wl)
            for c2 in range(2):
                fc = fg * 2 + c2
                for tb in range(ntb):
                    ps_ = (c2 * ntb + tb) % 2
                    hreads = [hT.b(tt) for tt in range(tb * 4, tb * 4 + 4)]
                    for k in range(8):
                        P.op("pe", L("matmul", pg[ps_][:, :], lhsT=wg[s][:, k, c2 * 128:(c2 + 1) * 128],
                                     rhs=hT[:, k, tb * 512:(tb + 1) * 512], start=(k == 0), stop=(k == 7)),
                             reads=[wg[s].b()] + hreads, writes=[pg[ps_].b()])
                    for k in range(8):
                        P.op("pe", L("matmul", pu[ps_][:, :], lhsT=wu[s][:, k, c2 * 128:(c2 + 1) * 128],
                                     rhs=hT[:, k, tb * 512:(tb + 1) * 512], start=(k == 0), stop=(k == 7)),
                             reads=[wu[s].b()] + hreads, writes=[pu[ps_].b()])
                    P.op("act", L("activation", out=sg[ps_][:, :], in_=pg[ps_][:, :], func=AF.Silu),
                         reads=[pg[ps_].b()], writes=[sg[ps_].b()])
                    P.op("dve", L("tensor_tensor", out=aT[:, fc, tb * 512:(tb + 1) * 512], in0=pu[ps_][:, :],
                                  in1=sg[ps_][:, :], op=ALU.mult),
                         reads=[sg[ps_].b(), pu[ps_].b()], writes=[aT.b((fc, tb))])
        for tt in range(ntt):
            s = tt % 2
            tb = tt // 4
            for nh in range(2):
                for fc in range(NFC):
                    P.op("pe", L("matmul", py[nh][:, :], lhsT=aT[:, fc, tt * 128:(tt + 1) * 128],
                                 rhs=wd[:, fc, nh * 512:(nh + 1) * 512], start=(fc == 0), stop=(fc == NFC - 1)),
                         reads=[aT.b((fc, tb)), wd.b(fc)], writes=[py[nh].b()])
                P.op("act", L("activation", out=st["sq"][:, nh * 512:(nh + 1) * 512], in_=py[nh][:, :],
                              func=AF.Square, accum_out=ss2[s][:, nh:nh + 1]),
                     reads=[py[nh].b()], writes=[st["sq"].b(), ss2[s].b(nh)])
            P.op("dve", L("tensor_tensor", out=ss2s[s][:, 0:1], in0=ss2[s][:, 0:1], in1=ss2[s][:, 1:2], op=ALU.add),
                 reads=[ss2[s].b(0), ss2[s].b(1)], writes=[ss2s[s].b()])
            rms_scale(P, ss2s[s], rstd2[s], D, C["eps4"], extra=0.5)
            for nh in range(2):
                P.op("dve", L("scalar_tensor_tensor", out=yt[s][:, nh * 512:(nh + 1) * 512], in0=py[nh][:, :],
                              scalar=rstd2[s][:, 0:1], in1=gpost[:, nh * 512:(nh + 1) * 512], op0=ALU.mult, op1=ALU.mult),
                     reads=[py[nh].b(), rstd2[s].b(), gpost.b()], writes=[yt[s].b(nh)])
            P.op("dve", L("tensor_tensor", out=yt[s][:, :], in0=yt[s][:, :], in1=xg[:, tt, :], op=ALU.add),
                 reads=[yt[s].b(0), yt[s].b(1), xg.b(tt)], writes=[yt[s].b(0), yt[s].b(1)])
            r0 = t0 + tt * 128
            P.op("sp", L("dma_start", out=y[r0:r0 + 128, :], in_=yt[s][:, :]),
                 reads=[yt[s].b(0), yt[s].b(1)], writes=[y.b(r0)], chan=stc)
    P.finish()
    return nc

DIN = 2840
C_Q, C_KC, C_VC, C_KS, C_VS, C_KW, C_VW, C_GT, C_BG, C_CG, C_XC = 0, 512, 640, 768, 896, 1024, 1152, 1280, 1304, 1816, 2328
TWO_PI = 2.0 * math.pi
CW1 = 6.28125
CW2 = TWO_PI - CW1


def build_inproj(ntok=NTOK, stage=99):
    nc = bass.Bass("TRN2", target_bir_lowering=False)
    P = Prog(nc)
    x = P.dram("x", [ntok, D], F32, kind="ExternalInput")
    g_d = P.dram("g_pre", [1, D], F32, kind="ExternalInput")
    win_d = P.dram("w_in", [D, DIN], F32, kind="ExternalInput")
    pos_d = P.dram("pos", [1, ntok], I32, kind="ExternalInput")
    rc_d = P.dram("ropec", [16, 4], F32, kind="ExternalInput")
    id_d = P.dram("ident", [128, 128], F32, kind="ExternalInput")
    qT_d = P.dram("qT", [8, 64, ntok], BF16, kind="ExternalOutput")
    qrT_d = P.dram("qrT", [8, 64, ntok], BF16, kind="ExternalOutput")
    ksT_d = P.dram("ksT", [2, 64, ntok], BF16, kind="ExternalOutput")
    kwT_d = P.dram("kwT", [2, 64, ntok], BF16, kind="ExternalOutput")
    kcvc_d = P.dram("kcvcT", [2, 128, ntok], BF16, kind="ExternalOutput")
    vsw_d = P.dram("vsw", [ntok, 256], BF16, kind="ExternalOutput")
    gates_d = P.dram("gates", [ntok, 24], F32, kind="ExternalOutput")
    bgT_d = P.dram("bgT", [4, 128, ntok], BF16, kind="ExternalOutput")
    uT_d = P.dram("uT", [4, 128, ntok], BF16, kind="ExternalOutput")
    ld = P.dma_chan("ld")
    wl = P.dma_chan("wl")
    stc = P.dma_chan("st")
    C = make_consts(P, id_d, ld)
    gpre = P.sb([128, D], F32)
    bcast_load(P, gpre, g_d, D, ld)
    st = prenorm_scratch(P)
    w = P.sb([128, 8, DIN], BF16)
    for k in range(8):
        for (c0, c1) in ((0, 1304), (1304, DIN)):
            P.op("pool", L("dma_start", out=w[:, k, c0:c1], in_=win_d[k * 128:(k + 1) * 128, c0:c1]),
                 writes=[w.b()], chan=wl)
    wP = P.sb([128, 8, 12, 32], BF16)
    P.op("dve", L("memset", wP[:, :, :, :], 0.0), writes=[wP.b()])
    for (u0, nu, c0) in ((0, 8, C_Q), (8, 2, C_KS), (10, 2, C_KW)):
        src = w[:, :, c0:c0 + nu * 64].rearrange("p k (u d) -> p k u d", d=64)
        P.op("dve", L("tensor_scalar", wP[:, :, u0:u0 + nu, 0:8], src[:, :, :, 8:16], -1.0, None, ALU.mult),
             reads=[w.b()], writes=[wP.b()])
        P.op("dve", L("tensor_copy", wP[:, :, u0:u0 + nu, 8:16], src[:, :, :, 0:8]), reads=[w.b()], writes=[wP.b()])
    wt = P.sb([128, 8, 280], BF16)
    for (d0, c0, n) in ((0, C_VS, 128), (128, C_VW, 128), (256, C_GT, 24)):
        P.op("dve", L("tensor_copy", wt[:, :, d0:d0 + n], w[:, :, c0:c0 + n]), reads=[w.b()], writes=[wt.b()])
    rc = P.sb([16, 4], F32)
    P.op("sp", L("dma_start", out=rc[:, :], in_=rc_d[:, :]), writes=[rc.b()], chan=ld)
    ctab = P.sb([32, 512], F32)
    stab = P.sb([32, 512], F32)
    P.op("dve", L("memset", ctab[:, :], 1.0), writes=[ctab.b()])
    P.op("dve", L("memset", stab[:, :], 0.0), writes=[stab.b()])
    posi = P.sb([16, 512], I32)
    ang = P.sb([16, 512], F32)
    rr = P.sb([16, 512], F32)
    sn = P.sb([16, 512], F32)
    xt = [P.sb([128, D], F32) for _ in range(2)]
    hT = P.sb([128, 8, 512], BF16)
    pq = [P.ps([64, 512], F32) for _ in range(2)]
    ppq = [P.ps([32, 512], F32) for _ in range(2)]
    pm = [P.ps([128, 512], F32) for _ in range(2)]
    ptk = P.ps([128, 280], F32)
    t1 = [P.sb([32, 512], F32) for _ in range(2)]
    t2 = [P.sb([32, 512], F32) for _ in range(2)]
    ob = [P.sb([64, 512], BF16) for _ in range(2)]
    obr = [P.sb([64, 512], BF16) for _ in range(2)]
    om = [P.sb([128, 512], BF16) for _ in range(2)]
    cgs = [P.sb([128, 512], F32) for _ in range(2)]
    otk = [P.sb([128, 256], BF16) for _ in range(2)]
    ogt = [P.sb([128, 24], F32) for _ in range(2)]
    units = [(h, C_Q + 64 * h, qT_d, h) for h in range(8)] + \
            [(8 + g, C_KS + 64 * g, ksT_d, g) for g in range(2)] + \
            [(10 + g, C_KW + 64 * g, kwT_d, g) for g in range(2)]
    nm = 0
    nu_ = 0
    for blk in range(ntok // 512):
        b0 = blk * 512
        P.op("sp", L("dma_start", out=posi[:, :], in_=pos_d[0:1, b0:b0 + 512].partition_broadcast(16)),
             writes=[posi.b()], chan=ld)
        P.op("dve", L("tensor_copy", ang[:, :], posi[:, :]), reads=[posi.b()], writes=[ang.b()])
        P.op("dve", L("tensor_scalar", ang[:, :], ang[:, :], rc[:, 0:1], None, ALU.mult),
             reads=[ang.b(), rc.b()], writes=[ang.b()])
        for (tab, shift) in ((stab, 0.0), (ctab, 0.5 * math.pi)):
            P.op("dve", L("tensor_scalar", rr[:, :], ang[:, :], 1.0 / TWO_PI, shift / TWO_PI, ALU.mult, ALU.add),
                 reads=[ang.b()], writes=[rr.b()])
            P.op("dve", L("tensor_copy", posi[:, :], rr[:, :]), reads=[rr.b()], writes=[posi.b()])
            P.op("dve", L("tensor_copy", rr[:, :], posi[:, :]), reads=[posi.b()], writes=[rr.b()])
            P.op("dve", L("scalar_tensor_tensor", out=sn[:, :], in0=rr[:, :], scalar=-CW1, in1=ang[:, :],
                          op0=ALU.mult, op1=ALU.add), reads=[rr.b(), ang.b()], writes=[sn.b()])
            P.op("dve", L("scalar_tensor_tensor", out=sn[:, :], in0=rr[:, :], scalar=-CW2, in1=sn[:, :],
                          op0=ALU.mult, op1=ALU.add), reads=[rr.b(), sn.b()], writes=[sn.b()])
            if shift:
                P.op("dve", L("tensor_scalar", sn[:, :], sn[:, :], shift, None, ALU.add), reads=[sn.b()], writes=[sn.b()])
            P.op("dve", L("tensor_scalar", rr[:, :], sn[:, :], math.pi, -TWO_PI, ALU.is_gt, ALU.mult),
                 reads=[sn.b()], writes=[rr.b()])
            P.op("dve", L("tensor_tensor", out=sn[:, :], in0=sn[:, :], in1=rr[:, :], op=ALU.add),
                 reads=[sn.b(), rr.b()], writes=[sn.b()])
            P.op("dve", L("tensor_scalar", sn[:, :], sn[:, :], math.pi, -math.pi, ALU.min, ALU.max),
                 reads=[sn.b()], writes=[sn.b()])
            P.op("act", L("activation", out=tab[0:16, :], in_=sn[:, :], func=AF.Sin), reads=[sn.b()], writes=[tab.b()])
        for tt in range(4):
            r0 = b0 + tt * 128
            s = tt % 2
            P.op("sp", L("dma_start", out=xt[s][:, :], in_=x[r0:r0 + 128, :]), writes=[xt[s].b()], chan=ld)
            prenorm_T(P, C, xt[s][:, :], xt[s].b(), gpre, hT, hT.b(tt), tt * 128, st)
        hreads = [hT.b(tt) for tt in range(4)]
        if stage < 1:
            continue
        for (u, c0, dst, di) in units:
            s = nu_ % 2
            nu_ += 1
            for k in range(8):
                P.op("pe", L("matmul", pq[s][:, :], lhsT=w[:, k, c0:c0 + 64], rhs=hT[:, k, :], start=(k == 0), stop=(k == 7)),
                     reads=[w.b()] + hreads, writes=[pq[s].b()])
            for k in range(8):
                P.op("pe", L("matmul", ppq[s][:, :], lhsT=wP[:, k, u, :], rhs=hT[:, k, :], start=(k == 0), stop=(k == 7)),
                     reads=[wP.b()] + hreads, writes=[ppq[s].b()])
            P.op("dve", L("tensor_tensor", out=t1[s][:, :], in0=pq[s][0:32, :], in1=ctab[:, :], op=ALU.mult),
                 reads=[pq[s].b(), ctab.b()], writes=[t1[s].b()])
            P.op("dve", L("tensor_tensor", out=t2[s][:, :], in0=ppq[s][:, :], in1=stab[:, :], op=ALU.mult),
                 reads=[ppq[s].b(), stab.b()], writes=[t2[s].b()])
            P.op("dve", L("tensor_tensor", out=ob[s][0:32, :], in0=t1[s][:, :], in1=t2[s][:, :], op=ALU.add),
                 reads=[t1[s].b(), t2[s].b()], writes=[ob[s].b(0)])
            P.op("act", L("copy", out=ob[s][32:64, :], in_=pq[s][32:64, :]), reads=[pq[s].b()], writes=[ob[s].b(1)])
            if u < 8:
                P.op("act", L("copy", out=obr[s][:, :], in_=pq[s][:, :]), reads=[pq[s].b()], writes=[obr[s].b()])
                P.op("sp", L("dma_start", out=qrT_d[di, :, b0:b0 + 512], in_=obr[s][:, :]),
                     reads=[obr[s].b()], writes=[qrT_d.b((di, blk))], chan=stc)
            P.op("sp", L("dma_start", out=dst[di, :, b0:b0 + 512], in_=ob[s][:, :]),
                 reads=[ob[s].b(0), ob[s].b(1)], writes=[dst.b((di, blk))], chan=stc)
        if stage < 2:
            continue
        for (c0, dst, di) in ((C_KC, kcvc_d, 0), (C_VC, kcvc_d, 1)) + tuple((C_BG + 128 * c, bgT_d, c) for c in range(4)):
            s = nm % 2
            nm += 1
            for k in range(8):
                P.op("pe", L("matmul", pm[s][:, :], lhsT=w[:, k, c0:c0 + 128], rhs=hT[:, k, :], start=(k == 0), stop=(k == 7)),
                     reads=[w.b()] + hreads, writes=[pm[s].b()])
            P.op("act", L("copy", out=om[s][:, :], in_=pm[s][:, :]), reads=[pm[s].b()], writes=[om[s].b()])
            P.op("sp", L("dma_start", out=dst[di, :, b0:b0 + 512], in_=om[s][:, :]),
                 reads=[om[s].b()], writes=[dst.b((di, blk))], chan=stc)
        for c in range(4):
            s0 = nm % 2
            s1 = (nm + 1) % 2
            nm += 2
            for (s, c0) in ((s0, C_CG + 128 * c), (s1, C_XC + 128 * c)):
                for k in range(8):
                    P.op("pe", L("matmul", pm[s][:, :], lhsT=w[:, k, c0:c0 + 128], rhs=hT[:, k, :], start=(k == 0), stop=(k == 7)),
                         reads=[w.b()] + hreads, writes=[pm[s].b()])
            P.op("act", L("copy", out=cgs[c % 2][:, :], in_=pm[s0][:, :]), reads=[pm[s0].b()], writes=[cgs[c % 2].b()])
            P.op("dve", L("tensor_tensor", out=om[s0][:, :], in0=pm[s1][:, :], in1=cgs[c % 2][:, :], op=ALU.mult),
                 reads=[cgs[c % 2].b(), pm[s1].b()], writes=[om[s0].b()])
            P.op("sp", L("dma_start", out=uT_d[c, :, b0:b0 + 512], in_=om[s0][:, :]),
                 reads=[om[s0].b()], writes=[uT_d.b((c, blk))], chan=stc)
        if stage < 3:
            continue
        for tt in range(4):
            s = tt % 2
            r0 = b0 + tt * 128
            for k in range(8):
                P.op("pe", L("matmul", ptk[:, :], lhsT=hT[:, k, tt * 128:(tt + 1) * 128], rhs=wt[:, k, :],
                             start=(k == 0), stop=(k == 7)),
                     reads=[wt.b(), hT.b(tt)], writes=[ptk.b()])
            P.op("act", L("copy", out=otk[s][:, :], in_=ptk[:, 0:256]), reads=[ptk.b()], writes=[otk[s].b()])
            P.op("act", L("copy", out=ogt[s][:, :], in_=ptk[:, 256:280]), reads=[ptk.b()], writes=[ogt[s].b()])
            P.op("sp", L("dma_start", out=vsw_d[r0:r0 + 128, :], in_=otk[s][:, :]),
                 reads=[otk[s].b()], writes=[vsw_d.b(r0)], chan=stc)
            P.op("sp", L("dma_start", out=gates_d[r0:r0 + 128, :], in_=ogt[s][:, :]),
                 reads=[ogt[s].b()], writes=[gates_d.b(r0)], chan=stc)
    P.finish()
    return nc


def rope_consts():
    half = 8
    invf = (np.float32(500000.0) ** (-np.arange(half, dtype=np.float32) * np.float32(2.0) / np.float32(16.0))).astype(np.float32)
    rc = np.zeros((16, 4), np.float32)
    rc[:, 0] = np.concatenate([invf, invf])
    rc[:, 1] = -math.pi
    return rc

SCALE = 0.125
LOOKAHEAD = 2
NEGB = -30000.0
GELU_C = 0.7978845608028654


def build_attn(jlist=None, ntok=NTOK, debug=False):
    if jlist is None:
        jlist = list(range(ntok // 128))
    nq = ntok // 128
    nc = bass.Bass("TRN2", target_bir_lowering=False)
    P = Prog(nc)
    EI = "ExternalInput"
    qT_d = P.dram("qT", [8, 64, ntok], BF16, kind=EI)
    qrT_d = P.dram("qrT", [8, 64, ntok], BF16, kind=EI)
    ksT_d = P.dram("ksT", [2, 64, SEQ], BF16, kind=EI)
    vs_d = P.dram("vs", [SEQ, 128], BF16, kind=EI)
    kwin_d = P.dram("kwin", [nq, 2, 64, 640], BF16, kind=EI)
    vwin_d = P.dram("vwin", [nq, 640, 128], BF16, kind=EI)
    kc2_d = P.dram("kc2", [2, 2, 128, 8192], BF16, kind=EI)
    gates_d = P.dram("gates", [ntok, 24], F32, kind=EI)
    bgT_d = P.dram("bgT", [4, 128, ntok], BF16, kind=EI)
    uT_d = P.dram("uT", [4, 128, ntok], BF16, kind=EI)
    uhalo_d = P.dram("uhalo", [nq, 4, 128, 2], BF16, kind=EI)
    x1_d = P.dram("x1", [ntok, D], F32, kind=EI)
    pe2_d = P.dram("pe2", [2, 128, 16], F32, kind=EI)
    w1_d = P.dram("w1", [2, 2048, 256], F32, kind=EI)
    w2_d = P.dram("w2", [2, 256, 64], F32, kind=EI)
    convw_d = P.dram("convw", [128, 12], F32, kind=EI)
    ga_d = P.dram("ga", [1, 512], F32, kind=EI)
    gc_d = P.dram("gc", [128, 4], F32, kind=EI)
    wout_d = P.dram("w_out", [D, D], F32, kind=EI)
    gpost_d = P.dram("g_post", [1, D], F32, kind=EI)
    id_d = P.dram("ident", [128, 128], F32, kind=EI)
    onehot_d = P.dram("onehot", [64, SEQ], BF16, kind=EI)
    amat_d = P.dram("amat", [1024, 256], BF16, kind=EI)
    masks_d = P.dram("masks", [128, 16, 128], BF16, kind=EI)
    fpat_d = P.dram("fpat", [128, 768], BF16, kind=EI)
    x2_d = P.dram("x2", [ntok, D], F32, kind="ExternalOutput")
    if debug:
        dbg_o = P.dram("dbg_o", [ntok, 3, 512], F32, kind="ExternalOutput")
        dbg_psl = P.dram("dbg_psl", [ntok, 2, 256], F32, kind="ExternalOutput")
        dbg_bias = P.dram("dbg_bias", [ntok, 2, 256], BF16, kind="ExternalOutput")
    ld = P.dma_chan("ld")
    wl = P.dma_chan("wl")
    stc = P.dma_chan("st")

    C = make_consts(P, id_d, ld)
    idb, idf = C["idb"], C["idf"]
    Kaug = P.sb([128, 2, SEQ], BF16)
    arena = P.sb([128, 16640], BF16)
    Vaug = arena[:, :].rearrange("p (t g e) -> p t g e", t=128, g=2, e=65)
    X2 = arena[:, 0:8192]
    w1b = arena[:, 8192:12288].rearrange("p (j c) -> p j c", j=16)
    hg = arena[:, 12288:13312].rearrange("p (c n) -> p c n", c=2)
    b_x2, b_w1, b_hg = arena.b("x2"), arena.b("w1"), arena.b("hg")
    wout = P.sb([128, 8, D], BF16)
    kcmpT = P.sb([64, 2, 1024], BF16)
    AV = P.sb([128, 8, 2, 321], BF16)
    masks = P.sb([128, 16, 128], BF16)
    fpat = P.sb([128, 768], BF16)
    gpost = P.sb([128, D], F32)
    ga = P.sb([128, 512], F32)
    gc = P.sb([128, 4], F32)
    cw = P.sb([128, 12], F32)
    ones_f = P.sb([128, 1], F32)
    QTs = [P.sb([64, 8, 128], BF16) for _ in range(2)]
    QRs = [P.sb([64, 8, 128], BF16) for _ in range(2)]
    Qaug = P.sb([128, 4, 2, 512], BF16)
    kwts = [P.sb([64, 2, 640], BF16) for _ in range(2)]
    vwas = [P.sb([128, 5, 2, 65], BF16) for _ in range(2)]
    gts = [P.sb([128, 24], F32) for _ in range(2)]
    sgt = P.sb([128, 24], F32)
    uts = [P.sb([128, 4, 130], BF16) for _ in range(2)]
    bgts = [P.sb([128, 4, 128], BF16) for _ in range(2)]
    cvy = P.sb([128, 128], F32)
    cvt = P.sb([128, 128], F32)
    cvq = P.sb([128, 4, 128], F32)
    x1t = P.sb([128, D], F32)
    EP = [P.sb([128, 512], BF16) for _ in range(4)]
    osb = [P.sb([65, 512], F32) for _ in range(2)]
    psl = P.sb([128, 256], F32)
    score = P.sb([128, 256], F32)
    swk = P.sb([128, 256], F32)
    m8 = P.sb([128, 16], F32)
    biaspad = P.sb([128, 320], BF16)
    oacc = P.sb([128, 512], F32)
    attb = P.sb([128, 512], BF16)
    mT = P.sb([128, 8, 128], BF16)
    hA = P.sb([128, D], F32)
    sq = P.sb([128, D], F32)
    sm = P.sb([128, 16], F32)
    b_sm = [sm.b(i) for i in range(16)]
    gx = P.sb([128, 512], F32)
    gtmp = P.sb([128, 512], F32)
    cb = P.sb([128, 2], F32)
    pe2f = P.sb([128, 16], F32)
    pe2b = P.sb([128, 16], BF16)
    w2b = P.sb([128, 2, 64], BF16)
    S = [P.ps([128, 512], F32) for _ in range(4)]
    G = [P.ps([128, 512], F32) for _ in range(3)]
    GB = P.ps([128, 1024], BF16)
    print("sbuf left", nc.sbuf_bytes_remaining, flush=True)

    def sp_load(dst_ap, src_ap, bufs):
        P.op("sp", L("dma_start", out=dst_ap, in_=src_ap), writes=bufs, chan=ld)

    sp_load(masks[:, :, :], masks_d[:, :, :], [masks.b()])
    sp_load(fpat[:, :], fpat_d[:, :], [fpat.b()])
    bcast_load(P, gpost, gpost_d, D, ld)
    bcast_load(P, ga, ga_d, 512, ld)
    sp_load(gc[:, :], gc_d[:, :], [gc.b()])
    sp_load(cw[:, :], convw_d[:, :], [cw.b()])
    P.op("dve", L("memset", ones_f[:, :], 1.0), writes=[ones_f.b()])
    for g in range(2):
        sp_load(AV[:, :, g, 0:256], amat_d[:, :].rearrange("(ct p) j -> p ct j", p=128), [AV.b(("a", g))])
    P.op("dve", L("memset", AV[:, :, :, 320:321], 1.0), writes=[AV.b("one")])
    for vwa in vwas:
        P.op("dve", L("memset", vwa[:, :, :, 64:65], 1.0), writes=[vwa.b("one")])
    P.op("dve", L("memset", kcmpT[:, :, :], 0.0), writes=[kcmpT.b(0), kcmpT.b(1)])
    P.op("dve", L("memset", hg, 0.0), writes=[b_hg])
    P.op("dve", L("memset", biaspad[:, 0:64], 0.0), writes=[biaspad.b("pad")])
    for k in range(8):
        P.op("pool", L("dma_start", out=wout[:, k, :], in_=wout_d[k * 128:(k + 1) * 128, :]), writes=[wout.b(k)], chan=wl)
    for g in range(2):
        sp_load(Kaug[0:64, g, :], ksT_d[g, :, :], [Kaug.b(("k", g))])
        sp_load(Kaug[64:128, g, :], onehot_d[:, :], [Kaug.b(("o", g))])

    for kv in range(2):
        P.op("pool", L("dma_start", out=w1b, in_=w1_d[kv, :, :].rearrange("(j p) c -> p j c", p=128)), writes=[b_w1], chan=wl)
        P.op("pool", L("dma_start", out=w2b[:, :, :], in_=w2_d[kv, :, :].rearrange("(c p) d -> p c d", p=128)),
             writes=[w2b.b()], chan=wl)
        sp_load(pe2f[:, :], pe2_d[kv, :, :], [pe2f.b()])
        P.op("dve", L("tensor_copy", pe2b[:, :], pe2f[:, :]), reads=[pe2f.b()], writes=[pe2b.b()])
        for c2 in range(2):
            for jj in range(16):
                P.op("pe", L("matmul", G[0][:, c2:c2 + 1], lhsT=w1b[:, jj, c2 * 128:(c2 + 1) * 128], rhs=pe2b[:, jj:jj + 1],
                             start=(jj == 0), stop=(jj == 15)), reads=[b_w1, pe2b.b()], writes=[G[0].b()])
        P.op("act", L("copy", out=cb[:, :], in_=G[0][:, 0:2]), reads=[G[0].b()], writes=[cb.b()])
        for g in range(2):
            sp_load(X2, kc2_d[kv, g, :, :], [b_x2])
            for nt in range(2):
                n0 = 512 * nt
                N = 512 if nt == 0 else 511
                for c2 in range(2):
                    Sx = S[c2]
                    for jj in range(16):
                        lo = 8 * n0 + jj
                        P.op("pe", L("matmul", Sx[:, 0:N], lhsT=w1b[:, jj, c2 * 128:(c2 + 1) * 128],
                                     rhs=X2[:, lo:lo + 8 * (N - 1) + 1:8], start=(jj == 0), stop=(jj == 15)),
                             reads=[b_w1, b_x2], writes=[Sx.b()])
                    P.op("act", L("activation", out=gx[:, 0:N], in_=Sx[:, 0:N], func=AF.Identity, bias=cb[:, c2:c2 + 1]),
                         reads=[Sx.b(), cb.b()], writes=[gx.b()])
                    P.op("dve", L("tensor_tensor", out=gtmp[:, 0:N], in0=gx[:, 0:N], in1=gx[:, 0:N], op=ALU.mult),
                         reads=[gx.b()], writes=[gtmp.b()])
                    P.op("dve", L("tensor_scalar", gtmp[:, 0:N], gtmp[:, 0:N], 0.044715, 1.0, ALU.mult, ALU.add),
                         reads=[gtmp.b()], writes=[gtmp.b()])
                    P.op("dve", L("tensor_tensor", out=gtmp[:, 0:N], in0=gtmp[:, 0:N], in1=gx[:, 0:N], op=ALU.mult),
                         reads=[gtmp.b(), gx.b()], writes=[gtmp.b()])
                    P.op("act", L("activation", out=gtmp[:, 0:N], in_=gtmp[:, 0:N], func=AF.Tanh, scale=GELU_C),
                         reads=[gtmp.b()], writes=[gtmp.b()])
                    P.op("dve", L("tensor_scalar", gtmp[:, 0:N], gtmp[:, 0:N], 0.5, 0.5, ALU.mult, ALU.add),
                         reads=[gtmp.b()], writes=[gtmp.b()])
                    P.op("dve", L("tensor_tensor", out=hg[:, c2, 0:N], in0=gtmp[:, 0:N], in1=gx[:, 0:N], op=ALU.mult),
                         reads=[gtmp.b(), gx.b()], writes=[b_hg])
                if kv == 0:
                    for c2 in range(2):
                        P.op("pe", L("matmul", S[2][0:64, 0:N], lhsT=w2b[:, c2, :], rhs=hg[:, c2, 0:N],
                                     start=(c2 == 0), stop=(c2 == 1)), reads=[w2b.b(), b_hg], writes=[S[2].b()])
                    P.op("act", L("copy", out=kcmpT[:, g, n0:n0 + N], in_=S[2][0:64, 0:N]),
                         reads=[S[2].b()], writes=[kcmpT.b(g)])
                else:
                    for t4 in range(4):
                        ct = nt * 4 + t4
                        for c2 in range(2):
                            P.op("pe", L("matmul", S[2][:, t4 * 64:(t4 + 1) * 64], lhsT=hg[:, c2, t4 * 128:(t4 + 1) * 128],
                                         rhs=w2b[:, c2, :], start=(c2 == 0), stop=(c2 == 1)),
                                 reads=[w2b.b(), b_hg], writes=[S[2].b()])
                    P.op("act", L("copy", out=AV[:, nt * 4:nt * 4 + 4, g, 256:320],
                                  in_=S[2][:, 0:256].rearrange("p (t d) -> p t d", t=4)),
                         reads=[S[2].b()], writes=[AV.b(("v", g, nt))])
    av_reads = {g: [AV.b(("a", g)), AV.b("one"), AV.b(("v", g, 0)), AV.b(("v", g, 1))] for g in range(2)}

    for c in range(8):
        for g in range(2):
            sp_load(Vaug[:, c * 16:(c + 1) * 16, g, 0:64],
                    vs_d[c * 2048:(c + 1) * 2048, g * 64:(g + 1) * 64].rearrange("(t p) d -> p t d", p=128),
                    [arena.b(("v", c, g)), b_x2, b_w1, b_hg])
    P.op("dve", L("memset", Vaug[:, :, :, 64:65], 1.0), writes=[arena.b("vone"), b_x2, b_w1, b_hg])

    def vreads(kt, g):
        return [arena.b(("v", kt // 16, g)), arena.b("vone")]

    def mask_rhs(i):
        return masks[:, i:i + 1, :].to_broadcast([128, 4, 128])

    def branch_epilogue(g, gate_idx, oT, Tt):
        first = False
        P.op("act", L("copy", out=osb[g][:, :], in_=oT[0:65, :]), reads=[oT.b()], writes=[osb[g].b()])
        T = Tt[:, 0:260].rearrange("p (h e) -> p h e", h=4)
        for h in range(4):
            P.op("pe", L("transpose", T[:, h, :], osb[g][:, h * 128:(h + 1) * 128], idf[0:65, 0:65]),
                 reads=[osb[g].b(), idf.b()], writes=[Tt.b()])
        rd = sm[:, 0:4]
        P.op("dve", L("tensor_scalar", rd, T[:, :, 64], 1e-30, None, ALU.max), reads=[Tt.b()], writes=[b_sm[0]])
        P.op("dve", L("reciprocal", rd, rd), reads=[b_sm[0]], writes=[b_sm[0]])
        sg3 = sgt[:, :].rearrange("p (h b) -> p h b", b=3)
        P.op("dve", L("tensor_tensor", out=rd, in0=rd, in1=sg3[:, 4 * g:4 * g + 4, gate_idx], op=ALU.mult),
             reads=[b_sm[0], sgt.b()], writes=[b_sm[0]])
        for h in range(4):
            c0 = (4 * g + h) * 64
            if first:
                P.op("dve", L("tensor_scalar", oacc[:, c0:c0 + 64], T[:, h, 0:64], sm[:, h:h + 1], None, ALU.mult),
                     reads=[Tt.b(), b_sm[0]], writes=[oacc.b(g)])
            else:
                P.op("dve", L("scalar_tensor_tensor", out=oacc[:, c0:c0 + 64], in0=T[:, h, 0:64], scalar=sm[:, h:h + 1],
                              in1=oacc[:, c0:c0 + 64], op0=ALU.mult, op1=ALU.add),
                     reads=[Tt.b(), b_sm[0], oacc.b(g)], writes=[oacc.b(g)])

    cnt = {"s": 0}

    def score_tile(lhsT, lhs_reads, rhs, rhs_reads, mask_i, nslots=4):
        si = cnt["s"] % nslots
        i = cnt["s"] % 4
        cnt["s"] += 1
        P.op("pe", L("matmul", S[si][:, :], lhsT=lhsT, rhs=rhs, start=True, stop=(mask_i is None)),
             reads=lhs_reads + rhs_reads, writes=[S[si].b()])
        if mask_i is not None:
            P.op("pe", L("matmul", S[si][:, :], lhsT=idb[:, :], rhs=mask_rhs(mask_i), start=False, stop=True),
                 reads=[idb.b(), masks.b()], writes=[S[si].b()])
        P.op("act", L("activation", out=EP[i][:, :], in_=S[si][:, :], func=AF.Exp, scale=SCALE),
             reads=[S[si].b()], writes=[EP[i].b()])
        return i

    for jidx, j in enumerate(jlist):
        t0 = j * 128
        QT, QR, kwt, vwa, gt, ut, bgt = (t_[jidx % 2] for t_ in (QTs, QRs, kwts, vwas, gts, uts, bgts))
        sp_load(QT[:, :, :], qT_d[:, :, t0:t0 + 128].rearrange("h d q -> d h q"), [QT.b()])
        sp_load(QR[:, :, :], qrT_d[:, :, t0:t0 + 128].rearrange("h d q -> d h q"), [QR.b()])
        sp_load(kwt[:, :, :], kwin_d[j, :, :, :].rearrange("g d k -> d g k"), [kwt.b()])
        for g in range(2):
            sp_load(vwa[:, :, g, 0:64], vwin_d[j, :, g * 64:(g + 1) * 64].rearrange("(r p) d -> p r d", p=128), [vwa.b(("v", g))])
        sp_load(gt[:, :], gates_d[t0:t0 + 128, :], [gt.b()])
        sp_load(ut[:, :, 2:130], uT_d[:, :, t0:t0 + 128].rearrange("c p q -> p c q"), [ut.b("m")])
        sp_load(ut[:, :, 0:2], uhalo_d[j, :, :, :].rearrange("c p k -> p c k"), [ut.b("h")])
        sp_load(bgt[:, :, :], bgT_d[:, :, t0:t0 + 128].rearrange("c p q -> p c q"), [bgt.b()])
        sp_load(x1t[:, :], x1_d[t0:t0 + 128, :], [x1t.b()])
        P.op("act", L("activation", out=sgt[:, :], in_=gt[:, :], func=AF.Exp, scale=-1.0), reads=[gt.b()], writes=[sgt.b()])
        P.op("dve", L("tensor_scalar", sgt[:, :], sgt[:, :], 1.0, None, ALU.add), reads=[sgt.b()], writes=[sgt.b()])
        P.op("dve", L("reciprocal", sgt[:, :], sgt[:, :]), reads=[sgt.b()], writes=[sgt.b()])
        for g in range(2):
            for w in range((4 * j + 3) // 32 + 1):
                P.op("pool", L("tensor_copy", Qaug[0:64, w, g, :].rearrange("p (h q) -> p h q", h=4), QT[:, 4 * g:4 * g + 4, :]),
                     reads=[QT.b()], writes=[Qaug.b((w, g, 0))])
        for c in range(4):
            P.op("pool", L("tensor_scalar", cvy[:, :], ut[:, c, 0:128], cw[:, 3 * c:3 * c + 1], None, ALU.mult),
                 reads=[ut.b("m"), ut.b("h"), cw.b()], writes=[cvy.b()])
            for k in (1, 2):
                P.op("pool", L("tensor_scalar", cvt[:, :], ut[:, c, k:k + 128], cw[:, 3 * c + k:3 * c + k + 1], None, ALU.mult),
                     reads=[ut.b("m"), ut.b("h"), cw.b()], writes=[cvt.b()])
                P.op("pool", L("tensor_tensor", out=cvy[:, :], in0=cvy[:, :], in1=cvt[:, :], op=ALU.add),
                     reads=[cvt.b(), cvy.b()], writes=[cvy.b()])
            P.op("pool", L("tensor_tensor", out=cvy[:, :], in0=cvy[:, :], in1=bgt[:, c, :], op=ALU.mult),
                 reads=[cvy.b(), bgt.b()], writes=[cvy.b()])
            P.op("pool", L("tensor_scalar", mT[:, 4 + c, :], cvy[:, :], gc[:, c:c + 1], None, ALU.mult),
                 reads=[cvy.b(), gc.b()], writes=[mT.b(4 + c)])
            P.op("pool", L("tensor_tensor", out=cvq[:, c, :], in0=cvy[:, :], in1=cvy[:, :], op=ALU.mult),
                 reads=[cvy.b()], writes=[cvq.b(c)])
        sg3 = sgt[:, :].rearrange("p (h b) -> p h b", b=3)
        nct = (32 * j + 30) // 128 + 1
        nkt = 4 * j + 4
        nW = (nkt - 1) // 32 + 1
        def front(g, part):
            qrhs = QT[:, 4 * g:4 * g + 4, :]
            pc = [S[2], S[3], G[0], G[1]]
            if part == "A":
                cslot = {}

                def cmp_qk(ct):
                    rp = 4 * j - 16 * ct
                    i = cnt["s"] % 2
                    cnt["s"] += 1
                    msk = rp // 4 if rp <= 16 else None
                    P.op("pe", L("matmul", S[i][:, :], lhsT=kcmpT[:, g, ct * 128:(ct + 1) * 128], rhs=QR[:, 4 * g:4 * g + 4, :],
                                 start=True, stop=(msk is None)), reads=[kcmpT.b(g), QR.b()], writes=[S[i].b()])
                    if msk is not None:
                        P.op("pe", L("matmul", S[i][:, :], lhsT=idb[:, :], rhs=mask_rhs(msk), start=False, stop=True),
                             reads=[idb.b(), masks.b()], writes=[S[i].b()])
                    P.op("act", L("activation", out=EP[i][:, :], in_=S[i][:, :], func=AF.Exp, scale=SCALE),
                         reads=[S[i].b()], writes=[EP[i].b()])
                    cslot[ct] = i

                cmp_qk(0)
                for ct in range(nct):
                    if ct + 1 < nct:
                        cmp_qk(ct + 1)
                    i = cslot[ct]
                    for h in range(4):
                        P.op("pe", L("matmul", pc[h][:, 0:321], lhsT=EP[i][:, h * 128:(h + 1) * 128], rhs=AV[:, ct, g, :],
                                     start=(ct == 0), stop=(ct == nct - 1)),
                             reads=[EP[i].b()] + av_reads[g], writes=[pc[h].b()])
            if part == "B":
                rd = sm[:, 4:8]
                for h in range(4):
                    P.op("dve", L("tensor_scalar", sm[:, 4 + h:5 + h], pc[h][:, 320:321], 1e-30, None, ALU.max),
                         reads=[pc[h].b()], writes=[b_sm[1]])
                P.op("dve", L("reciprocal", rd, rd), reads=[b_sm[1]], writes=[b_sm[1]])
                P.op("dve", L("tensor_scalar", psl[:, :], pc[0][:, 0:256], sm[:, 4:5], None, ALU.mult),
                     reads=[pc[0].b(), b_sm[1]], writes=[psl.b()])
                for h in range(1, 4):
                    P.op("dve", L("scalar_tensor_tensor", out=psl[:, :], in0=pc[h][:, 0:256], scalar=sm[:, 4 + h:5 + h],
                                  in1=psl[:, :], op0=ALU.mult, op1=ALU.add),
                         reads=[pc[h].b(), b_sm[1], psl.b()], writes=[psl.b()])
                wc = sm[:, 8:12]
                P.op("dve", L("tensor_tensor", out=wc, in0=rd, in1=sg3[:, 4 * g:4 * g + 4, 0], op=ALU.mult),
                     reads=[b_sm[1], sgt.b()], writes=[b_sm[2]])
                for h in range(4):
                    c0 = (4 * g + h) * 64
                    P.op("dve", L("tensor_scalar", oacc[:, c0:c0 + 64], pc[h][:, 256:320], sm[:, 8 + h:9 + h], None, ALU.mult),
                         reads=[pc[h].b(), b_sm[2]], writes=[oacc.b(g)])
                P.op("dve", L("tensor_tensor", out=score[:, :], in0=psl[:, :], in1=fpat[:, 256 - 8 * j:512 - 8 * j], op=ALU.add),
                     reads=[psl.b(), fpat.b()], writes=[score.b()])
                P.op("dve", L("memset", score[:, 0:1], 1e9), reads=[], writes=[score.b()])
                P.op("dve", L("max", out=m8[:, 0:8], in_=score[:, :]), reads=[score.b()], writes=[m8.b(0)])
                P.op("dve", L("match_replace", out=swk[:, :], in_to_replace=m8[:, 0:8], in_values=score[:, :], imm_value=-3e9),
                     reads=[score.b(), m8.b(0)], writes=[swk.b()])
                P.op("dve", L("max", out=m8[:, 8:16], in_=swk[:, :]), reads=[swk.b()], writes=[m8.b(1)])
                P.op("dve", L("tensor_reduce", out=sm[:, 15:16], in_=m8[:, 8:16], axis=AX.X, op=ALU.min),
                     reads=[m8.b(1)], writes=[b_sm[6]])
                P.op("dve", L("tensor_scalar", swk[:, :], score[:, :], sm[:, 15:16], None, ALU.is_lt),
                     reads=[score.b(), b_sm[6]], writes=[swk.b()])
                P.op("dve", L("tensor_scalar", biaspad[:, 64:320], swk[:, :], NEGB, None, ALU.mult),
                     reads=[swk.b()], writes=[biaspad.b("b")])
                if debug:
                    P.op("sp", L("dma_start", out=dbg_psl[t0:t0 + 128, g, :], in_=psl[:, :]), reads=[psl.b()],
                         writes=[dbg_psl.b((t0, g))], chan=stc)
                    P.op("sp", L("dma_start", out=dbg_bias[t0:t0 + 128, g, :], in_=biaspad[:, 64:320]), reads=[biaspad.b("b")],
                         writes=[dbg_bias.b((t0, g))], chan=stc)
            if part == "C":
                for w in range(nW):
                    P.op("pe", L("transpose", GB[:, w * 128:(w + 1) * 128], biaspad[:, 64 * w:64 * w + 128], idb[:, :]),
                         reads=[biaspad.b("b"), biaspad.b("pad"), idb.b()], writes=[GB.b()])
                for w in range(nW):
                    P.op("act", L("copy", out=Qaug[64:128, w, g, :].rearrange("p (h q) -> p h q", h=4),
                                  in_=GB[64:128, w * 128:(w + 1) * 128].unsqueeze(1).to_broadcast([64, 4, 128])),
                         reads=[GB.b()], writes=[Qaug.b((w, g, 1))])
        if debug:
            P.op("sp", L("dma_start", out=dbg_o[t0:t0 + 128, 0, :], in_=oacc[:, :]), reads=[oacc.b(0), oacc.b(1)],
                 writes=[dbg_o.b((t0, 0))], chan=stc)
        def sel_loop(g, oT, nslots):
            slot = {}
            for n in range(nkt + LOOKAHEAD):
                if n < nkt:
                    kt = n
                    w = kt // 32
                    slot[n] = score_tile(Kaug[:, g, kt * 128:(kt + 1) * 128], [Kaug.b(("k", g)), Kaug.b(("o", g))],
                                         Qaug[:, w, g, :], [Qaug.b((w, g, 0)), Qaug.b((w, g, 1))],
                                         (5 + kt - 4 * j) if kt >= 4 * j else None, nslots)
                m = n - LOOKAHEAD
                if m >= 0:
                    kt = m
                    i = slot[m]
                    P.op("pe", L("matmul", oT[0:65, :], lhsT=Vaug[:, kt, g, :], rhs=EP[i][:, :],
                                 start=(kt == 0), stop=(kt == nkt - 1)), reads=[EP[i].b()] + vreads(kt, g), writes=[oT.b()])

        front(0, "A")
        front(0, "B")
        front(1, "A")
        front(0, "C")
        sel_loop(0, G[2], 2)
        front(1, "B")
        front(1, "C")
        branch_epilogue(0, 1, G[2], G[0])
        sel_loop(1, G[1], 4)
        branch_epilogue(1, 1, G[1], G[2])
        if debug:
            P.op("sp", L("dma_start", out=dbg_o[t0:t0 + 128, 1, :], in_=oacc[:, :]), reads=[oacc.b(0), oacc.b(1)],
                 writes=[dbg_o.b((t0, 1))], chan=stc)
        tiles = [(r, g) for r in range(5) for g in range(2)]
        slot = {}
        for n in range(len(tiles) + LOOKAHEAD):
            if n < len(tiles):
                r, g = tiles[n]
                if j == 0:
                    mi = 9 + r
                else:
                    mi = 14 if r == 0 else (15 if r == 4 else None)
                slot[n] = score_tile(kwt[:, g, r * 128:(r + 1) * 128], [kwt.b()], QT[:, 4 * g:4 * g + 4, :], [QT.b()], mi)
            m = n - LOOKAHEAD
            if m >= 0:
                r, g = tiles[m]
                i = slot[m]
                P.op("pe", L("matmul", G[g][0:65, :], lhsT=vwa[:, r, g, :], rhs=EP[i][:, :],
                             start=(r == 0), stop=(r == 4)), reads=[EP[i].b(), vwa.b(("v", g)), vwa.b("one")], writes=[G[g].b()])
        for g in range(2):
            branch_epilogue(g, 2, G[g], G[2])
        if debug:
            P.op("sp", L("dma_start", out=dbg_o[t0:t0 + 128, 2, :], in_=oacc[:, :]), reads=[oacc.b(0), oacc.b(1)],
                 writes=[dbg_o.b((t0, 2))], chan=stc)
        for c in range(4):
            P.op("pe", L("matmul", G[2][:, 300:301], lhsT=cvq[:, c, :], rhs=ones_f[:, :], start=(c == 0), stop=(c == 3)),
                 reads=[cvq.b(c), ones_f.b()], writes=[G[2].b()])
        P.op("act", L("copy", out=sm[:, 13:14], in_=G[2][:, 300:301]), reads=[G[2].b()], writes=[b_sm[4]])
        P.op("act", L("activation", out=sq[:, 0:512], in_=oacc[:, :], func=AF.Square, accum_out=sm[:, 12:13]),
             reads=[oacc.b(0), oacc.b(1)], writes=[sq.b(), b_sm[4]])
        P.op("act", L("activation", out=sm[:, 12:14], in_=sm[:, 12:14], func=AF.Sqrt, bias=C["eps1"][:, 0:1], scale=1.0 / 512),
             reads=[b_sm[4], C["eps1"].b()], writes=[b_sm[4]])
        P.op("dve", L("reciprocal", sm[:, 12:14], sm[:, 12:14]), reads=[b_sm[4]], writes=[b_sm[4]])
        P.op("dve", L("scalar_tensor_tensor", out=attb[:, :], in0=oacc[:, :], scalar=sm[:, 12:13], in1=ga[:, :],
                      op0=ALU.mult, op1=ALU.mult), reads=[oacc.b(0), oacc.b(1), b_sm[4], ga.b()], writes=[attb.b()])
        for k in range(4):
            P.op("pe", L("transpose", GB[:, k * 128:(k + 1) * 128], attb[:, k * 128:(k + 1) * 128], idb[:, :]),
                 reads=[attb.b(), idb.b()], writes=[GB.b()])
        P.op("act", L("copy", out=mT[:, 0:4, :], in_=GB[:, 0:512].rearrange("p (k q) -> p k q", k=4)),
             reads=[GB.b()], writes=[mT.b(k) for k in range(4)])
        for nh in range(2):
            for k in range(4):
                P.op("pe", L("matmul", S[nh][:, :], lhsT=mT[:, k, :], rhs=wout[:, k, nh * 512:(nh + 1) * 512],
                             start=(k == 0), stop=(k == 3)), reads=[mT.b(k), wout.b(k)], writes=[S[nh].b()])
            for k in range(4, 8):
                P.op("pe", L("matmul", S[2 + nh][:, :], lhsT=mT[:, k, :], rhs=wout[:, k, nh * 512:(nh + 1) * 512],
                             start=(k == 4), stop=(k == 7)), reads=[mT.b(k), wout.b(k)], writes=[S[2 + nh].b()])
        for nh in range(2):
            P.op("act", L("copy", out=hA[:, nh * 512:(nh + 1) * 512], in_=S[nh][:, :]), reads=[S[nh].b()], writes=[hA.b(nh)])
            P.op("dve", L("scalar_tensor_tensor", out=hA[:, nh * 512:(nh + 1) * 512], in0=S[2 + nh][:, :], scalar=sm[:, 13:14],
                          in1=hA[:, nh * 512:(nh + 1) * 512], op0=ALU.mult, op1=ALU.add),
                 reads=[S[2 + nh].b(), b_sm[4], hA.b(nh)], writes=[hA.b(nh)])
        P.op("act", L("activation", out=sq[:, :], in_=hA[:, :], func=AF.Square, accum_out=sm[:, 14:15]),
             reads=[hA.b(0), hA.b(1)], writes=[sq.b(), b_sm[5]])
        P.op("act", L("activation", out=sm[:, 14:15], in_=sm[:, 14:15], func=AF.Sqrt, bias=C["eps1"][:, 0:1], scale=1.0 / D),
             reads=[b_sm[5], C["eps1"].b()], writes=[b_sm[5]])
        P.op("dve", L("reciprocal", sm[:, 14:15], sm[:, 14:15]), reads=[b_sm[5]], writes=[b_sm[5]])
        P.op("dve", L("scalar_tensor_tensor", out=hA[:, :], in0=hA[:, :], scalar=sm[:, 14:15], in1=gpost[:, :],
                      op0=ALU.mult, op1=ALU.mult), reads=[hA.b(0), hA.b(1), b_sm[5], gpost.b()], writes=[hA.b(0), hA.b(1)])
        P.op("dve", L("tensor_tensor", out=hA[:, :], in0=hA[:, :], in1=x1t[:, :], op=ALU.add),
             reads=[hA.b(0), hA.b(1), x1t.b()], writes=[hA.b(0), hA.b(1)])
        P.op("sp", L("dma_start", out=x2_d[t0:t0 + 128, :], in_=hA[:, :]), reads=[hA.b(0), hA.b(1)],
             writes=[x2_d.b(t0)], chan=stc)
    P.finish()
    return nc

import ml_dtypes
NPBF = ml_dtypes.bfloat16
NQB = NTOK // 128


def own_tokens(arr_seq, i):
    a = arr_seq.reshape(NQB, 4, 128, *arr_seq.shape[1:])
    return np.ascontiguousarray(a[:, i].reshape(NTOK, *arr_seq.shape[1:]))


def scatter_tokens(shards):
    a = np.stack([s.reshape(NQB, 128, *s.shape[1:]) for s in shards], axis=1)
    return a.reshape(SEQ, *shards[0].shape[1:])


def full_T(shards):
    lead = shards[0].shape[:-1]
    a = np.stack([s.reshape(*lead, NQB, 128) for s in shards], axis=-2)
    return a.reshape(*lead, SEQ)


def attn_consts(i):
    kk = np.arange(128)[:, None]
    q = np.arange(128)[None, :]
    m = np.zeros((128, 16, 128), np.float32)
    for mi in range(5):
        valid = (16 * kk + 31 - q) <= 128 * (4 * mi + i)
        m[:, mi, :] = np.where(valid, 0.0, NEGB)
    for r in range(4):
        if r == i:
            m[:, 5 + r, :] = np.where(kk > q, NEGB, 0.0)
        elif r > i:
            m[:, 5 + r, :] = NEGB
    for r in range(5):
        diff = 512 - 128 * r + q - kk
        key = 128 * i - 512 + 128 * r + kk
        valid = (key >= 0) & (diff >= 0) & (diff < 512)
        m[:, 9 + r, :] = np.where(valid, 0.0, NEGB)
    m[:, 14, :] = np.where(kk > q, 0.0, NEGB)
    m[:, 15, :] = np.where(kk <= q, 0.0, NEGB)
    fp = np.zeros((128, 768), np.float32)
    for cp in range(768):
        c = cp - 2 * i
        if c < 0:
            continue
        for half, cur in ((slice(0, 64), 256), (slice(64, 128), 257)):
            if c == cur or c == cur - 1:
                fp[half, cp] = 1e9
            elif c > cur:
                fp[half, cp] = -1e9
    return m.astype(NPBF), fp.astype(NPBF)


def static_consts():
    key = np.arange(SEQ)
    onehot = (((key // 64) % 64)[None, :] == np.arange(64)[:, None]).astype(NPBF)
    agg = [1.0, 2.0, 2.0, 2.0, 1.0]
    A = np.zeros((1024, 256), np.float32)
    for jb in range(256):
        for o in range(5):
            n = 4 * jb + o - 1
            if 0 <= n < 1023:
                A[n, jb] = agg[o]
    return onehot, A.astype(NPBF)


def attn_in_maps(batch_outs, x1_shards, params):
    ksT = full_T([o["ksT"] for o in batch_outs])
    kwT = full_T([o["kwT"] for o in batch_outs])
    kcvc = full_T([o["kcvcT"] for o in batch_outs])
    uT = full_T([o["uT"] for o in batch_outs])
    vsw = scatter_tokens([o["vsw"] for o in batch_outs])
    vs = np.ascontiguousarray(vsw[:, :128])
    vw = vsw[:, 128:]
    kc2 = np.ascontiguousarray(kcvc.reshape(2, 2, 64, SEQ // 2, 2).transpose(0, 1, 4, 2, 3).reshape(2, 2, 128, SEQ // 2))
    kw_pad = np.concatenate([np.zeros((2, 64, 512), NPBF), kwT], axis=2)
    vw_pad = np.concatenate([np.zeros((512, 128), NPBF), vw], axis=0)
    u_pad = np.concatenate([np.zeros((4, 128, 2), NPBF), uT], axis=2)
    onehot, amat = static_consts()
    maps = []
    for i in range(4):
        s0 = (4 * np.arange(NQB) + i) * 128
        kwin = np.stack([kw_pad[:, :, s:s + 640] for s in s0])
        vwin = np.stack([vw_pad[s:s + 640] for s in s0])
        uhalo = np.stack([u_pad[:, :, s:s + 2] for s in s0])
        masks, fpat = attn_consts(i)
        o = batch_outs[i]
        m = {"qT": o["qT"], "qrT": o["qrT"], "ksT": ksT, "vs": vs, "kwin": np.ascontiguousarray(kwin), "vwin": np.ascontiguousarray(vwin),
             "kc2": kc2, "gates": o["gates"], "bgT": o["bgT"], "uT": o["uT"], "uhalo": np.ascontiguousarray(uhalo),
             "x1": x1_shards[i], "ident": _ident(), "onehot": onehot, "amat": amat, "masks": masks, "fpat": fpat}
        m.update(params)
        maps.append({k: np.ascontiguousarray(v) for k, v in m.items()})
    return maps


def attn_params(inp, l):
    pe2 = np.stack([inp[n][l].reshape(16, 2, 64).transpose(1, 2, 0).reshape(128, 16) for n in ("cmp_pe_k", "cmp_pe_v")])
    return {
        "pe2": np.ascontiguousarray(pe2), "w1": np.stack([inp["cmp_w1_k"][l], inp["cmp_w1_v"][l]]),
        "w2": np.stack([inp["cmp_w2_k"][l], inp["cmp_w2_v"][l]]),
        "convw": np.ascontiguousarray(inp["conv_w"][l].reshape(3, 4, 128).transpose(2, 1, 0).reshape(128, 12)),
        "ga": inp["attn_out_norm"][l].reshape(1, 512),
        "gc": np.ascontiguousarray(inp["conv_out_norm"][l].reshape(4, 128).T),
        "w_out": inp["w_out"][l], "g_post": inp["mix_norm_post"][l].reshape(1, D),
    }


def _ident():
    return np.eye(128, dtype=np.float32)


def _launch(nc, in_maps):
    res = run_bass_kernel_spmd(nc, in_maps, core_ids=list(range(NCORES)))
    return res.results


def _run_ffn(xs, inp, pref, l):
    nc = build_ffn()
    maps = [{"x": np.ascontiguousarray(x_), "g_pre": np.ascontiguousarray(inp[pref + "_norm_pre"][l].reshape(1, D)),
             "g_post": np.ascontiguousarray(inp[pref + "_norm_post"][l].reshape(1, D)),
             "w_gate": np.ascontiguousarray(inp[pref + "_w_gate"][l]), "w_up": np.ascontiguousarray(inp[pref + "_w_up"][l]),
             "w_down": np.ascontiguousarray(inp[pref + "_w_down"][l]), "ident": _ident()} for x_ in xs]
    return [r["y"] for r in _launch(nc, maps)]


def _run_inproj(xs, pos_shards, inp, l):
    nc = build_inproj()
    rc = rope_consts()
    maps = [{"x": np.ascontiguousarray(x_), "g_pre": np.ascontiguousarray(inp["mix_norm_pre"][l].reshape(1, D)),
             "w_in": np.ascontiguousarray(inp["w_in"][l]), "pos": p_, "ropec": rc, "ident": _ident()}
            for x_, p_ in zip(xs, pos_shards)]
    return _launch(nc, maps)


def _run_attn(outs, xs, inp, l):
    nc = build_attn()
    params = attn_params(inp, l)
    maps = []
    for b in range(2):
        maps += attn_in_maps(outs[4 * b:4 * b + 4], xs[4 * b:4 * b + 4], params)
    return [r["x2"] for r in _launch(nc, maps)]


def kernel(**inp):
    inp = {k: np.asarray(v) for k, v in inp.items()}
    x = inp["x"].astype(np.float32, copy=False)
    pos = inp["positions"].astype(np.int32, copy=False)
    xs = [own_tokens(x[c // 4], c % 4) for c in range(NCORES)]
    ps = [np.ascontiguousarray(own_tokens(pos[c // 4], c % 4).reshape(1, NTOK)) for c in range(NCORES)]
    for l in range(2):
        xs = _run_ffn(xs, inp, "ffn1", l)
        outs = _run_inproj(xs, ps, inp, l)
        xs = _run_attn(outs, xs, inp, l)
        xs = _run_ffn(xs, inp, "ffn2", l)
    out = np.stack([scatter_tokens(xs[4 * b:4 * b + 4]) for b in range(2)])
    return np.ascontiguousarray(out.astype(np.float32, copy=False))
```
